# Optimizing a Trainium2 kernel written in Bass

```python
import math
import jax
import jax.numpy as jnp
from jax import lax
import numpy as np


D_MODEL = 2048
BATCH = 4
SEQ = 4096
DEPTH = 2

HEAD_DIM = 128
N_MIXERS = 4
GROUP_HEADS = D_MODEL // (N_MIXERS * HEAD_DIM)
GROUP_WIDTH = GROUP_HEADS * HEAD_DIM
MIX_WIDTH = N_MIXERS * GROUP_WIDTH
ROPE_THETA = 10000.0
Q_BLOCK = 128
NORM_EPS = 1e-6

DIFF_HEADS = GROUP_HEADS
DIFF_QK_DIM = HEAD_DIM // 2
DIFF_V_DIM = HEAD_DIM

GRID_W = 64
NA_HEADS = GROUP_HEADS
NA_WIN_ROWS = 8
NA_WIN_COLS = 16
NA_QCOLS = 16
NA_KV_COLS = 2 * NA_WIN_COLS

SWA_Q_HEADS = GROUP_HEADS
SWA_KV_HEADS = GROUP_HEADS // 2
SWA_WINDOW = 128

MLA_HEADS = GROUP_HEADS
MLA_Q_LORA = 512
MLA_KV_LORA = 512
MLA_NOPE = 128
MLA_ROPE = 64
MLA_V = HEAD_DIM

DIFF_COLS = 2 * DIFF_HEADS * 2 * DIFF_QK_DIM + DIFF_HEADS * DIFF_V_DIM
NA_COLS = 3 * NA_HEADS * HEAD_DIM
SWA_COLS = (SWA_Q_HEADS + 2 * SWA_KV_HEADS) * HEAD_DIM
MLA_COLS = MLA_Q_LORA + MLA_KV_LORA + MLA_ROPE
IN_COLS = DIFF_COLS + NA_COLS + SWA_COLS + MLA_COLS

FFN_DIM = 7 * D_MODEL // 2
N_EXPERTS = 8
TOP_K = 2
MOE_BLOCK = 512
N_DENSE = (DEPTH + 1) // 2
N_MOE = DEPTH // 2

PLE_DIM = 256

kernel_name = "hybrid_parallel_heads_encoder"


def rms_norm(x, g):
    x32 = x.astype(jnp.float32)
    y = x32 * lax.rsqrt(jnp.mean(x32 * x32, axis=-1, keepdims=True) + NORM_EPS)
    return (y * g.astype(jnp.float32)).astype(x.dtype)


def rope_tables(seq, dim):
    inv = ROPE_THETA ** (-jnp.arange(0, dim, 2, dtype=jnp.float32) / dim)
    ang = jnp.arange(seq, dtype=jnp.float32)[:, None] * inv[None, :]
    ang = jnp.concatenate([ang, ang], axis=-1)
    return jnp.cos(ang), jnp.sin(ang)


def apply_rope(x, cos, sin):
    x32 = x.astype(jnp.float32)
    half = x.shape[-1] // 2
    rot = jnp.concatenate([-x32[..., half:], x32[..., :half]], axis=-1)
    return (x32 * cos + rot * sin).astype(x.dtype)


def to_heads(t, n_heads):
    b, s, _ = t.shape
    return t.reshape(b, s, n_heads, -1).transpose(0, 2, 1, 3)


def from_heads(t):
    b, h, s, d = t.shape
    return t.transpose(0, 2, 1, 3).reshape(b, s, h * d)


def to_q_blocks(t):
    *lead, s, d = t.shape
    t = t.reshape(*lead, s // Q_BLOCK, Q_BLOCK, d)
    return jnp.moveaxis(t, -3, 0)


def from_q_blocks(o):
    o = jnp.moveaxis(o, 0, -3)
    *lead, nq, qb, d = o.shape
    return o.reshape(*lead, nq * qb, d)


def dense_attention(q, k, v, scale):
    def block(qb):
        s = jnp.einsum('bhqd,bhkd->bhqk', qb, k).astype(jnp.float32) * scale
        return jnp.einsum('bhqk,bhkd->bhqd', jax.nn.softmax(s, axis=-1).astype(v.dtype), v)
    return from_q_blocks(lax.map(block, to_q_blocks(q)))


def diff_attention(z, lq1, lk1, lq2, lk2, subln, lambda_init, cos, sin):
    b, s, _ = z.shape
    qk = DIFF_HEADS * 2 * DIFF_QK_DIM
    q = z[..., :qk].reshape(b, s, DIFF_HEADS, 2, DIFF_QK_DIM).transpose(0, 2, 3, 1, 4)
    k = z[..., qk:2 * qk].reshape(b, s, DIFF_HEADS, 2, DIFF_QK_DIM).transpose(0, 2, 3, 1, 4)
    v = to_heads(z[..., 2 * qk:], DIFF_HEADS)
    q = apply_rope(q, cos, sin)
    k = apply_rope(k, cos, sin)
    k1, k2 = k[:, :, 0], k[:, :, 1]
    lam = (jnp.exp(jnp.sum(lq1.astype(jnp.float32) * lk1.astype(jnp.float32)))
           - jnp.exp(jnp.sum(lq2.astype(jnp.float32) * lk2.astype(jnp.float32)))
           + lambda_init)
    scale = DIFF_QK_DIM ** -0.5

    def block(qb):
        s1 = jnp.einsum('bhqd,bhkd->bhqk', qb[:, :, 0], k1).astype(jnp.float32) * scale
        s2 = jnp.einsum('bhqd,bhkd->bhqk', qb[:, :, 1], k2).astype(jnp.float32) * scale
        w = jax.nn.softmax(s1, axis=-1) - lam * jax.nn.softmax(s2, axis=-1)
        return jnp.einsum('bhqk,bhkd->bhqd', w.astype(v.dtype), v)

    o = from_q_blocks(lax.map(block, to_q_blocks(q)))
    o = rms_norm(o, subln) * (1.0 - lambda_init)
    return from_heads(o)


def neighbourhood_attention(z, rpb):
    b, s, _ = z.shape
    w = NA_HEADS * HEAD_DIM
    q = to_heads(z[..., :w], NA_HEADS)
    k = to_heads(z[..., w:2 * w], NA_HEADS)
    v = to_heads(z[..., 2 * w:], NA_HEADS)
    rows = s // GRID_W
    kr = min(NA_WIN_ROWS, rows)
    q5 = q.reshape(b, NA_HEADS, rows, GRID_W, HEAD_DIM)
    k5 = k.reshape(b, NA_HEADS, rows, GRID_W, HEAD_DIM)
    v5 = v.reshape(b, NA_HEADS, rows, GRID_W, HEAD_DIM)
    row_start = np.clip(np.arange(rows) - kr // 2, 0, rows - kr).astype(np.int32)
    nj = GRID_W // NA_QCOLS
    blk_start = np.clip(np.arange(nj) * NA_QCOLS - NA_WIN_COLS // 2, 0, GRID_W - NA_KV_COLS)
    key_cols = blk_start[:, None] + np.arange(NA_KV_COLS)[None, :]
    q_cols = np.arange(GRID_W).reshape(nj, NA_QCOLS)
    win_start = np.clip(q_cols - NA_WIN_COLS // 2, 0, GRID_W - NA_WIN_COLS)
    col_mask = ((key_cols[:, None, :] >= win_start[..., None])
                & (key_cols[:, None, :] < win_start[..., None] + NA_WIN_COLS))
    col_idx = np.clip(key_cols[:, None, :] - q_cols[..., None],
                      -(NA_WIN_COLS - 1), NA_WIN_COLS - 1) + NA_WIN_COLS - 1
    rpb_cols = rpb[:, :, col_idx].astype(jnp.float32)
    scale = HEAD_DIM ** -0.5

    def row_step(args):
        q_row, r, rs = args
        kwin = lax.dynamic_slice_in_dim(k5, rs, kr, axis=2)
        vwin = lax.dynamic_slice_in_dim(v5, rs, kr, axis=2)
        kb = kwin[:, :, :, key_cols]
        vb = vwin[:, :, :, key_cols]
        qb = q_row.reshape(b, NA_HEADS, nj, NA_QCOLS, HEAD_DIM)
        sc = jnp.einsum('bhjqd,bhajcd->bhjqac', qb, kb).astype(jnp.float32) * scale
        row_idx = rs + jnp.arange(kr) - r + NA_WIN_ROWS - 1
        bias = jnp.take(rpb_cols, row_idx, axis=1).transpose(0, 2, 3, 1, 4)
        sc = jnp.where(col_mask[None, None, :, :, None, :], sc + bias[None], -jnp.inf)
        prob = jax.nn.softmax(sc, axis=(-2, -1))
        o = jnp.einsum('bhjqac,bhajcd->bhjqd', prob.astype(v.dtype), vb)
        return o.reshape(b, NA_HEADS, GRID_W, HEAD_DIM)

    out = lax.map(row_step, (jnp.moveaxis(q5, 2, 0), jnp.arange(rows, dtype=jnp.int32),
                             jnp.asarray(row_start)))
    out = jnp.moveaxis(out, 0, 2).reshape(b, NA_HEADS, s, HEAD_DIM)
    return from_heads(out)


def sliding_window_attention(z, sinks, cos, sin):
    b, s, _ = z.shape
    nq = SWA_Q_HEADS * HEAD_DIM
    nkv = SWA_KV_HEADS * HEAD_DIM
    q = apply_rope(to_heads(z[..., :nq], SWA_Q_HEADS), cos, sin)
    k = apply_rope(to_heads(z[..., nq:nq + nkv], SWA_KV_HEADS), cos, sin)
    v = to_heads(z[..., nq + nkv:], SWA_KV_HEADS)
    g = SWA_Q_HEADS // SWA_KV_HEADS
    nb = s // Q_BLOCK
    n_side = SWA_WINDOW // Q_BLOCK
    span = (2 * n_side + 1) * Q_BLOCK

    def band(t):
        tp = jnp.pad(t, ((0, 0), (0, 0), (SWA_WINDOW, SWA_WINDOW), (0, 0)))
        tp = tp.reshape(b, SWA_KV_HEADS, nb + 2 * n_side, Q_BLOCK, HEAD_DIM)
        return jnp.concatenate([tp[:, :, o:o + nb] for o in range(2 * n_side + 1)], axis=3)

    kb, vb = band(k), band(v)
    qb = q.reshape(b, SWA_KV_HEADS, g, nb, Q_BLOCK, HEAD_DIM)
    sc = jnp.einsum('bkgnqd,bkncd->bkgnqc', qb, kb).astype(jnp.float32) * HEAD_DIM ** -0.5
    qpos = jnp.arange(nb)[:, None, None] * Q_BLOCK + jnp.arange(Q_BLOCK)[None, :, None]
    kpos = jnp.arange(nb)[:, None, None] * Q_BLOCK - SWA_WINDOW + jnp.arange(span)[None, None, :]
    valid = (kpos >= 0) & (kpos < s) & (jnp.abs(qpos - kpos) <= SWA_WINDOW)
    sc = jnp.where(valid, sc, -jnp.inf)
    sink = jnp.broadcast_to(sinks.astype(jnp.float32).reshape(1, SWA_KV_HEADS, g, 1, 1, 1),
                            sc.shape[:-1] + (1,))
    prob = jax.nn.softmax(jnp.concatenate([sc, sink], axis=-1), axis=-1)[..., :-1]
    o = jnp.einsum('bkgnqc,bkncd->bkgnqd', prob.astype(v.dtype), vb)
    return from_heads(o.reshape(b, SWA_Q_HEADS, s, HEAD_DIM))


def latent_attention(z, q_norm, kv_norm, w_uq, w_ukv, cos, sin):
    b, s, _ = z.shape
    cq = rms_norm(z[..., :MLA_Q_LORA], q_norm)
    ckv = rms_norm(z[..., MLA_Q_LORA:MLA_Q_LORA + MLA_KV_LORA], kv_norm)
    k_rope = apply_rope(z[..., MLA_Q_LORA + MLA_KV_LORA:][:, None], cos, sin)
    q = to_heads(cq @ w_uq, MLA_HEADS)
    q = jnp.concatenate([q[..., :MLA_NOPE], apply_rope(q[..., MLA_NOPE:], cos, sin)], axis=-1)
    kv = to_heads(ckv @ w_ukv, MLA_HEADS)
    k = jnp.concatenate([kv[..., :MLA_NOPE],
                         jnp.broadcast_to(k_rope, (b, MLA_HEADS, s, MLA_ROPE))], axis=-1)
    v = kv[..., MLA_NOPE:]
    return from_heads(dense_attention(q, k, v, (MLA_NOPE + MLA_ROPE) ** -0.5))


def swiglu(x, w_gate, w_up, w_down):
    return (jax.nn.silu(x @ w_gate) * (x @ w_up)) @ w_down


def moe_swiglu(h, w_router, w_gate, w_up, w_down):
    n, d = h.shape
    logits = (h @ w_router).astype(jnp.float32)
    top_val, top_idx = lax.top_k(logits, TOP_K)
    gates = jax.nn.softmax(top_val, axis=-1)
    n_assign = n * TOP_K
    flat_e = top_idx.reshape(-1)
    flat_tok = jnp.repeat(jnp.arange(n, dtype=jnp.int32), TOP_K)
    flat_g = gates.reshape(-1)
    order = jnp.argsort(flat_e)
    e_s, tok_s, g_s = flat_e[order], flat_tok[order], flat_g[order]
    counts = jnp.bincount(flat_e, length=N_EXPERTS)
    padded = (counts + MOE_BLOCK - 1) // MOE_BLOCK * MOE_BLOCK
    grp_start = jnp.cumsum(counts) - counts
    pad_end = jnp.cumsum(padded)
    pad_start = pad_end - padded
    dest = pad_start[e_s] + jnp.arange(n_assign) - grp_start[e_s]
    n_blk = n_assign // MOE_BLOCK + N_EXPERTS
    buf_tok = jnp.full((n_blk * MOE_BLOCK,), n, dtype=jnp.int32).at[dest].set(tok_s)
    blk_exp = jnp.minimum(jnp.searchsorted(pad_end, jnp.arange(n_blk) * MOE_BLOCK, side='right'),
                          N_EXPERTS - 1)
    h_pad = jnp.concatenate([h, jnp.zeros((1, d), h.dtype)], axis=0)

    def expert_block(args):
        tok, e = args
        xb = h_pad[tok]
        return swiglu(xb, w_gate[e], w_up[e], w_down[e])

    y_buf = lax.map(expert_block, (buf_tok.reshape(n_blk, MOE_BLOCK), blk_exp))
    y = y_buf.reshape(-1, d)[dest]
    return jnp.zeros((n, d), h.dtype).at[tok_s].add(g_s[:, None].astype(h.dtype) * y)


def setup_inputs(seed: int = 0) -> dict:
    key = jax.random.key(seed)
    ks = iter(jax.random.split(key, 32))

    def nrm(shape, scale):
        return jax.random.normal(next(ks), shape, jnp.float32) * scale

    def gain(shape):
        return 1.0 + 0.05 * jax.random.normal(next(ks), shape, jnp.float32)

    return {
        'x': nrm((BATCH, SEQ, D_MODEL), 1.0),
        'p': nrm((DEPTH, BATCH, SEQ, PLE_DIM), 1.0),
        'attn_norm': gain((DEPTH, D_MODEL)),
        'w_in': nrm((DEPTH, D_MODEL, IN_COLS), D_MODEL ** -0.5),
        'diff_lq1': nrm((DEPTH, DIFF_QK_DIM), 0.1),
        'diff_lk1': nrm((DEPTH, DIFF_QK_DIM), 0.1),
        'diff_lq2': nrm((DEPTH, DIFF_QK_DIM), 0.1),
        'diff_lk2': nrm((DEPTH, DIFF_QK_DIM), 0.1),
        'diff_subln': gain((DEPTH, DIFF_V_DIM)),
        'na_rpb': nrm((DEPTH, NA_HEADS, 2 * NA_WIN_ROWS - 1, 2 * NA_WIN_COLS - 1), 0.1),
        'swa_sinks': nrm((DEPTH, SWA_Q_HEADS), 0.5),
        'mla_q_norm': gain((DEPTH, MLA_Q_LORA)),
        'mla_kv_norm': gain((DEPTH, MLA_KV_LORA)),
        'mla_w_uq': nrm((DEPTH, MLA_Q_LORA, MLA_HEADS * (MLA_NOPE + MLA_ROPE)), MLA_Q_LORA ** -0.5),
        'mla_w_ukv': nrm((DEPTH, MLA_KV_LORA, MLA_HEADS * (MLA_NOPE + MLA_V)), MLA_KV_LORA ** -0.5),
        'w_out': nrm((DEPTH, MIX_WIDTH, D_MODEL), MIX_WIDTH ** -0.5),
        'ffn_norm': gain((DEPTH, D_MODEL)),
        'dense_w_gate': nrm((N_DENSE, D_MODEL, FFN_DIM), D_MODEL ** -0.5),
        'dense_w_up': nrm((N_DENSE, D_MODEL, FFN_DIM), D_MODEL ** -0.5),
        'dense_w_down': nrm((N_DENSE, FFN_DIM, D_MODEL), FFN_DIM ** -0.5),
        'moe_router': nrm((N_MOE, D_MODEL, N_EXPERTS), D_MODEL ** -0.5),
        'moe_w_gate': nrm((N_MOE, N_EXPERTS, D_MODEL, FFN_DIM), D_MODEL ** -0.5),
        'moe_w_up': nrm((N_MOE, N_EXPERTS, D_MODEL, FFN_DIM), D_MODEL ** -0.5),
        'moe_w_down': nrm((N_MOE, N_EXPERTS, FFN_DIM, D_MODEL), FFN_DIM ** -0.5),
        'ple_norm': gain((DEPTH, D_MODEL)),
        'ple_gate': nrm((DEPTH, D_MODEL, D_MODEL), D_MODEL ** -0.5),
        'ple_proj': nrm((DEPTH, PLE_DIM, D_MODEL), PLE_DIM ** -0.5),
        'final_norm': gain((D_MODEL,)),
    }


def reference(x, p, attn_norm, w_in, diff_lq1, diff_lk1, diff_lq2, diff_lk2, diff_subln,
              na_rpb, swa_sinks, mla_q_norm, mla_kv_norm, mla_w_uq, mla_w_ukv, w_out,
              ffn_norm, dense_w_gate, dense_w_up, dense_w_down, moe_router, moe_w_gate,
              moe_w_up, moe_w_down, ple_norm, ple_gate, ple_proj, final_norm):
    b, s, _ = x.shape
    cos_d, sin_d = rope_tables(s, DIFF_QK_DIM)
    cos_h, sin_h = rope_tables(s, HEAD_DIM)
    cos_r, sin_r = rope_tables(s, MLA_ROPE)
    o0 = DIFF_COLS
    o1 = o0 + NA_COLS
    o2 = o1 + SWA_COLS
    h = x
    for i in range(DEPTH):
        z = rms_norm(h, attn_norm[i]) @ w_in[i]
        lambda_init = 0.8 - 0.6 * math.exp(-0.3 * i)
        mixed = jnp.concatenate([
            diff_attention(z[..., :o0], diff_lq1[i], diff_lk1[i], diff_lq2[i], diff_lk2[i],
                           diff_subln[i], lambda_init, cos_d, sin_d),
            neighbourhood_attention(z[..., o0:o1], na_rpb[i]),
            sliding_window_attention(z[..., o1:o2], swa_sinks[i], cos_h, sin_h),
            latent_attention(z[..., o2:], mla_q_norm[i], mla_kv_norm[i], mla_w_uq[i],
                             mla_w_ukv[i], cos_r, sin_r),
        ], axis=-1)
        h = h + mixed @ w_out[i]
        hn = rms_norm(h, ffn_norm[i])
        j = i // 2
        if i % 2 == 0:
            h = h + swiglu(hn, dense_w_gate[j], dense_w_up[j], dense_w_down[j])
        else:
            h = h + moe_swiglu(hn.reshape(b * s, -1), moe_router[j], moe_w_gate[j],
                               moe_w_up[j], moe_w_down[j]).reshape(b, s, -1)
        gate = jax.nn.sigmoid(rms_norm(h, ple_norm[i]) @ ple_gate[i])
        h = h + gate * (p[i] @ ple_proj[i])
    return rms_norm(h, final_norm)
```

```python
import math
from contextlib import ExitStack
import numpy as np
import ml_dtypes
import concourse.bass as bass
import concourse.mybir as mybir
from concourse.bass_utils import run_bass_kernel_spmd

F32 = mybir.dt.float32
BF16 = mybir.dt.bfloat16
U8 = mybir.dt.uint8
AF = mybir.ActivationFunctionType
ALU = mybir.AluOpType
AX = mybir.AxisListType

D = 2048
S = 4096
NC16 = 16
FFN = 7168
NFT = FFN // 128
EPS = 1e-6
NEG = -30000.0
SAME_ENGINE_SYNC = True
KSLOT = 8


class Buf:
    __slots__ = ("name", "w", "r")

    def __init__(self, name=""):
        self.name = name
        self.w = None
        self.r = {}


class T:
    __slots__ = ("ap", "buf")

    def __init__(self, ap, buf=None, name=""):
        self.ap = ap
        self.buf = buf if buf is not None else Buf(name)

    def __getitem__(self, k):
        return self.ap[k]


class LazyIn:
    def __init__(self, P, name, shape, dtype):
        self.P, self.name, self.shape, self.dtype = P, name, list(shape), dtype
        self._t = None

    def _get(self):
        if self._t is None:
            t = self.P.nc.dram_tensor(self.name, self.shape, self.dtype, kind="ExternalInput")
            self.P.inputs[self.name] = (tuple(self.shape), self.dtype)
            self._t = T(t.ap(), name=self.name)
        return self._t

    @property
    def ap(self):
        return self._get().ap

    @property
    def buf(self):
        return self._get().buf


def _bufs(xs):
    out = []
    for x in xs:
        if x is None:
            continue
        out.append(x.buf if isinstance(x, (T, LazyIn)) else x)
    return out


class Sched:
    def __init__(self, nc, es):
        self.nc = nc
        self.names = ["pe", "act", "dve", "pool", "sp"]
        self.ops = {e: [] for e in self.names}
        self.cnt = {e: 0 for e in self.names}
        self.seen = {e: {} for e in self.names}
        self.sem = {e: es.enter_context(nc.semaphore("c_" + e)) for e in self.names}
        self.dq = {}
        for q in ("sp", "pool", "act"):
            self.dq[q] = {"n": 0, "sems": [es.enter_context(nc.semaphore("d_%s%d" % (q, i))) for i in range(KSLOT)]}

    def _wait(self, E, tok):
        if tok[0] == "c":
            _, F, idx = tok
            if F == E and (E == "pe" or not SAME_ENGINE_SYNC):
                return
            key, val, sem = F, idx, self.sem[F]
        else:
            _, q, slot, c = tok
            key, val, sem = (q, slot), 16 * c, self.dq[q]["sems"][slot]
        if self.seen[E].get(key, 0) >= val:
            return
        self.seen[E][key] = val
        self.ops[E].append(lambda e, sem=sem, val=val: e.wait_ge(sem, val))

    @staticmethod
    def _key(tok):
        return tok[1] if tok[0] == "c" else (tok[1], tok[2])

    def _deps(self, reads, writes):
        toks = []
        for b in reads:
            if b.w is not None:
                toks.append(b.w)
        for b in writes:
            if b.w is not None:
                toks.append(b.w)
            toks.extend(b.r.values())
        return toks

    def _mark(self, tok, reads, writes):
        k = self._key(tok)
        for b in reads:
            b.r[k] = tok
        for b in writes:
            b.w = tok
            b.r = {}

    def op(self, E, fn, reads=(), writes=()):
        reads = _bufs(reads)
        writes = _bufs(writes)
        for t in self._deps(reads, writes):
            self._wait(E, t)
        self.cnt[E] += 1
        n = self.cnt[E]
        sem = self.sem[E]
        self.ops[E].append(lambda e, fn=fn, sem=sem: fn(e).then_inc(sem, 1))
        self._mark(("c", E, n), reads, writes)

    def dma(self, q, out, in_, reads=(), writes=()):
        reads = _bufs(reads)
        writes = _bufs(writes)
        dq = self.dq[q]
        n = dq["n"]
        dq["n"] += 1
        slot, c = n % KSLOT, n // KSLOT + 1
        toks = self._deps(reads, writes)
        if c > 1:
            toks.append(("d", q, slot, c - 1))
        for t in toks:
            self._wait(q, t)
        sem = dq["sems"][slot]
        self.ops[q].append(lambda e, out=out, in_=in_, sem=sem: e.dma_start(out=out, in_=in_).then_inc(sem, 16))
        self._mark(("d", q, slot, c), reads, writes)

    def barrier(self):
        toks = [("c", e, self.cnt[e]) for e in self.names if self.cnt[e] > 0]
        for q, dq in self.dq.items():
            n = dq["n"]
            for slot in range(KSLOT):
                c = (n - slot + KSLOT - 1) // KSLOT
                if c > 0:
                    toks.append(("d", q, slot, c))
        save = SAME_ENGINE_SYNC
        for E in self.names:
            for t in toks:
                if t[0] == "c" and t[1] == E:
                    continue
                self._wait(E, t)

    def emit(self):
        nc = self.nc
        with nc.Block() as block:
            @block.tensor
            def _(e):
                for f in self.ops["pe"]:
                    f(e)

            @block.scalar
            def _(e):
                for f in self.ops["act"]:
                    f(e)

            @block.vector
            def _(e):
                for f in self.ops["dve"]:
                    f(e)

            @block.gpsimd
            def _(e):
                for f in self.ops["pool"]:
                    f(e)

            @block.sync
            def _(e):
                for f in self.ops["sp"]:
                    f(e)


class Arena:
    def __init__(self, nc, nbytes):
        self.t = nc.alloc_sbuf_tensor("arena", [128, nbytes], U8)
        self.size = nbytes
        self.off = 0
        self.top = nbytes

    def reset(self):
        self.off = 0

    def alloc(self, shape, dtype, name="", persist=False):
        esz = 2 if dtype == BF16 else 4
        n = 1
        for s in shape[1:]:
            n *= s
        nb = (n * esz + 63) // 64 * 64
        if persist:
            self.top -= nb
            o = self.top
        else:
            o = self.off
            self.off += nb
        assert self.off <= self.top, "SBUF arena overflow %s %d %d" % (name, self.off, self.top)
        ap = self.t[:, o:o + n * esz].bitcast(dtype)
        if len(shape) == 3:
            ap = ap.rearrange("p (a b) -> p a b", a=shape[1])
        elif len(shape) == 4:
            ap = ap.rearrange("p (a b c) -> p a b c", a=shape[1], b=shape[2])
        if shape[0] < 128:
            ap = ap[0:shape[0]]
        return T(ap, name=name)


def _win_layout():
    cols = []
    tiles = []

    def add(name, src, rope=None, q_only=False, partner=None):
        tiles.append(dict(name=name, off=len(cols), rope=rope, q_only=q_only))
        cols.extend(src)
        if rope is not None:
            tiles.append(dict(name=name + "p", off=len(cols), rope="partner", q_only=q_only))
            cols.extend(partner)

    def swap64x2(base):
        return [base + blk * 64 + (i + 32) % 64 for blk in range(2) for i in range(64)]

    def swap128(base):
        return [base + (i + 64) % 128 for i in range(128)]

    for h in range(4):
        add("QD%d" % h, list(range(h * 128, h * 128 + 128)), "D", True, swap64x2(h * 128))
    for h in range(4):
        add("KD%d" % h, list(range(512 + h * 128, 512 + h * 128 + 128)), "D", False, swap64x2(512 + h * 128))
    for h in range(4):
        add("QS%d" % h, list(range(3072 + h * 128, 3072 + h * 128 + 128)), "H", True, swap128(3072 + h * 128))
    for g in range(2):
        add("KS%d" % g, list(range(3584 + g * 128, 3584 + g * 128 + 128)), "H", False, swap128(3584 + g * 128))
    kr = list(range(5120, 5184))
    krp = [5120 + (i + 32) % 64 for i in range(64)]
    add("KR", kr + kr, "D", False, krp + krp)
    assert len(tiles) % 2 == 0
    for h in range(4):
        add("QN%d" % h, list(range(1536 + h * 128, 1536 + h * 128 + 128)), None, True)
    for h in range(4):
        add("CQ%d" % h, list(range(4096 + h * 128, 4096 + h * 128 + 128)), None, True)
    for h in range(4):
        add("KN%d" % h, list(range(2048 + h * 128, 2048 + h * 128 + 128)), None, False)
    for h in range(4):
        add("CKV%d" % h, list(range(4608 + h * 128, 4608 + h * 128 + 128)), None, False)
    tm = []
    for name, lo, n in (("VD", 1024, 512), ("VN", 2560, 512), ("VS", 3840, 256)):
        tm.append(dict(name=name, off=len(cols), n=n))
        cols.extend(range(lo, lo + n))
    return np.array(cols, dtype=np.int64), tiles, tm


WIN_COLS, FM_TILES, TM_BLOCKS = _win_layout()
NWEXT = len(WIN_COLS)


def _uq_cols():
    nope = [192 * h + i for h in range(4) for i in range(128)]
    rope = [192 * h + 128 + i for h in range(4) for i in range(64)]
    ropep = [192 * h + 128 + (i + 32) % 64 for h in range(4) for i in range(64)]
    return np.array(nope + rope[0:128] + ropep[0:128] + rope[128:256] + ropep[128:256], dtype=np.int64)


def _ukv_cols():
    kn = [256 * h + i for h in range(4) for i in range(128)]
    v = [256 * h + 128 + i for h in range(4) for i in range(128)]
    return np.array(kn + v, dtype=np.int64)


UQ_COLS = _uq_cols()
UKV_COLS = _ukv_cols()


class Prog:
    def __init__(self, stop_after=None, dump=()):
        self.stop_after = stop_after
        self.dump = set(dump)
        self.es = ExitStack()
        nc = bass.Bass("TRN2", target_bir_lowering=False)
        self.nc = nc
        self.sc = Sched(nc, self.es)
        self.ar = Arena(nc, 206 * 1024)
        self.psbig = [nc.alloc_psum_tensor("psb%d" % i, [128, 1024], F32)[:] for i in range(4)]
        self.ps = [T(self.psbig[i // 2][:, (i % 2) * 512:(i % 2 + 1) * 512], name="ps%d" % i) for i in range(8)]
        self.inputs = {}
        self.scr = {}
        self.done = False

    def inp(self, name, shape, dtype=F32):
        return LazyIn(self, name, shape, dtype)

    def scratch(self, name, shape, dtype):
        kind = "ExternalOutput" if name in self.dump else "Internal"
        t = self.nc.dram_tensor(name, list(shape), dtype, kind=kind)
        r = T(t.ap(), name=name)
        self.scr[name] = r
        return r

    def phase_end(self, name):
        self.sc.barrier()
        self.ar.reset()
        if self.stop_after == name:
            self.done = True
        return self.done


def build_program(stop_after=None, dump=()):
    P = Prog(stop_after, dump)
    nc, sc, ar, ps = P.nc, P.sc, P.ar, P.ps
    op, dma = sc.op, sc.dma

    x_in = P.inp("x", [S, D])
    pT_in = P.inp("pT", [2, 256, S])
    wext = P.inp("wext", [2, D, NWEXT])
    wout = P.inp("wout", [2, D, D])
    wuq = P.inp("wuq", [2, 512, 1024])
    wukv = P.inp("wukv", [2, 512, 1024])
    dwg = P.inp("dwg", [D, FFN])
    dwu = P.inp("dwu", [D, FFN])
    dwd = P.inp("dwd", [FFN, D])
    mwg = P.inp("mwg", [8, D, FFN])
    mwu = P.inp("mwu", [8, D, FFN])
    mwd = P.inp("mwd", [8, FFN, D])
    routerT = P.inp("routerT", [8, D])
    plegate = P.inp("plegate", [2, D, D])
    pleproj = P.inp("pleproj", [2, 256, D])
    gains = P.inp("gains", [6, 128, 16])
    finalg = P.inp("finalg", [1, D])
    ffng1 = P.inp("ffng1", [1, D])
    mlan = P.inp("mlan", [2, 128, 8])
    subln = P.inp("subln", [2, 128, 1])
    lam_in = P.inp("lamv", [2, 4, 64])
    sinks = P.inp("sinks", [2, 4])
    natab = P.inp("natab", [2, 8, 4, 8, 128, 512], BF16)
    swatab = P.inp("swatab", [8, 6, 128, 512], BF16)
    ropeD = P.inp("ropeD", [2, 128, S])
    ropeH = P.inp("ropeH", [2, 128, S])
    ident_in = P.inp("ident", [128, 128], BF16)
    out_t = P.nc.dram_tensor("out", [2048, D], F32, kind="ExternalOutput")
    out_ap = T(out_t.ap(), name="out")

    Hres = P.scratch("Hres", [S, D], F32)
    HNT = P.scratch("HNT", [16, 128, S], BF16)
    fm_names = [t["name"] for t in FM_TILES if t["rope"] != "partner"]
    FMS = {n: P.scratch("z_" + n, [128, S], BF16) for n in fm_names}
    TMS = {b["name"]: P.scratch("z_" + b["name"], [S, b["n"]], BF16) for b in TM_BLOCKS}
    QMN = [P.scratch("QMN%d" % h, [128, S], BF16) for h in range(4)]
    QMR = [P.scratch("QMR%d" % h, [128, S], BF16) for h in range(2)]
    KMN = [P.scratch("KMN%d" % h, [128, S], BF16) for h in range(4)]
    VM = P.scratch("VM", [S, 512], BF16)
    MIXT = P.scratch("MIXT", [16, 128, S], BF16)
    ACTT = P.scratch("ACTT", [16, 128, NFT, 128], BF16)
    GATES = P.scratch("GATES", [2048, 8], F32)

    H_b = [Buf("H%d" % i) for i in range(32)]
    HNT_b = [Buf("HNT%d" % i) for i in range(32)]

    ident = ar.alloc([128, 128], BF16, "ident", persist=True)
    ones_f = ar.alloc([128, 128], F32, "ones_f", persist=True)
    ones_b = ar.alloc([128, 128], BF16, "ones_b", persist=True)
    gains_sb = ar.alloc([128, 6, 16], F32, "gains", persist=True)
    dma("sp", ident.ap, ident_in.ap, [ident_in], [ident])
    dma("sp", gains_sb.ap, gains.ap.rearrange("g p c -> p g c"), [gains], [gains_sb])
    op("dve", lambda e: e.memset(ones_f.ap, 1.0), [], [ones_f])
    op("dve", lambda e: e.memset(ones_b.ap, 1.0), [], [ones_b])
    eps_t = ar.alloc([128, 1], F32, "eps", persist=True)
    op("dve", lambda e: e.memset(eps_t.ap, EPS), [], [eps_t])

    def norm_transpose(h_sb, gidx, tt, pbank, extra_hn_f32=None):
        raise NotImplementedError

    class NormCtx:
        def __init__(self):
            self.junk = ar.alloc([128, D], BF16, "nt_junk")
            self.ss = [ar.alloc([128, 1], F32, "nt_ss%d" % i) for i in range(2)]
            self.rstd = [ar.alloc([128, 1], F32, "nt_rstd%d" % i) for i in range(2)]
            self.hs = [ar.alloc([128, D], BF16, "nt_hs%d" % i) for i in range(2)]
            self.hT = [ar.alloc([128, 16, 128], BF16, "nt_hT%d" % i) for i in range(2)]
            self.i = 0

        def stats(self, h_sb):
            k = self.i % 2
            self.i += 1
            ss, rstd, hs = self.ss[k], self.rstd[k], self.hs[k]
            op("act", lambda e: e.activation(out=self.junk.ap, in_=h_sb.ap, func=AF.Square, accum_out=ss.ap),
               [h_sb], [self.junk, ss])
            op("act", lambda e: e.activation(out=rstd.ap, in_=ss.ap, func=AF.Sqrt, scale=1.0 / D, bias=eps_t.ap), [ss, eps_t], [rstd])
            op("dve", lambda e: e.reciprocal(rstd.ap, rstd.ap), [rstd], [rstd])
            op("act", lambda e: e.activation(out=hs.ap, in_=h_sb.ap, func=AF.Copy, scale=rstd.ap), [h_sb, rstd], [hs])
            return dict(k=k, rstd=rstd, hs=hs)

        def trans(self, ctx, gidx, tt, pbanks, to_hnt=True):
            hs, hT = ctx["hs"], self.hT[ctx["k"]]
            for half in range(2):
                pb = pbanks[half]
                pv = pb.ap.bitcast(BF16).rearrange("p (a b) -> p a b", a=8)
                for c in range(8):
                    cc = half * 8 + c
                    op("pe", lambda e, c=c, cc=cc, pv=pv: e.transpose(pv[:, c, :], hs.ap[:, cc * 128:(cc + 1) * 128], ident.ap),
                       [hs, ident], [pb])
                g = gains_sb.ap[:, gidx, half * 8:(half + 1) * 8]
                gb = g.unsqueeze(2).to_broadcast([128, 8, 128])
                op("dve", lambda e, pv=pv, gb=gb, half=half: e.tensor_tensor(hT.ap[:, half * 8:(half + 1) * 8, :], pv, gb, ALU.mult),
                   [pb, gains_sb], [hT])
            if to_hnt:
                dma("sp", HNT.ap[:, :, tt * 128:(tt + 1) * 128].rearrange("c p t -> p c t"), hT.ap, [hT], [HNT_b[tt]])
            return hT

        def run(self, h_sb, gidx, tt, pbanks, hn_f32=None, to_hnt=True):
            ctx = self.stats(h_sb)
            hT = self.trans(ctx, gidx, tt, pbanks, to_hnt)
            return ctx["rstd"] if to_hnt else hT

    def phase_prologue():
        nctx = NormCtx()
        hb = [ar.alloc([128, D], F32, "pro_h%d" % i) for i in range(3)]
        prev = None
        for tt in range(32):
            h = hb[tt % 3]
            dma("sp", h.ap, x_in.ap[tt * 128:(tt + 1) * 128, :], [x_in], [h])
            dma("sp", Hres.ap[tt * 128:(tt + 1) * 128, :], h.ap, [h], [H_b[tt]])
            ctx = nctx.stats(h)
            if prev is not None:
                nctx.trans(prev[0], 0, prev[1], (ps[(2 * prev[1]) % 8], ps[(2 * prev[1] + 1) % 8]))
            prev = (ctx, tt)
        nctx.trans(prev[0], 0, prev[1], (ps[(2 * prev[1]) % 8], ps[(2 * prev[1] + 1) % 8]))

    def phase_inproj(L):
        nq_chunks = 8 if L == 0 else 4
        hn = [ar.alloc([128, 16, 1024], BF16, "ip_hn%d" % i) for i in range(2)]
        wu = [ar.alloc([128, 16, 256], BF16, "ip_w%d" % i) for i in range(3)]
        wb = [ar.alloc([128, 16, 512], BF16, "ip_wb%d" % i) for i in range(2)]
        rt = [ar.alloc([128, 4, 512], F32, "ip_rt%d" % i) for i in range(2)]
        t1 = [ar.alloc([128, 512], F32, "ip_t1%d" % i) for i in range(2)]
        t2 = [ar.alloc([128, 512], F32, "ip_t2%d" % i) for i in range(2)]
        ob = [ar.alloc([128, 512], BF16, "ip_ob%d" % i) for i in range(4)]
        units = [(FM_TILES[i], FM_TILES[i + 1]) for i in range(0, len(FM_TILES), 2)]
        wi = 0
        oi = 0
        pi = 0
        def load_hn(sc_j):
            dma("sp", hn[sc_j % 2].ap, HNT.ap[:, :, sc_j * 1024:(sc_j + 1) * 1024].rearrange("c p t -> p c t"),
                [HNT_b[sc_j * 8 + j] for j in range(8)], [hn[sc_j % 2]])

        load_hn(0)
        for sc_i in range(4):
            hnb = hn[sc_i % 2]
            t0 = sc_i * 1024
            for ch in range(2):
                tok0 = t0 + ch * 512
                dma("sp", rt[ch].ap[:, 0:2, :], ropeD.ap[:, :, tok0:tok0 + 512].rearrange("a p t -> p a t"), [ropeD], [rt[ch]])
                dma("sp", rt[ch].ap[:, 2:4, :], ropeH.ap[:, :, tok0:tok0 + 512].rearrange("a p t -> p a t"), [ropeH], [rt[ch]])
            if sc_i + 1 < 4:
                load_hn(sc_i + 1)
            for (ta, tb) in units:
                chunks = [ch for ch in range(2) if not (ta["q_only"] and (sc_i * 2 + ch) >= nq_chunks)]
                if not chunks:
                    continue
                w = wu[wi % 3]
                wi += 1
                dma("pool", w.ap, wext.ap[L, :, ta["off"]:ta["off"] + 256].rearrange("(c p) n -> p c n", p=128), [wext], [w])
                for ch in chunks:
                    tok0 = t0 + ch * 512
                    pa, pb = ps[pi % 8], ps[(pi + 1) % 8]
                    pi += 2
                    for c in range(16):
                        op("pe", lambda e, c=c, pa=pa, w=w, hnb=hnb, ch=ch: e.matmul(
                            pa.ap, w.ap[:, c, 0:128], hnb.ap[:, c, ch * 512:(ch + 1) * 512], start=(c == 0), stop=(c == 15)),
                           [w, hnb], [pa])
                    for c in range(16):
                        op("pe", lambda e, c=c, pb=pb, w=w, hnb=hnb, ch=ch: e.matmul(
                            pb.ap, w.ap[:, c, 128:256], hnb.ap[:, c, ch * 512:(ch + 1) * 512], start=(c == 0), stop=(c == 15)),
                           [w, hnb], [pb])
                    if ta["rope"] is not None:
                        r = rt[ch]
                        ro = 0 if ta["rope"] == "D" else 2
                        a1, a2 = t1[oi % 2], t2[oi % 2]
                        o = ob[oi % 4]
                        oi += 1
                        op("dve", lambda e, a1=a1, pa=pa, r=r, ro=ro: e.tensor_tensor(a1.ap, pa.ap, r.ap[:, ro, :], ALU.mult), [pa, r], [a1])
                        op("dve", lambda e, a2=a2, pb=pb, r=r, ro=ro: e.tensor_tensor(a2.ap, pb.ap, r.ap[:, ro + 1, :], ALU.mult), [pb, r], [a2])
                        op("dve", lambda e, a1=a1, a2=a2, o=o: e.tensor_tensor(o.ap, a1.ap, a2.ap, ALU.add), [a1, a2], [o])
                        dst = FMS[ta["name"]]
                        dma("sp", dst.ap[:, tok0:tok0 + 512], o.ap, [o], [dst])
                    else:
                        for (tl, pp) in ((ta, pa), (tb, pb)):
                            o = ob[oi % 4]
                            oi += 1
                            op("act", lambda e, o=o, pp=pp: e.activation(out=o.ap, in_=pp.ap, func=AF.Copy), [pp], [o])
                            dst = FMS[tl["name"]]
                            dma("sp", dst.ap[:, tok0:tok0 + 512], o.ap, [o], [dst])
            for bi, blk in enumerate(TM_BLOCKS):
                w = wb[bi % 2]
                n = blk["n"]
                dma("pool", w.ap[:, :, 0:n], wext.ap[L, :, blk["off"]:blk["off"] + n].rearrange("(c p) n -> p c n", p=128), [wext], [w])
                for tt in range(8):
                    pa = ps[pi % 8]
                    pi += 1
                    for c in range(16):
                        op("pe", lambda e, c=c, pa=pa, w=w, hnb=hnb, tt=tt, n=n: e.matmul(
                            pa.ap[:, 0:n], hnb.ap[:, c, tt * 128:(tt + 1) * 128], w.ap[:, c, 0:n], start=(c == 0), stop=(c == 15)),
                           [w, hnb], [pa])
                    o = ob[oi % 4]
                    oi += 1
                    op("act", lambda e, o=o, pa=pa, n=n: e.activation(out=o.ap[:, 0:n], in_=pa.ap[:, 0:n], func=AF.Copy), [pa], [o])
                    dst = TMS[blk["name"]]
                    dma("sp", dst.ap[t0 + tt * 128:t0 + (tt + 1) * 128, :], o.ap[:, 0:n], [o], [dst])

    def phase_mla_prep(L):
        nq_chunks = 8 if L == 0 else 4
        wq = ar.alloc([128, 4, 1024], BF16, "mp_wq")
        wk = ar.alloc([128, 4, 1024], BF16, "mp_wk")
        nrm = ar.alloc([128, 8], F32, "mp_nrm")
        dma("pool", wq.ap, wuq.ap[L].rearrange("(c p) n -> p c n", p=128), [wuq], [wq])
        dma("pool", wk.ap, wukv.ap[L].rearrange("(c p) n -> p c n", p=128), [wukv], [wk])
        dma("sp", nrm.ap, mlan.ap[L], [mlan], [nrm])
        cin = [ar.alloc([128, 4, 512], BF16, "mp_cin%d" % i) for i in range(2)]
        sq = [ar.alloc([128, 4, 512], F32, "mp_sq%d" % i) for i in range(2)]
        rstd = [ar.alloc([128, 512], F32, "mp_rstd%d" % i) for i in range(2)]
        cn = [ar.alloc([128, 4, 512], BF16, "mp_cn%d" % i) for i in range(2)]
        rt = [ar.alloc([128, 2, 512], F32, "mp_rt%d" % i) for i in range(2)]
        t1 = [ar.alloc([128, 512], F32, "mp_t1%d" % i) for i in range(2)]
        t2 = [ar.alloc([128, 512], F32, "mp_t2%d" % i) for i in range(2)]
        ob = [ar.alloc([128, 512], BF16, "mp_ob%d" % i) for i in range(4)]
        st = dict(i=0, oi=0, pi=0)

        def normed(src_names, noff, tok0):
            k = st["i"] % 2
            st["i"] += 1
            ci, s2, rs, cno = cin[k], sq[k], rstd[k], cn[k]
            for t in range(4):
                src = FMS[src_names[t]]
                dma("sp", ci.ap[:, t, :], src.ap[:, tok0:tok0 + 512], [src], [ci])
            op("act", lambda e: e.activation(out=s2.ap, in_=ci.ap, func=AF.Square), [ci], [s2])
            pm = ps[st["pi"] % 8]
            st["pi"] += 1
            for t in range(4):
                op("pe", lambda e, t=t: e.matmul(pm.ap, ones_f.ap, s2.ap[:, t, :], start=(t == 0), stop=(t == 3)), [ones_f, s2], [pm])
            op("act", lambda e: e.activation(out=rs.ap, in_=pm.ap, func=AF.Sqrt, scale=1.0 / 512, bias=eps_t.ap), [pm, eps_t], [rs])
            op("dve", lambda e: e.reciprocal(rs.ap, rs.ap), [rs], [rs])
            for t in range(4):
                op("dve", lambda e, t=t: e.scalar_tensor_tensor(cno.ap[:, t, :], ci.ap[:, t, :], nrm.ap[:, noff + t:noff + t + 1],
                                                                  rs.ap, ALU.mult, ALU.mult), [ci, nrm, rs], [cno])
            return cno

        def evac_plain(pp, dst, tok0, n=512):
            o = ob[st["oi"] % 4]
            st["oi"] += 1
            op("act", lambda e: e.activation(out=o.ap[:, 0:n], in_=pp.ap[:, 0:n], func=AF.Copy), [pp], [o])
            return o

        for ch in range(8):
            tok0 = ch * 512
            cno = normed(["CKV%d" % t for t in range(4)], 4, tok0)
            for h in range(4):
                pp = ps[st["pi"] % 8]
                st["pi"] += 1
                for c in range(4):
                    op("pe", lambda e, c=c, h=h, pp=pp, cno=cno: e.matmul(pp.ap, wk.ap[:, c, h * 128:(h + 1) * 128], cno.ap[:, c, :],
                                                                start=(c == 0), stop=(c == 3)), [wk, cno], [pp])
                o = evac_plain(pp, None, tok0)
                dma("sp", KMN[h].ap[:, tok0:tok0 + 512], o.ap, [o], [KMN[h]])
            for tt in range(4):
                pp = ps[st["pi"] % 8]
                st["pi"] += 1
                for c in range(4):
                    op("pe", lambda e, c=c, tt=tt, pp=pp, cno=cno: e.matmul(pp.ap, cno.ap[:, c, tt * 128:(tt + 1) * 128], wk.ap[:, c, 512:1024],
                                                                 start=(c == 0), stop=(c == 3)), [wk, cno], [pp])
                o = evac_plain(pp, None, tok0)
                dma("sp", VM.ap[tok0 + tt * 128:tok0 + (tt + 1) * 128, :], o.ap, [o], [VM])
            if ch >= nq_chunks:
                continue
            cno = normed(["CQ%d" % t for t in range(4)], 0, tok0)
            for h in range(4):
                pp = ps[st["pi"] % 8]
                st["pi"] += 1
                for c in range(4):
                    op("pe", lambda e, c=c, h=h, pp=pp, cno=cno: e.matmul(pp.ap, wq.ap[:, c, h * 128:(h + 1) * 128], cno.ap[:, c, :],
                                                                start=(c == 0), stop=(c == 3)), [wq, cno], [pp])
                o = evac_plain(pp, None, tok0)
                dma("sp", QMN[h].ap[:, tok0:tok0 + 512], o.ap, [o], [QMN[h]])
            r = rt[ch % 2]
            dma("sp", r.ap, ropeD.ap[:, :, tok0:tok0 + 512].rearrange("a p t -> p a t"), [ropeD], [r])
            for pr in range(2):
                pa, pb = ps[st["pi"] % 8], ps[(st["pi"] + 1) % 8]
                st["pi"] += 2
                base = 512 + pr * 256
                for c in range(4):
                    op("pe", lambda e, c=c, pa=pa, base=base, cno=cno: e.matmul(pa.ap, wq.ap[:, c, base:base + 128], cno.ap[:, c, :],
                                                                       start=(c == 0), stop=(c == 3)), [wq, cno], [pa])
                for c in range(4):
                    op("pe", lambda e, c=c, pb=pb, base=base, cno=cno: e.matmul(pb.ap, wq.ap[:, c, base + 128:base + 256], cno.ap[:, c, :],
                                                                       start=(c == 0), stop=(c == 3)), [wq, cno], [pb])
                a1, a2 = t1[pr], t2[pr]
                o = ob[st["oi"] % 4]
                st["oi"] += 1
                op("dve", lambda e, a1=a1, pa=pa, r=r: e.tensor_tensor(a1.ap, pa.ap, r.ap[:, 0, :], ALU.mult), [pa, r], [a1])
                op("dve", lambda e, a2=a2, pb=pb, r=r: e.tensor_tensor(a2.ap, pb.ap, r.ap[:, 1, :], ALU.mult), [pb, r], [a2])
                op("dve", lambda e, a1=a1, a2=a2, o=o: e.tensor_tensor(o.ap, a1.ap, a2.ap, ALU.add), [a1, a2], [o])
                dma("sp", QMR[pr].ap[:, tok0:tok0 + 512], o.ap, [o], [QMR[pr]])

    def attention(L, kind):
        LA = 1
        nq_chunks = 8 if L == 0 else 4
        lam_init = 0.8 - 0.6 * math.exp(-0.3 * L)
        kTb = [ar.alloc([128, S], BF16, "at_kT%d" % i) for i in range(2)]
        kT2 = ar.alloc([128, S], BF16, "at_kT2") if kind == "mla" else None
        Vb = [ar.alloc([128, 32, 128], BF16, "at_V%d" % i) for i in range(2)]
        qT = [ar.alloc([128, 512], BF16, "at_qT%d" % i) for i in range(2)]
        qT2 = [ar.alloc([128, 512], BF16, "at_qT2%d" % i) for i in range(2)] if kind == "mla" else None
        pT = [ar.alloc([128, 1024], BF16, "at_pT%d" % i) for i in range(3)]
        sb = [ar.alloc([128, 1024], F32, "at_sb%d" % i) for i in range(2)] if kind in ("na", "swa") else None
        accb = [ar.alloc([128, 1024], F32, "at_acc%d" % i) for i in range(2)]
        tab = [ar.alloc([128, 8, 512], BF16, "at_tab%d" % i) for i in range(2)] if kind in ("na", "swa") else None
        rl = ar.alloc([128, 512], F32, "at_rl")
        o1 = ar.alloc([128, 512], F32, "at_o1")
        o2 = ar.alloc([128, 512], F32, "at_o2")
        osq = ar.alloc([128, 512], F32, "at_osq")
        ob = [ar.alloc([128, 512], BF16, "at_ob%d" % i) for i in range(2)]
        small = ar.alloc([128, 16], F32, "at_small")
        lamt = ar.alloc([128, 4, 64], F32, "at_lamt")
        junk = ar.alloc([128, 64], F32, "at_junk")
        S_banks = ps[0:4]
        O_banks = ps[4:6]
        L_banks = ps[6:8]
        mix0 = dict(diff=0, na=4, swa=8, mla=12)[kind]
        scale = dict(diff=64 ** -0.5, na=128 ** -0.5, swa=128 ** -0.5, mla=192 ** -0.5)[kind]
        if kind == "diff":
            dma("sp", lamt.ap, lam_in.ap[L].unsqueeze(0).to_broadcast([128, 4, 64]), [lam_in], [lamt])
            op("dve", lambda e: e.tensor_tensor(junk.ap, lamt.ap[:, 0, :], lamt.ap[:, 1, :], ALU.mult), [lamt], [junk])
            op("dve", lambda e: e.reduce_sum(small.ap[:, 0:1], junk.ap, AX.X), [junk], [small])
            op("dve", lambda e: e.tensor_tensor(junk.ap, lamt.ap[:, 2, :], lamt.ap[:, 3, :], ALU.mult), [lamt, small], [junk])
            op("dve", lambda e: e.reduce_sum(small.ap[:, 1:2], junk.ap, AX.X), [junk], [small])
            op("act", lambda e: e.activation(out=small.ap[:, 2:4], in_=small.ap[:, 0:2], func=AF.Exp), [small], [small])
            op("dve", lambda e: e.tensor_tensor(small.ap[:, 4:5], small.ap[:, 3:4], small.ap[:, 2:3], ALU.subtract), [small], [small])
            op("dve", lambda e: e.tensor_scalar(small.ap[:, 4:5], small.ap[:, 4:5], -lam_init, None, ALU.add), [small], [small])
            dma("sp", small.ap[:, 5:6], subln.ap[L], [subln], [small])
            op("dve", lambda e: e.tensor_scalar(small.ap[:, 5:6], small.ap[:, 5:6], 1.0 - lam_init, None, ALU.mult), [small], [small])
        if kind == "swa":
            dma("sp", small.ap[:, 0:4], sinks.ap[L:L + 1, :].to_broadcast([128, 4]), [sinks], [small])
            op("act", lambda e: e.activation(out=small.ap[:, 4:8], in_=small.ap[:, 0:4], func=AF.Exp), [small], [small])
        st = dict(si=0, pi=0, oi=0, sbi=0)

        def kvbuf(h):
            return (h // 2) % 2 if kind == "swa" else h % 2

        def load_head(h):
            if kind == "swa" and h % 2 == 1:
                return
            if kind == "diff":
                ksrc, vsrc, vcol = FMS["KD%d" % h], TMS["VD"], h * 128
            elif kind == "na":
                ksrc, vsrc, vcol = FMS["KN%d" % h], TMS["VN"], h * 128
            elif kind == "swa":
                ksrc, vsrc, vcol = FMS["KS%d" % (h // 2)], TMS["VS"], (h // 2) * 128
            else:
                ksrc, vsrc, vcol = KMN[h], VM, h * 128
            kT, V = kTb[kvbuf(h)], Vb[kvbuf(h)]
            dma("sp", kT.ap, ksrc.ap, [ksrc], [kT])
            dma("sp", V.ap, vsrc.ap[:, vcol:vcol + 128].rearrange("(t p) d -> p t d", p=128), [vsrc], [V])

        groups = [(h, qc) for h in range(4) for qc in range(nq_chunks)]

        def load_group(gi):
            h, qc = groups[gi]
            qsrc = dict(diff=lambda: FMS["QD%d" % h], na=lambda: FMS["QN%d" % h], swa=lambda: FMS["QS%d" % h], mla=lambda: QMN[h])[kind]()
            q = qT[gi % 2]
            dma("sp", q.ap, qsrc.ap[:, qc * 512:(qc + 1) * 512], [qsrc], [q])
            if kind == "mla":
                q2 = qT2[gi % 2]
                dma("sp", q2.ap, QMR[h // 2].ap[:, qc * 512:(qc + 1) * 512], [QMR[h // 2]], [q2])
            if kind == "na":
                tb = tab[gi % 2]
                dma("sp", tb.ap, natab.ap[L, qc, h].rearrange("j p q -> p j q"), [natab], [tb])
            elif kind == "swa":
                tb = tab[gi % 2]
                dma("sp", tb.ap[:, 0:6, :], swatab.ap[qc].rearrange("j p q -> p j q"), [swatab], [tb])

        if kind == "mla":
            dma("sp", kT2.ap, FMS["KR"].ap, [FMS["KR"]], [kT2])
        load_head(0)
        load_group(0)

        dense = kind in ("diff", "mla")

        def emit_S(kT, q, q2, tb, h, comp, pair):
            pp = st["si"] % 2
            st["si"] += 1
            banks = (ps[2 * pp], ps[2 * pp + 1])
            big = P.psbig[pp]
            for (kt, j), Sb in zip(pair, banks):
                ks = slice(kt * 128, (kt + 1) * 128)
                if kind == "diff":
                    pr = slice(comp * 64, comp * 64 + 64)
                    op("pe", lambda e, Sb=Sb, ks=ks, pr=pr: e.matmul(Sb.ap, kT.ap[pr, ks], q.ap[pr, :], start=True, stop=True), [kT, q], [Sb])
                elif kind == "mla":
                    pr = slice((h % 2) * 64, (h % 2) * 64 + 64)
                    op("pe", lambda e, Sb=Sb, ks=ks: e.matmul(Sb.ap, kT.ap[:, ks], q.ap, start=True, stop=False), [kT, q], [Sb])
                    op("pe", lambda e, Sb=Sb, ks=ks, pr=pr: e.matmul(Sb.ap, kT2.ap[pr, ks], q2.ap[pr, :], start=False, stop=True), [kT2, q2], [Sb])
                else:
                    op("pe", lambda e, Sb=Sb, ks=ks: e.matmul(Sb.ap, kT.ap[:, ks], q.ap, start=True, stop=True), [kT, q], [Sb])
            p = pT[st["pi"] % 3]
            st["pi"] += 1
            j0 = pair[0][1]
            if j0 is None:
                op("act", lambda e: e.activation(out=p.ap, in_=big, func=AF.Exp, scale=scale), list(banks), [p])
            else:
                s_ = sb[st["sbi"] % 2]
                st["sbi"] += 1
                tbv = tb.ap[:, j0:j0 + 2, :].rearrange("p a b -> p (a b)")
                op("dve", lambda e: e.scalar_tensor_tensor(s_.ap, big, scale, tbv, ALU.mult, ALU.add), list(banks) + [tb], [s_])
                op("act", lambda e: e.activation(out=p.ap, in_=s_.ap, func=AF.Exp), [s_], [p])
            return p

        def emit_OL(V, Ob, Lb, pair, p, pi_, npairs, acc):
            for t, (kt, j) in enumerate(pair):
                first = (pi_ == 0 and t == 0)
                last = (pi_ == npairs - 1 and t == 1)
                pv = p.ap[:, t * 512:(t + 1) * 512]
                op("pe", lambda e, kt=kt, pv=pv, first=first, last=last: e.matmul(Ob.ap, V.ap[:, kt, :], pv, start=first, stop=last), [V, p], [Ob])
                if not dense:
                    op("pe", lambda e, pv=pv, first=first, last=last: e.matmul(Lb.ap, ones_b.ap, pv, start=first, stop=last), [ones_b, p], [Lb])
            if dense:
                if pi_ == 0:
                    op("dve", lambda e: e.tensor_copy(acc.ap, p.ap), [p], [acc])
                else:
                    op("dve", lambda e: e.tensor_tensor(acc.ap, acc.ap, p.ap, ALU.add), [p, acc], [acc])

        def finalize(h, qc, comp, Ob, Lb, acc):
            if dense:
                op("dve", lambda e: e.tensor_tensor(acc.ap[:, 0:512], acc.ap[:, 0:512], acc.ap[:, 512:1024], ALU.add), [acc], [acc])
                op("pe", lambda e: e.matmul(Lb.ap, ones_f.ap, acc.ap[:, 0:512], start=True, stop=True), [ones_f, acc], [Lb])
            if kind == "swa":
                op("dve", lambda e: e.tensor_scalar(rl.ap, Lb.ap, small.ap[:, 4 + h:5 + h], None, ALU.add), [Lb, small], [rl])
                op("dve", lambda e: e.reciprocal(rl.ap, rl.ap), [rl], [rl])
            else:
                op("dve", lambda e: e.reciprocal(rl.ap, Lb.ap), [Lb], [rl])
            o = ob[st["oi"] % 2]
            dst = MIXT.ap[mix0 + h, :, qc * 512:(qc + 1) * 512]
            if kind != "diff":
                op("dve", lambda e: e.tensor_tensor(o.ap, Ob.ap, rl.ap, ALU.mult), [Ob, rl], [o])
                dma("sp", dst, o.ap, [o], [MIXT])
            elif comp == 0:
                op("dve", lambda e: e.tensor_tensor(o1.ap, Ob.ap, rl.ap, ALU.mult), [Ob, rl], [o1])
            else:
                op("dve", lambda e: e.tensor_tensor(o2.ap, Ob.ap, rl.ap, ALU.mult), [Ob, rl], [o2])
                op("dve", lambda e: e.scalar_tensor_tensor(o1.ap, o2.ap, small.ap[:, 4:5], o1.ap, ALU.mult, ALU.add), [o2, small, o1], [o1])
                op("act", lambda e: e.activation(out=osq.ap, in_=o1.ap, func=AF.Square), [o1], [osq])
                Mb = ps[2 * (st["si"] % 2)]
                op("pe", lambda e: e.matmul(Mb.ap, ones_f.ap, osq.ap, start=True, stop=True), [ones_f, osq], [Mb])
                op("act", lambda e: e.activation(out=rl.ap, in_=Mb.ap, func=AF.Sqrt, scale=1.0 / 128, bias=eps_t.ap), [Mb, eps_t], [rl])
                op("dve", lambda e: e.reciprocal(rl.ap, rl.ap), [rl], [rl])
                op("dve", lambda e: e.scalar_tensor_tensor(o.ap, o1.ap, small.ap[:, 5:6], rl.ap, ALU.mult, ALU.mult), [o1, small, rl], [o])
                dma("sp", dst, o.ap, [o], [MIXT])

        FINLAG = 2
        steps = []
        for gi, (h, qc) in enumerate(groups):
            if kind == "na":
                klist = [((4 * qc - 2 + j) % 32, j) for j in range(8)]
            elif kind == "swa":
                klist = [((4 * qc - 1 + j) % 32, j) for j in range(6)]
            else:
                klist = [(kt, None) for kt in range(32)]
            pairs = [(klist[2 * i], klist[2 * i + 1]) for i in range(len(klist) // 2)]
            for comp in range(2 if kind == "diff" else 1):
                sub = dict(gi=gi, h=h, qc=qc, comp=comp, npairs=len(pairs), first_of_group=(comp == 0))
                for pi_, pair in enumerate(pairs):
                    steps.append((sub, pi_, pair))
        pend = []
        fins = []
        subidx = dict(n=0)

        def do_OL(item):
            sub, pi_, pair, p = item
            if pi_ == 0:
                sub["Ob"] = O_banks[subidx["n"] % 2]
                sub["Lb"] = L_banks[subidx["n"] % 2]
                sub["acc"] = accb[subidx["n"] % 2]
                subidx["n"] += 1
            h = sub["h"]
            if pi_ == 0 and sub["first_of_group"] and sub["qc"] == 0 and h + 1 < 4:
                load_head(h + 1)
            emit_OL(Vb[kvbuf(h)], sub["Ob"], sub["Lb"], pair, p, pi_, sub["npairs"], sub["acc"])
            if pi_ == sub["npairs"] - 1:
                fins.append([sub, FINLAG])

        def tick_fins(force=False):
            for f in list(fins):
                f[1] -= 1
                if f[1] <= 0 or force:
                    sub = f[0]
                    finalize(sub["h"], sub["qc"], sub["comp"], sub["Ob"], sub["Lb"], sub["acc"])
                    fins.remove(f)

        for (sub, pi_, pair) in steps:
            gi, h, qc, comp = sub["gi"], sub["h"], sub["qc"], sub["comp"]
            if pi_ == 0 and sub["first_of_group"]:
                if gi + 1 < len(groups):
                    load_group(gi + 1)
            kT = kTb[kvbuf(h)]
            q = qT[gi % 2]
            q2 = qT2[gi % 2] if kind == "mla" else None
            tb = tab[gi % 2] if kind in ("na", "swa") else None
            p = emit_S(kT, q, q2, tb, h, comp, pair)
            pend.append((sub, pi_, pair, p))
            if len(pend) > LA:
                do_OL(pend.pop(0))
                tick_fins()
        while pend:
            do_OL(pend.pop(0))
            tick_fins()
        while fins:
            tick_fins(force=True)

    def gates_for(lt, tt):
        L8, A8, B8 = lt.ap[:, 0:8], lt.ap[:, 8:16], lt.ap[:, 16:24]
        m1, m2, nm1, den = lt.ap[:, 24:25], lt.ap[:, 25:26], lt.ap[:, 26:27], lt.ap[:, 27:28]
        op("dve", lambda e: e.reduce_max(m1, L8, AX.X), [lt], [lt])
        op("dve", lambda e: e.tensor_scalar(A8, L8, m1, None, ALU.is_equal), [lt], [lt])
        op("dve", lambda e: e.scalar_tensor_tensor(A8, A8, -1e30, L8, ALU.mult, ALU.add), [lt], [lt])
        op("dve", lambda e: e.reduce_max(m2, A8, AX.X), [lt], [lt])
        op("dve", lambda e: e.tensor_scalar(B8, L8, m2, None, ALU.is_ge), [lt], [lt])
        op("dve", lambda e: e.tensor_scalar(nm1, m1, -1.0, None, ALU.mult), [lt], [lt])
        op("act", lambda e: e.activation(out=A8, in_=L8, func=AF.Exp, bias=nm1, scale=1.0), [lt], [lt])
        op("dve", lambda e: e.tensor_tensor(B8, B8, A8, ALU.mult), [lt], [lt])
        op("dve", lambda e: e.reduce_sum(den, B8, AX.X), [lt], [lt])
        op("dve", lambda e: e.reciprocal(den, den), [lt], [lt])
        op("dve", lambda e: e.tensor_scalar(B8, B8, den, None, ALU.mult), [lt], [lt])
        dma("sp", GATES.ap[tt * 128:(tt + 1) * 128, :], B8, [lt], [GATES])

    def phase_outproj(L):
        ntt = 32 if L == 0 else 16
        w = ar.alloc([128, 16, D], BF16, "op_w")
        for c4 in range(4):
            dma("pool", w.ap[:, c4 * 4:(c4 + 1) * 4, :], wout.ap[L, c4 * 512:(c4 + 1) * 512, :].rearrange("(c p) n -> p c n", p=128), [wout], [w])
        mx = [ar.alloc([128, 16, 128], BF16, "op_mx%d" % i) for i in range(2)]
        hb = [ar.alloc([128, D], F32, "op_h%d" % i) for i in range(3)]
        nctx = NormCtx()
        moe = (L == 1)
        if moe:
            rTg = ar.alloc([128, 8, D], F32, "op_rT")
            dma("sp", rTg.ap, routerT.ap.unsqueeze(0).to_broadcast([128, 8, D]), [routerT], [rTg])
            grow = ar.alloc([128, D], F32, "op_grow")
            dma("sp", grow.ap, ffng1.ap.to_broadcast([128, D]), [ffng1], [grow])
            for e_ in range(8):
                op("dve", lambda e, e_=e_: e.tensor_tensor(rTg.ap[:, e_, :], rTg.ap[:, e_, :], grow.ap, ALU.mult), [rTg, grow], [rTg])
            junk = ar.alloc([128, D], F32, "op_junk")
            lg = [ar.alloc([128, 32], F32, "op_lg%d" % i) for i in range(2)]
        def op_loads(tt):
            m, h = mx[tt % 2], hb[tt % 3]
            dma("sp", m.ap, MIXT.ap[:, :, tt * 128:(tt + 1) * 128].rearrange("c p t -> p c t"), [MIXT], [m])
            dma("sp", h.ap, Hres.ap[tt * 128:(tt + 1) * 128, :], [H_b[tt]], [h])

        op_loads(0)
        prev = None
        for tt in range(ntt):
            m, h = mx[tt % 2], hb[tt % 3]
            if tt + 1 < ntt:
                op_loads(tt + 1)
            for db in range(4):
                pb = ps[(tt * 4 + db) % 4]
                for c in range(16):
                    op("pe", lambda e, c=c, pb=pb, m=m, db=db: e.matmul(pb.ap, m.ap[:, c, :], w.ap[:, c, db * 512:(db + 1) * 512],
                                                                        start=(c == 0), stop=(c == 15)), [m, w], [pb])
                op("dve", lambda e, pb=pb, h=h, db=db: e.tensor_tensor(h.ap[:, db * 512:(db + 1) * 512], h.ap[:, db * 512:(db + 1) * 512], pb.ap, ALU.add),
                   [pb, h], [h])
            dma("sp", Hres.ap[tt * 128:(tt + 1) * 128, :], h.ap, [h], [H_b[tt]])
            ctx = nctx.stats(h)
            rstd = ctx["rstd"]
            if prev is not None:
                nctx.trans(prev[0], 3 * L + 1, prev[1], (ps[4 + (prev[1] % 2) * 2], ps[5 + (prev[1] % 2) * 2]))
            prev = (ctx, tt)
            if moe:
                lt = lg[tt % 2]
                for e_ in range(8):
                    op("dve", lambda e, e_=e_, lt=lt, h=h, rstd=rstd: e.scalar_tensor_tensor(
                        junk.ap, h.ap, rstd.ap, rTg.ap[:, e_, :], ALU.mult, ALU.mult, accum_out=lt.ap[:, e_:e_ + 1]),
                       [h, rstd, rTg], [junk, lt])
                gates_for(lt, tt)
        nctx.trans(prev[0], 3 * L + 1, prev[1], (ps[4 + (prev[1] % 2) * 2], ps[5 + (prev[1] % 2) * 2]))

    def ffn_up(sci, wg_ap, wu_ap, wsrc):
        hn = ar.alloc([128, 16, 2048], BF16, "fu_hn")
        t0 = sci * 2048
        for q4 in range(4):
            dma("sp", hn.ap[:, :, q4 * 512:(q4 + 1) * 512], HNT.ap[:, :, t0 + q4 * 512:t0 + (q4 + 1) * 512].rearrange("c p t -> p c t"),
                [HNT_b[sci * 16 + q4 * 4 + j] for j in range(4)], [hn])
        wgb = [ar.alloc([128, 16, 256], BF16, "fu_wg%d" % i) for i in range(3)]
        wub = [ar.alloc([128, 16, 256], BF16, "fu_wu%d" % i) for i in range(3)]
        sg = [ar.alloc([128, 512], F32, "fu_sg%d" % i) for i in range(2)]
        ab = [ar.alloc([128, 512], BF16, "fu_ab%d" % i) for i in range(4)]
        k = 0
        for fb in range(NFT // 2):
            wg_, wu_ = wgb[fb % 3], wub[fb % 3]
            dma("pool", wg_.ap, wg_ap[:, fb * 256:(fb + 1) * 256].rearrange("(c p) n -> p c n", p=128), [wsrc], [wg_])
            dma("pool", wu_.ap, wu_ap[:, fb * 256:(fb + 1) * 256].rearrange("(c p) n -> p c n", p=128), [wsrc], [wu_])
            for f2 in range(2):
                ft = fb * 2 + f2
                for ch in range(4):
                    pg, pu = ps[(2 * k) % 8], ps[(2 * k + 1) % 8]
                    s_, a_ = sg[k % 2], ab[k % 4]
                    k += 1
                    for c in range(16):
                        op("pe", lambda e, c=c, pg=pg, wg_=wg_, f2=f2, ch=ch: e.matmul(
                            pg.ap, wg_.ap[:, c, f2 * 128:(f2 + 1) * 128], hn.ap[:, c, ch * 512:(ch + 1) * 512], start=(c == 0), stop=(c == 15)),
                           [wg_, hn], [pg])
                    for c in range(16):
                        op("pe", lambda e, c=c, pu=pu, wu_=wu_, f2=f2, ch=ch: e.matmul(
                            pu.ap, wu_.ap[:, c, f2 * 128:(f2 + 1) * 128], hn.ap[:, c, ch * 512:(ch + 1) * 512], start=(c == 0), stop=(c == 15)),
                           [wu_, hn], [pu])
                    op("act", lambda e, s_=s_, pg=pg: e.activation(out=s_.ap, in_=pg.ap, func=AF.Silu), [pg], [s_])
                    op("dve", lambda e, s_=s_, pu=pu, a_=a_: e.tensor_tensor(a_.ap, s_.ap, pu.ap, ALU.mult), [s_, pu], [a_])
                    dma("sp", ACTT.ap[ch * 4:(ch + 1) * 4, :, ft, :].rearrange("t p k -> p t k"),
                        a_.ap.rearrange("p (t k) -> p t k", t=4), [a_], [ACTT])

    def ffn_down(sci, wd_ap, wsrc, gate_e):
        wd = [ar.alloc([128, NFT, 512], BF16, "fd_w%d" % i) for i in range(2)]
        at = [ar.alloc([128, NFT, 128], BF16, "fd_a%d" % i) for i in range(2)]
        hb = [ar.alloc([128, 512], F32, "fd_h%d" % i) for i in range(3)]
        gt = None
        if gate_e is not None:
            gt = ar.alloc([128, 16, 8], F32, "fd_g")
            dma("sp", gt.ap, GATES.ap.rearrange("(t p) e -> p t e", p=128), [GATES], [gt])

        def load_w(db):
            w = wd[db % 2]
            for q4 in range(4):
                dma("pool", w.ap[:, q4 * 14:(q4 + 1) * 14, :],
                    wd_ap[q4 * 14 * 128:(q4 + 1) * 14 * 128, db * 512:(db + 1) * 512].rearrange("(c p) n -> p c n", p=128), [wsrc], [w])

        its = [(db, tt) for db in range(4) for tt in range(16)]

        def loads(k):
            db, tt = its[k]
            gtt = sci * 16 + tt
            dma("sp", at[k % 2].ap, ACTT.ap[tt], [ACTT], [at[k % 2]])
            dma("sp", hb[k % 3].ap, Hres.ap[gtt * 128:(gtt + 1) * 128, db * 512:(db + 1) * 512], [H_b[gtt]], [hb[k % 3]])

        def compute(k):
            db, tt = its[k]
            gtt = sci * 16 + tt
            a_, h, pb, w = at[k % 2], hb[k % 3], ps[k % 8], wd[db % 2]
            for ft in range(NFT):
                op("pe", lambda e, ft=ft: e.matmul(pb.ap, a_.ap[:, ft, :], w.ap[:, ft, :], start=(ft == 0), stop=(ft == NFT - 1)),
                   [a_, w], [pb])
            if gate_e is None:
                op("dve", lambda e: e.tensor_tensor(h.ap, h.ap, pb.ap, ALU.add), [h, pb], [h])
            else:
                op("dve", lambda e: e.scalar_tensor_tensor(h.ap, pb.ap, gt.ap[:, tt, gate_e:gate_e + 1], h.ap, ALU.mult, ALU.add),
                   [h, pb, gt], [h])
            dma("sp", Hres.ap[gtt * 128:(gtt + 1) * 128, db * 512:(db + 1) * 512], h.ap, [h], [H_b[gtt]])

        load_w(0)
        load_w(1)
        loads(0)
        for k, (db, tt) in enumerate(its):
            if k + 1 < len(its):
                loads(k + 1)
            if tt == 15 and db + 2 < 4:
                pass
            compute(k)
            if tt == 15 and db + 2 < 4:
                load_w(db + 2)

    def phase_ple(L):
        ntt = 32 if L == 0 else 16
        pg = ar.alloc([128, 16, D], BF16, "pl_pg")
        for c4 in range(4):
            dma("pool", pg.ap[:, c4 * 4:(c4 + 1) * 4, :], plegate.ap[L, c4 * 512:(c4 + 1) * 512, :].rearrange("(c p) n -> p c n", p=128), [plegate], [pg])
        pp = ar.alloc([128, 2, D], BF16, "pl_pp")
        dma("pool", pp.ap, pleproj.ap[L].rearrange("(c p) n -> p c n", p=128), [pleproj], [pp])
        nc1 = NormCtx()
        nc2 = NormCtx() if L == 0 else None
        hb = [ar.alloc([128, D], F32, "pl_h%d" % i) for i in range(3)]
        ptb = [ar.alloc([128, 2, 128], BF16, "pl_pt%d" % i) for i in range(3)]
        sg = [ar.alloc([128, 512], F32, "pl_sg%d" % i) for i in range(2)]
        if L == 1:
            gfin = ar.alloc([128, D], F32, "pl_gfin")
            dma("sp", gfin.ap, finalg.ap.to_broadcast([128, D]), [finalg], [gfin])
            ob = [ar.alloc([128, D], F32, "pl_ob%d" % i) for i in range(2)]
            ss = [ar.alloc([128, 2], F32, "pl_ss%d" % i) for i in range(2)]
            junk = ar.alloc([128, D], BF16, "pl_junk")
        k = 0
        def pl_loads(tt):
            h, pt = hb[tt % 3], ptb[tt % 3]
            dma("sp", h.ap, Hres.ap[tt * 128:(tt + 1) * 128, :], [H_b[tt]], [h])
            dma("pool", pt.ap, pT_in.ap[L, :, tt * 128:(tt + 1) * 128].rearrange("(c p) t -> p c t", p=128), [pT_in], [pt])

        pl_loads(0)
        pl_loads(1)
        hT_next = nc1.trans(nc1.stats(hb[0]), 3 * L + 2, 0, (ps[6], ps[7]), to_hnt=False)
        prev2 = None
        for tt in range(ntt):
            h, pt = hb[tt % 3], ptb[tt % 3]
            hT = hT_next
            if tt + 1 < ntt:
                hT_next = nc1.trans(nc1.stats(hb[(tt + 1) % 3]), 3 * L + 2, tt + 1, (ps[6], ps[7]), to_hnt=False)
            for db in range(4):
                pG, pE = ps[(2 * k) % 6], ps[(2 * k + 1) % 6]
                s_ = sg[k % 2]
                k += 1
                for c in range(16):
                    op("pe", lambda e, c=c, pG=pG, hT=hT, db=db: e.matmul(pG.ap, hT.ap[:, c, :], pg.ap[:, c, db * 512:(db + 1) * 512],
                                                                          start=(c == 0), stop=(c == 15)), [hT, pg], [pG])
                for c in range(2):
                    op("pe", lambda e, c=c, pE=pE, pt=pt, db=db: e.matmul(pE.ap, pt.ap[:, c, :], pp.ap[:, c, db * 512:(db + 1) * 512],
                                                                          start=(c == 0), stop=(c == 1)), [pt, pp], [pE])
                op("act", lambda e, s_=s_, pG=pG: e.activation(out=s_.ap, in_=pG.ap, func=AF.Sigmoid), [pG], [s_])
                op("dve", lambda e, s_=s_, pE=pE: e.tensor_tensor(s_.ap, s_.ap, pE.ap, ALU.mult), [s_, pE], [s_])
                op("dve", lambda e, s_=s_, h=h, db=db: e.tensor_tensor(h.ap[:, db * 512:(db + 1) * 512], h.ap[:, db * 512:(db + 1) * 512], s_.ap, ALU.add),
                   [s_, h], [h])
            if tt + 2 < ntt:
                pl_loads(tt + 2)
            if L == 0:
                dma("sp", Hres.ap[tt * 128:(tt + 1) * 128, :], h.ap, [h], [H_b[tt]])
                ctx2 = nc2.stats(h)
                if prev2 is not None:
                    nc2.trans(prev2[0], 3, prev2[1], (ps[6], ps[7]))
                prev2 = (ctx2, tt)
            else:
                s2, o = ss[tt % 2], ob[tt % 2]
                op("act", lambda e, s2=s2, h=h: e.activation(out=junk.ap, in_=h.ap, func=AF.Square, accum_out=s2.ap[:, 0:1]), [h], [junk, s2])
                op("act", lambda e, s2=s2: e.activation(out=s2.ap[:, 1:2], in_=s2.ap[:, 0:1], func=AF.Sqrt, scale=1.0 / D, bias=eps_t.ap), [s2, eps_t], [s2])
                op("dve", lambda e, s2=s2: e.reciprocal(s2.ap[:, 1:2], s2.ap[:, 1:2]), [s2], [s2])
                op("dve", lambda e, s2=s2, o=o, h=h: e.scalar_tensor_tensor(o.ap, h.ap, s2.ap[:, 1:2], gfin.ap, ALU.mult, ALU.mult), [h, s2, gfin], [o])
                dma("sp", out_ap.ap[tt * 128:(tt + 1) * 128, :], o.ap, [o], [out_ap])
        if prev2 is not None:
            nc2.trans(prev2[0], 3, prev2[1], (ps[6], ps[7]))

    def run_all():
        phase_prologue()
        if P.phase_end("prologue"):
            return
        for L in range(2):
            phase_inproj(L)
            if P.phase_end("inproj%d" % L):
                return
            phase_mla_prep(L)
            if P.phase_end("mlaprep%d" % L):
                return
            for kind in ("diff", "na", "swa", "mla"):
                attention(L, kind)
                if P.phase_end("%s%d" % (kind, L)):
                    return
            phase_outproj(L)
            if P.phase_end("outproj%d" % L):
                return
            if L == 0:
                for sci in range(2):
                    ffn_up(sci, dwg.ap, dwu.ap, dwg)
                    if P.phase_end("ffnup%d_%d" % (L, sci)):
                        return
                    ffn_down(sci, dwd.ap, dwd, None)
                    if P.phase_end("ffndown%d_%d" % (L, sci)):
                        return
            else:
                for e_ in range(8):
                    ffn_up(0, mwg.ap[e_], mwu.ap[e_], mwg)
                    if P.phase_end("moeup%d" % e_):
                        return
                    ffn_down(0, mwd.ap[e_], mwd, e_)
                    if P.phase_end("moedown%d" % e_):
                        return
            phase_ple(L)
            if P.phase_end("ple%d" % L):
                return

    run_all()
    sc.barrier()
    sc.emit()
    return P


def _rope_tables(pos):
    pos = pos.astype(np.float32)
    f = np.arange(128)

    def tab(dim, fidx, sign):
        inv = (10000.0 ** (-np.arange(0, dim, 2, dtype=np.float32) / dim)).astype(np.float32)
        ang = pos[None, :] * inv[fidx][:, None]
        return np.stack([np.cos(ang), np.sin(ang) * sign[:, None]]).astype(np.float32)

    ropeD = tab(64, (f % 64) % 32, np.where((f % 64) < 32, -1.0, 1.0).astype(np.float32))
    ropeH = tab(128, f % 64, np.where(f < 64, -1.0, 1.0).astype(np.float32))
    return ropeD, ropeH


def _na_tables(rpb, half):
    out = np.full((8, 4, 8, 128, 512), NEG, dtype=np.float32)
    ki = np.arange(128)
    qi = np.arange(512)
    kc = ki % 64
    qr_l = qi // 64
    qcol = qi % 64
    ws = np.clip(qcol - 8, 0, 48)
    colok = (kc[:, None] >= ws[None, :]) & (kc[:, None] < ws[None, :] + 16)
    cidx = np.clip(kc[:, None] - qcol[None, :], -15, 15) + 15
    for qc in range(8):
        R0 = (8 * qc + 32 * half) % 64
        r = R0 + qr_l
        rs = np.clip(r - 4, 0, 56)
        for j in range(8):
            kr0 = R0 - 4 + 2 * j
            if kr0 < 0 or kr0 > 62:
                continue
            kr = kr0 + ki // 64
            rowok = (kr[:, None] >= rs[None, :]) & (kr[:, None] < rs[None, :] + 8)
            ridx = np.clip(kr[:, None] - r[None, :] + 7, 0, 14)
            ok = rowok & colok
            for h in range(4):
                out[qc, h, j] = np.where(ok, rpb[h][ridx, cidx], NEG)
    return out.astype(ml_dtypes.bfloat16)


def _swa_tables(half):
    out = np.full((8, 6, 128, 512), NEG, dtype=np.float32)
    ki = np.arange(128)
    qi = np.arange(512)
    for qc in range(8):
        Q0 = (512 * qc + 2048 * half) % 4096
        for j in range(6):
            k0 = Q0 - 128 + 128 * j
            if k0 < 0 or k0 >= 4096:
                continue
            ok = np.abs((Q0 + qi)[None, :] - (k0 + ki)[:, None]) <= 128
            out[qc, j] = np.where(ok, 0.0, NEG)
    return out.astype(ml_dtypes.bfloat16)


def _fm_cols(g):
    return np.ascontiguousarray(g.reshape(16, 128).T)


def make_in_maps(inputs, cores=range(8)):
    f32 = lambda a: np.ascontiguousarray(a, dtype=np.float32)
    I = inputs
    shared = {}
    shared["wext"] = f32(I["w_in"][:, :, WIN_COLS])
    shared["wout"] = f32(I["w_out"])
    shared["wuq"] = f32(I["mla_w_uq"][:, :, UQ_COLS])
    shared["wukv"] = f32(I["mla_w_ukv"][:, :, UKV_COLS])
    shared["dwg"] = f32(I["dense_w_gate"][0])
    shared["dwu"] = f32(I["dense_w_up"][0])
    shared["dwd"] = f32(I["dense_w_down"][0])
    shared["mwg"] = f32(I["moe_w_gate"][0])
    shared["mwu"] = f32(I["moe_w_up"][0])
    shared["mwd"] = f32(I["moe_w_down"][0])
    shared["routerT"] = f32(I["moe_router"][0].T)
    shared["plegate"] = f32(I["ple_gate"])
    shared["pleproj"] = f32(I["ple_proj"])
    gl = []
    for L in range(2):
        gl += [_fm_cols(I["attn_norm"][L]), _fm_cols(I["ffn_norm"][L]), _fm_cols(I["ple_norm"][L])]
    shared["gains"] = f32(np.stack(gl))
    shared["ffng1"] = f32(I["ffn_norm"][1][None, :])
    shared["finalg"] = f32(I["final_norm"][None, :])
    shared["mlan"] = f32(np.stack([np.concatenate([I["mla_q_norm"][L].reshape(4, 128).T, I["mla_kv_norm"][L].reshape(4, 128).T], axis=1)
                                   for L in range(2)]))
    shared["subln"] = f32(I["diff_subln"][:, :, None])
    shared["lamv"] = f32(np.stack([I["diff_lq1"], I["diff_lk1"], I["diff_lq2"], I["diff_lk2"]], axis=1))
    shared["sinks"] = f32(I["swa_sinks"])
    shared["ident"] = np.eye(128, dtype=np.float32).astype(ml_dtypes.bfloat16)
    per_half = {}
    for half in range(2):
        pos = (np.arange(S) + 2048 * half) % S
        ropeD, ropeH = _rope_tables(pos)
        per_half[half] = dict(
            ropeD=ropeD, ropeH=ropeH,
            natab=np.stack([_na_tables(np.asarray(I["na_rpb"][L], dtype=np.float32), half) for L in range(2)]),
            swatab=_swa_tables(half),
        )
    maps = []
    for c in cores:
        b, half = c // 2, c % 2
        m = dict(shared)
        m.update(per_half[half])
        m["x"] = f32(np.roll(I["x"][b], -2048 * half, axis=0))
        m["pT"] = f32(np.stack([np.roll(I["p"][L, b], -2048 * half, axis=0).T for L in range(2)]))
        maps.append(m)
    return maps


_PROG = None


def kernel(**inputs):
    global _PROG
    if _PROG is None:
        _PROG = build_program()
    P = _PROG
    maps = make_in_maps(inputs)
    maps = [{k: v for k, v in m.items() if k in P.inputs} for m in maps]
    res = run_bass_kernel_spmd(P.nc, maps, core_ids=list(range(8)))
    out = np.empty((4, S, D), dtype=np.float32)
    for c in range(8):
        b, half = c // 2, c % 2
        out[b, 2048 * half:2048 * (half + 1)] = np.asarray(res.results[c]["out"], dtype=np.float32)
    return out
```

```python
import math
from contextlib import ExitStack
import numpy as np
import ml_dtypes
import concourse.bass as bass
import concourse.mybir as mybir
from concourse.bass_utils import run_bass_kernel_spmd

F32 = mybir.dt.float32
BF16 = mybir.dt.bfloat16
U8 = mybir.dt.uint8
AF = mybir.ActivationFunctionType
ALU = mybir.AluOpType
AX = mybir.AxisListType

D = 2048
S = 4096
NC16 = 16
FFN = 7168
NFT = FFN // 128
EPS = 1e-6
NEG = -30000.0
SAME_ENGINE_SYNC = True
KSLOT = 8


class Buf:
    __slots__ = ("name", "w", "r")

    def __init__(self, name=""):
        self.name = name
        self.w = None
        self.r = {}


class T:
    __slots__ = ("ap", "buf")

    def __init__(self, ap, buf=None, name=""):
        self.ap = ap
        self.buf = buf if buf is not None else Buf(name)

    def __getitem__(self, k):
        return self.ap[k]


class LazyIn:
    def __init__(self, P, name, shape, dtype):
        self.P, self.name, self.shape, self.dtype = P, name, list(shape), dtype
        self._t = None

    def _get(self):
        if self._t is None:
            t = self.P.nc.dram_tensor(self.name, self.shape, self.dtype, kind="ExternalInput")
            self.P.inputs[self.name] = (tuple(self.shape), self.dtype)
            self._t = T(t.ap(), name=self.name)
        return self._t

    @property
    def ap(self):
        return self._get().ap

    @property
    def buf(self):
        return self._get().buf


def _bufs(xs):
    out = []
    for x in xs:
        if x is None:
            continue
        out.append(x.buf if isinstance(x, (T, LazyIn)) else x)
    return out


class Sched:
    def __init__(self, nc, es):
        self.nc = nc
        self.names = ["pe", "act", "dve", "pool", "sp"]
        self.ops = {e: [] for e in self.names}
        self.cnt = {e: 0 for e in self.names}
        self.seen = {e: {} for e in self.names}
        self.sem = {e: es.enter_context(nc.semaphore("c_" + e)) for e in self.names}
        self.dq = {}
        for q in ("sp", "pool", "act"):
            self.dq[q] = {"n": 0, "sems": [es.enter_context(nc.semaphore("d_%s%d" % (q, i))) for i in range(KSLOT)]}

    def _wait(self, E, tok):
        if tok[0] == "c":
            _, F, idx = tok
            if F == E and (E == "pe" or not SAME_ENGINE_SYNC):
                return
            key, val, sem = F, idx, self.sem[F]
        else:
            _, q, slot, c = tok
            key, val, sem = (q, slot), 16 * c, self.dq[q]["sems"][slot]
        if self.seen[E].get(key, 0) >= val:
            return
        self.seen[E][key] = val
        self.ops[E].append(lambda e, sem=sem, val=val: e.wait_ge(sem, val))

    @staticmethod
    def _key(tok):
        return tok[1] if tok[0] == "c" else (tok[1], tok[2])

    def _deps(self, reads, writes):
        toks = []
        for b in reads:
            if b.w is not None:
                toks.append(b.w)
        for b in writes:
            if b.w is not None:
                toks.append(b.w)
            toks.extend(b.r.values())
        return toks

    def _mark(self, tok, reads, writes):
        k = self._key(tok)
        for b in reads:
            b.r[k] = tok
        for b in writes:
            b.w = tok
            b.r = {}

    def op(self, E, fn, reads=(), writes=()):
        reads = _bufs(reads)
        writes = _bufs(writes)
        for t in self._deps(reads, writes):
            self._wait(E, t)
        self.cnt[E] += 1
        n = self.cnt[E]
        sem = self.sem[E]
        self.ops[E].append(lambda e, fn=fn, sem=sem: fn(e).then_inc(sem, 1))
        self._mark(("c", E, n), reads, writes)

    def dma(self, q, out, in_, reads=(), writes=()):
        reads = _bufs(reads)
        writes = _bufs(writes)
        dq = self.dq[q]
        n = dq["n"]
        dq["n"] += 1
        slot, c = n % KSLOT, n // KSLOT + 1
        toks = self._deps(reads, writes)
        if c > 1:
            toks.append(("d", q, slot, c - 1))
        for t in toks:
            self._wait(q, t)
        sem = dq["sems"][slot]
        self.ops[q].append(lambda e, out=out, in_=in_, sem=sem: e.dma_start(out=out, in_=in_).then_inc(sem, 16))
        self._mark(("d", q, slot, c), reads, writes)

    def barrier(self):
        toks = [("c", e, self.cnt[e]) for e in self.names if self.cnt[e] > 0]
        for q, dq in self.dq.items():
            n = dq["n"]
            for slot in range(KSLOT):
                c = (n - slot + KSLOT - 1) // KSLOT
                if c > 0:
                    toks.append(("d", q, slot, c))
        save = SAME_ENGINE_SYNC
        for E in self.names:
            for t in toks:
                if t[0] == "c" and t[1] == E:
                    continue
                self._wait(E, t)

    def emit(self):
        nc = self.nc
        with nc.Block() as block:
            @block.tensor
            def _(e):
                for f in self.ops["pe"]:
                    f(e)

            @block.scalar
            def _(e):
                for f in self.ops["act"]:
                    f(e)

            @block.vector
            def _(e):
                for f in self.ops["dve"]:
                    f(e)

            @block.gpsimd
            def _(e):
                for f in self.ops["pool"]:
                    f(e)

            @block.sync
            def _(e):
                for f in self.ops["sp"]:
                    f(e)


class Arena:
    def __init__(self, nc, nbytes):
        self.t = nc.alloc_sbuf_tensor("arena", [128, nbytes], U8)
        self.size = nbytes
        self.off = 0
        self.top = nbytes

    def reset(self):
        self.off = 0

    def alloc(self, shape, dtype, name="", persist=False):
        esz = 2 if dtype == BF16 else 4
        n = 1
        for s in shape[1:]:
            n *= s
        nb = (n * esz + 63) // 64 * 64
        if persist:
            self.top -= nb
            o = self.top
        else:
            o = self.off
            self.off += nb
        assert self.off <= self.top, "SBUF arena overflow %s %d %d" % (name, self.off, self.top)
        ap = self.t[:, o:o + n * esz].bitcast(dtype)
        if len(shape) == 3:
            ap = ap.rearrange("p (a b) -> p a b", a=shape[1])
        elif len(shape) == 4:
            ap = ap.rearrange("p (a b c) -> p a b c", a=shape[1], b=shape[2])
        if shape[0] < 128:
            ap = ap[0:shape[0]]
        return T(ap, name=name)


def _win_layout():
    cols = []
    tiles = []

    def add(name, src, rope=None, q_only=False, partner=None):
        tiles.append(dict(name=name, off=len(cols), rope=rope, q_only=q_only))
        cols.extend(src)
        if rope is not None:
            tiles.append(dict(name=name + "p", off=len(cols), rope="partner", q_only=q_only))
            cols.extend(partner)

    def swap64x2(base):
        return [base + blk * 64 + (i + 32) % 64 for blk in range(2) for i in range(64)]

    def swap128(base):
        return [base + (i + 64) % 128 for i in range(128)]

    for h in range(4):
        add("QD%d" % h, list(range(h * 128, h * 128 + 128)), "D", True, swap64x2(h * 128))
    for h in range(4):
        add("KD%d" % h, list(range(512 + h * 128, 512 + h * 128 + 128)), "D", False, swap64x2(512 + h * 128))
    for h in range(4):
        add("QS%d" % h, list(range(3072 + h * 128, 3072 + h * 128 + 128)), "H", True, swap128(3072 + h * 128))
    for g in range(2):
        add("KS%d" % g, list(range(3584 + g * 128, 3584 + g * 128 + 128)), "H", False, swap128(3584 + g * 128))
    kr = list(range(5120, 5184))
    krp = [5120 + (i + 32) % 64 for i in range(64)]
    add("KR", kr + kr, "D", False, krp + krp)
    assert len(tiles) % 2 == 0
    for h in range(4):
        add("QN%d" % h, list(range(1536 + h * 128, 1536 + h * 128 + 128)), None, True)
    for h in range(4):
        add("CQ%d" % h, list(range(4096 + h * 128, 4096 + h * 128 + 128)), None, True)
    for h in range(4):
        add("KN%d" % h, list(range(2048 + h * 128, 2048 + h * 128 + 128)), None, False)
    for h in range(4):
        add("CKV%d" % h, list(range(4608 + h * 128, 4608 + h * 128 + 128)), None, False)
    tm = []
    for name, lo, n in (("VD", 1024, 512), ("VN", 2560, 512), ("VS", 3840, 256)):
        tm.append(dict(name=name, off=len(cols), n=n))
        cols.extend(range(lo, lo + n))
    return np.array(cols, dtype=np.int64), tiles, tm


WIN_COLS, FM_TILES, TM_BLOCKS = _win_layout()
NWEXT = len(WIN_COLS)


def _uq_cols():
    nope = [192 * h + i for h in range(4) for i in range(128)]
    rope = [192 * h + 128 + i for h in range(4) for i in range(64)]
    ropep = [192 * h + 128 + (i + 32) % 64 for h in range(4) for i in range(64)]
    return np.array(nope + rope[0:128] + ropep[0:128] + rope[128:256] + ropep[128:256], dtype=np.int64)


def _ukv_cols():
    kn = [256 * h + i for h in range(4) for i in range(128)]
    v = [256 * h + 128 + i for h in range(4) for i in range(128)]
    return np.array(kn + v, dtype=np.int64)


UQ_COLS = _uq_cols()
UKV_COLS = _ukv_cols()


class Prog:
    def __init__(self, stop_after=None, dump=()):
        self.stop_after = stop_after
        self.dump = set(dump)
        self.es = ExitStack()
        nc = bass.Bass("TRN2", target_bir_lowering=False)
        self.nc = nc
        self.sc = Sched(nc, self.es)
        self.ar = Arena(nc, 206 * 1024)
        self.psbig = [nc.alloc_psum_tensor("psb%d" % i, [128, 1024], F32)[:] for i in range(4)]
        self.ps = [T(self.psbig[i // 2][:, (i % 2) * 512:(i % 2 + 1) * 512], name="ps%d" % i) for i in range(8)]
        self.inputs = {}
        self.scr = {}
        self.done = False

    def inp(self, name, shape, dtype=F32):
        return LazyIn(self, name, shape, dtype)

    def scratch(self, name, shape, dtype):
        kind = "ExternalOutput" if name in self.dump else "Internal"
        t = self.nc.dram_tensor(name, list(shape), dtype, kind=kind)
        r = T(t.ap(), name=name)
        self.scr[name] = r
        return r

    def phase_end(self, name):
        self.sc.barrier()
        self.ar.reset()
        if self.stop_after == name:
            self.done = True
        return self.done


def build_program(stop_after=None, dump=()):
    P = Prog(stop_after, dump)
    nc, sc, ar, ps = P.nc, P.sc, P.ar, P.ps
    op, dma = sc.op, sc.dma

    x_in = P.inp("x", [S, D])
    pT_in = P.inp("pT", [2, 256, S])
    wext = P.inp("wext", [2, D, NWEXT])
    wout = P.inp("wout", [2, D, D])
    wuq = P.inp("wuq", [2, 512, 1024])
    wukv = P.inp("wukv", [2, 512, 1024])
    dwg = P.inp("dwg", [D, FFN])
    dwu = P.inp("dwu", [D, FFN])
    dwd = P.inp("dwd", [FFN, D])
    mwg = P.inp("mwg", [8, D, FFN])
    mwu = P.inp("mwu", [8, D, FFN])
    mwd = P.inp("mwd", [8, FFN, D])
    routerT = P.inp("routerT", [8, D])
    plegate = P.inp("plegate", [2, D, D])
    pleproj = P.inp("pleproj", [2, 256, D])
    gains = P.inp("gains", [6, 128, 16])
    finalg = P.inp("finalg", [1, D])
    ffng1 = P.inp("ffng1", [1, D])
    mlan = P.inp("mlan", [2, 128, 8])
    subln = P.inp("subln", [2, 128, 1])
    lam_in = P.inp("lamv", [2, 4, 64])
    sinks = P.inp("sinks", [2, 4])
    natab = P.inp("natab", [2, 8, 4, 8, 128, 512], BF16)
    swatab = P.inp("swatab", [8, 6, 128, 512], BF16)
    ropeD = P.inp("ropeD", [2, 128, S])
    ropeH = P.inp("ropeH", [2, 128, S])
    ident_in = P.inp("ident", [128, 128], BF16)
    out_t = P.nc.dram_tensor("out", [2048, D], F32, kind="ExternalOutput")
    out_ap = T(out_t.ap(), name="out")

    Hres = P.scratch("Hres", [S, D], F32)
    HNT = P.scratch("HNT", [16, 128, S], BF16)
    fm_names = [t["name"] for t in FM_TILES if t["rope"] != "partner"]
    FMS = {n: P.scratch("z_" + n, [128, S], BF16) for n in fm_names}
    TMS = {b["name"]: P.scratch("z_" + b["name"], [S, b["n"]], BF16) for b in TM_BLOCKS}
    QMN = [P.scratch("QMN%d" % h, [128, S], BF16) for h in range(4)]
    QMR = [P.scratch("QMR%d" % h, [128, S], BF16) for h in range(2)]
    KMN = [P.scratch("KMN%d" % h, [128, S], BF16) for h in range(4)]
    VM = P.scratch("VM", [S, 512], BF16)
    MIXT = P.scratch("MIXT", [16, 128, S], BF16)
    ACTT = P.scratch("ACTT", [16, 128, NFT, 128], BF16)
    GATES = P.scratch("GATES", [2048, 8], F32)

    H_b = [Buf("H%d" % i) for i in range(32)]
    HNT_b = [Buf("HNT%d" % i) for i in range(32)]

    ident = ar.alloc([128, 128], BF16, "ident", persist=True)
    ones_f = ar.alloc([128, 128], F32, "ones_f", persist=True)
    ones_b = ar.alloc([128, 128], BF16, "ones_b", persist=True)
    gains_sb = ar.alloc([128, 6, 16], F32, "gains", persist=True)
    dma("sp", ident.ap, ident_in.ap, [ident_in], [ident])
    dma("sp", gains_sb.ap, gains.ap.rearrange("g p c -> p g c"), [gains], [gains_sb])
    op("dve", lambda e: e.memset(ones_f.ap, 1.0), [], [ones_f])
    op("dve", lambda e: e.memset(ones_b.ap, 1.0), [], [ones_b])
    eps_t = ar.alloc([128, 1], F32, "eps", persist=True)
    op("dve", lambda e: e.memset(eps_t.ap, EPS), [], [eps_t])

    def norm_transpose(h_sb, gidx, tt, pbank, extra_hn_f32=None):
        raise NotImplementedError

    class NormCtx:
        def __init__(self):
            self.junk = ar.alloc([128, D], BF16, "nt_junk")
            self.ss = [ar.alloc([128, 1], F32, "nt_ss%d" % i) for i in range(2)]
            self.rstd = [ar.alloc([128, 1], F32, "nt_rstd%d" % i) for i in range(2)]
            self.hs = [ar.alloc([128, D], BF16, "nt_hs%d" % i) for i in range(2)]
            self.hT = [ar.alloc([128, 16, 128], BF16, "nt_hT%d" % i) for i in range(2)]
            self.i = 0

        def stats(self, h_sb):
            k = self.i % 2
            self.i += 1
            ss, rstd, hs = self.ss[k], self.rstd[k], self.hs[k]
            op("act", lambda e: e.activation(out=self.junk.ap, in_=h_sb.ap, func=AF.Square, accum_out=ss.ap),
               [h_sb], [self.junk, ss])
            op("act", lambda e: e.activation(out=rstd.ap, in_=ss.ap, func=AF.Sqrt, scale=1.0 / D, bias=eps_t.ap), [ss, eps_t], [rstd])
            op("dve", lambda e: e.reciprocal(rstd.ap, rstd.ap), [rstd], [rstd])
            op("act", lambda e: e.activation(out=hs.ap, in_=h_sb.ap, func=AF.Copy, scale=rstd.ap), [h_sb, rstd], [hs])
            return dict(k=k, rstd=rstd, hs=hs)

        def trans(self, ctx, gidx, tt, pbanks, to_hnt=True):
            hs, hT = ctx["hs"], self.hT[ctx["k"]]
            for half in range(2):
                pb = pbanks[half]
                pv = pb.ap.bitcast(BF16).rearrange("p (a b) -> p a b", a=8)
                for c in range(8):
                    cc = half * 8 + c
                    op("pe", lambda e, c=c, cc=cc, pv=pv: e.transpose(pv[:, c, :], hs.ap[:, cc * 128:(cc + 1) * 128], ident.ap),
                       [hs, ident], [pb])
                g = gains_sb.ap[:, gidx, half * 8:(half + 1) * 8]
                gb = g.unsqueeze(2).to_broadcast([128, 8, 128])
                op("dve", lambda e, pv=pv, gb=gb, half=half: e.tensor_tensor(hT.ap[:, half * 8:(half + 1) * 8, :], pv, gb, ALU.mult),
                   [pb, gains_sb], [hT])
            if to_hnt:
                dma("sp", HNT.ap[:, :, tt * 128:(tt + 1) * 128].rearrange("c p t -> p c t"), hT.ap, [hT], [HNT_b[tt]])
            return hT

        def run(self, h_sb, gidx, tt, pbanks, hn_f32=None, to_hnt=True):
            ctx = self.stats(h_sb)
            hT = self.trans(ctx, gidx, tt, pbanks, to_hnt)
            return ctx["rstd"] if to_hnt else hT

    def phase_prologue():
        nctx = NormCtx()
        hb = [ar.alloc([128, D], F32, "pro_h%d" % i) for i in range(3)]
        prev = None
        for tt in range(32):
            h = hb[tt % 3]
            dma("sp", h.ap, x_in.ap[tt * 128:(tt + 1) * 128, :], [x_in], [h])
            dma("sp", Hres.ap[tt * 128:(tt + 1) * 128, :], h.ap, [h], [H_b[tt]])
            ctx = nctx.stats(h)
            if prev is not None:
                nctx.trans(prev[0], 0, prev[1], (ps[(2 * prev[1]) % 8], ps[(2 * prev[1] + 1) % 8]))
            prev = (ctx, tt)
        nctx.trans(prev[0], 0, prev[1], (ps[(2 * prev[1]) % 8], ps[(2 * prev[1] + 1) % 8]))

    def phase_inproj(L):
        nq_chunks = 8 if L == 0 else 4
        hn = [ar.alloc([128, 16, 1024], BF16, "ip_hn%d" % i) for i in range(2)]
        wu = [ar.alloc([128, 16, 256], BF16, "ip_w%d" % i) for i in range(3)]
        wb = [ar.alloc([128, 16, 512], BF16, "ip_wb%d" % i) for i in range(2)]
        rt = [ar.alloc([128, 4, 512], F32, "ip_rt%d" % i) for i in range(2)]
        t1 = [ar.alloc([128, 512], F32, "ip_t1%d" % i) for i in range(2)]
        t2 = [ar.alloc([128, 512], F32, "ip_t2%d" % i) for i in range(2)]
        ob = [ar.alloc([128, 512], BF16, "ip_ob%d" % i) for i in range(4)]
        units = [(FM_TILES[i], FM_TILES[i + 1]) for i in range(0, len(FM_TILES), 2)]
        wi = 0
        oi = 0
        pi = 0
        def load_hn(sc_j):
            dma("sp", hn[sc_j % 2].ap, HNT.ap[:, :, sc_j * 1024:(sc_j + 1) * 1024].rearrange("c p t -> p c t"),
                [HNT_b[sc_j * 8 + j] for j in range(8)], [hn[sc_j % 2]])

        load_hn(0)
        for sc_i in range(4):
            hnb = hn[sc_i % 2]
            t0 = sc_i * 1024
            for ch in range(2):
                tok0 = t0 + ch * 512
                dma("sp", rt[ch].ap[:, 0:2, :], ropeD.ap[:, :, tok0:tok0 + 512].rearrange("a p t -> p a t"), [ropeD], [rt[ch]])
                dma("sp", rt[ch].ap[:, 2:4, :], ropeH.ap[:, :, tok0:tok0 + 512].rearrange("a p t -> p a t"), [ropeH], [rt[ch]])
            if sc_i + 1 < 4:
                load_hn(sc_i + 1)
            for (ta, tb) in units:
                chunks = [ch for ch in range(2) if not (ta["q_only"] and (sc_i * 2 + ch) >= nq_chunks)]
                if not chunks:
                    continue
                w = wu[wi % 3]
                wi += 1
                dma("pool", w.ap, wext.ap[L, :, ta["off"]:ta["off"] + 256].rearrange("(c p) n -> p c n", p=128), [wext], [w])
                for ch in chunks:
                    tok0 = t0 + ch * 512
                    pa, pb = ps[pi % 8], ps[(pi + 1) % 8]
                    pi += 2
                    for c in range(16):
                        op("pe", lambda e, c=c, pa=pa, w=w, hnb=hnb, ch=ch: e.matmul(
                            pa.ap, w.ap[:, c, 0:128], hnb.ap[:, c, ch * 512:(ch + 1) * 512], start=(c == 0), stop=(c == 15)),
                           [w, hnb], [pa])
                    for c in range(16):
                        op("pe", lambda e, c=c, pb=pb, w=w, hnb=hnb, ch=ch: e.matmul(
                            pb.ap, w.ap[:, c, 128:256], hnb.ap[:, c, ch * 512:(ch + 1) * 512], start=(c == 0), stop=(c == 15)),
                           [w, hnb], [pb])
                    if ta["rope"] is not None:
                        r = rt[ch]
                        ro = 0 if ta["rope"] == "D" else 2
                        a1, a2 = t1[oi % 2], t2[oi % 2]
                        o = ob[oi % 4]
                        oi += 1
                        op("dve", lambda e, a1=a1, pa=pa, r=r, ro=ro: e.tensor_tensor(a1.ap, pa.ap, r.ap[:, ro, :], ALU.mult), [pa, r], [a1])
                        op("dve", lambda e, a2=a2, pb=pb, r=r, ro=ro: e.tensor_tensor(a2.ap, pb.ap, r.ap[:, ro + 1, :], ALU.mult), [pb, r], [a2])
                        op("dve", lambda e, a1=a1, a2=a2, o=o: e.tensor_tensor(o.ap, a1.ap, a2.ap, ALU.add), [a1, a2], [o])
                        dst = FMS[ta["name"]]
                        dma("sp", dst.ap[:, tok0:tok0 + 512], o.ap, [o], [dst])
                    else:
                        for (tl, pp) in ((ta, pa), (tb, pb)):
                            o = ob[oi % 4]
                            oi += 1
                            op("act", lambda e, o=o, pp=pp: e.activation(out=o.ap, in_=pp.ap, func=AF.Copy), [pp], [o])
                            dst = FMS[tl["name"]]
                            dma("sp", dst.ap[:, tok0:tok0 + 512], o.ap, [o], [dst])
            for bi, blk in enumerate(TM_BLOCKS):
                w = wb[bi % 2]
                n = blk["n"]
                dma("pool", w.ap[:, :, 0:n], wext.ap[L, :, blk["off"]:blk["off"] + n].rearrange("(c p) n -> p c n", p=128), [wext], [w])
                for tt in range(8):
                    pa = ps[pi % 8]
                    pi += 1
                    for c in range(16):
                        op("pe", lambda e, c=c, pa=pa, w=w, hnb=hnb, tt=tt, n=n: e.matmul(
                            pa.ap[:, 0:n], hnb.ap[:, c, tt * 128:(tt + 1) * 128], w.ap[:, c, 0:n], start=(c == 0), stop=(c == 15)),
                           [w, hnb], [pa])
                    o = ob[oi % 4]
                    oi += 1
                    op("act", lambda e, o=o, pa=pa, n=n: e.activation(out=o.ap[:, 0:n], in_=pa.ap[:, 0:n], func=AF.Copy), [pa], [o])
                    dst = TMS[blk["name"]]
                    dma("sp", dst.ap[t0 + tt * 128:t0 + (tt + 1) * 128, :], o.ap[:, 0:n], [o], [dst])

    def phase_mla_prep(L):
        nq_chunks = 8 if L == 0 else 4
        wq = ar.alloc([128, 4, 1024], BF16, "mp_wq")
        wk = ar.alloc([128, 4, 1024], BF16, "mp_wk")
        nrm = ar.alloc([128, 8], F32, "mp_nrm")
        dma("pool", wq.ap, wuq.ap[L].rearrange("(c p) n -> p c n", p=128), [wuq], [wq])
        dma("pool", wk.ap, wukv.ap[L].rearrange("(c p) n -> p c n", p=128), [wukv], [wk])
        dma("sp", nrm.ap, mlan.ap[L], [mlan], [nrm])
        cin = [ar.alloc([128, 4, 512], BF16, "mp_cin%d" % i) for i in range(2)]
        sq = [ar.alloc([128, 4, 512], F32, "mp_sq%d" % i) for i in range(2)]
        rstd = [ar.alloc([128, 512], F32, "mp_rstd%d" % i) for i in range(2)]
        cn = [ar.alloc([128, 4, 512], BF16, "mp_cn%d" % i) for i in range(2)]
        rt = [ar.alloc([128, 2, 512], F32, "mp_rt%d" % i) for i in range(2)]
        t1 = [ar.alloc([128, 512], F32, "mp_t1%d" % i) for i in range(2)]
        t2 = [ar.alloc([128, 512], F32, "mp_t2%d" % i) for i in range(2)]
        ob = [ar.alloc([128, 512], BF16, "mp_ob%d" % i) for i in range(4)]
        st = dict(i=0, oi=0, pi=0)

        def normed(src_names, noff, tok0):
            k = st["i"] % 2
            st["i"] += 1
            ci, s2, rs, cno = cin[k], sq[k], rstd[k], cn[k]
            for t in range(4):
                src = FMS[src_names[t]]
                dma("sp", ci.ap[:, t, :], src.ap[:, tok0:tok0 + 512], [src], [ci])
            op("act", lambda e: e.activation(out=s2.ap, in_=ci.ap, func=AF.Square), [ci], [s2])
            pm = ps[st["pi"] % 8]
            st["pi"] += 1
            for t in range(4):
                op("pe", lambda e, t=t: e.matmul(pm.ap, ones_f.ap, s2.ap[:, t, :], start=(t == 0), stop=(t == 3)), [ones_f, s2], [pm])
            op("act", lambda e: e.activation(out=rs.ap, in_=pm.ap, func=AF.Sqrt, scale=1.0 / 512, bias=eps_t.ap), [pm, eps_t], [rs])
            op("dve", lambda e: e.reciprocal(rs.ap, rs.ap), [rs], [rs])
            for t in range(4):
                op("dve", lambda e, t=t: e.scalar_tensor_tensor(cno.ap[:, t, :], ci.ap[:, t, :], nrm.ap[:, noff + t:noff + t + 1],
                                                                  rs.ap, ALU.mult, ALU.mult), [ci, nrm, rs], [cno])
            return cno

        def evac_plain(pp, dst, tok0, n=512):
            o = ob[st["oi"] % 4]
            st["oi"] += 1
            op("act", lambda e: e.activation(out=o.ap[:, 0:n], in_=pp.ap[:, 0:n], func=AF.Copy), [pp], [o])
            return o

        for ch in range(8):
            tok0 = ch * 512
            cno = normed(["CKV%d" % t for t in range(4)], 4, tok0)
            for h in range(4):
                pp = ps[st["pi"] % 8]
                st["pi"] += 1
                for c in range(4):
                    op("pe", lambda e, c=c, h=h, pp=pp, cno=cno: e.matmul(pp.ap, wk.ap[:, c, h * 128:(h + 1) * 128], cno.ap[:, c, :],
                                                                start=(c == 0), stop=(c == 3)), [wk, cno], [pp])
                o = evac_plain(pp, None, tok0)
                dma("sp", KMN[h].ap[:, tok0:tok0 + 512], o.ap, [o], [KMN[h]])
            for tt in range(4):
                pp = ps[st["pi"] % 8]
                st["pi"] += 1
                for c in range(4):
                    op("pe", lambda e, c=c, tt=tt, pp=pp, cno=cno: e.matmul(pp.ap, cno.ap[:, c, tt * 128:(tt + 1) * 128], wk.ap[:, c, 512:1024],
                                                                 start=(c == 0), stop=(c == 3)), [wk, cno], [pp])
                o = evac_plain(pp, None, tok0)
                dma("sp", VM.ap[tok0 + tt * 128:tok0 + (tt + 1) * 128, :], o.ap, [o], [VM])
            if ch >= nq_chunks:
                continue
            cno = normed(["CQ%d" % t for t in range(4)], 0, tok0)
            for h in range(4):
                pp = ps[st["pi"] % 8]
                st["pi"] += 1
                for c in range(4):
                    op("pe", lambda e, c=c, h=h, pp=pp, cno=cno: e.matmul(pp.ap, wq.ap[:, c, h * 128:(h + 1) * 128], cno.ap[:, c, :],
                                                                start=(c == 0), stop=(c == 3)), [wq, cno], [pp])
                o = evac_plain(pp, None, tok0)
                dma("sp", QMN[h].ap[:, tok0:tok0 + 512], o.ap, [o], [QMN[h]])
            r = rt[ch % 2]
            dma("sp", r.ap, ropeD.ap[:, :, tok0:tok0 + 512].rearrange("a p t -> p a t"), [ropeD], [r])
            for pr in range(2):
                pa, pb = ps[st["pi"] % 8], ps[(st["pi"] + 1) % 8]
                st["pi"] += 2
                base = 512 + pr * 256
                for c in range(4):
                    op("pe", lambda e, c=c, pa=pa, base=base, cno=cno: e.matmul(pa.ap, wq.ap[:, c, base:base + 128], cno.ap[:, c, :],
                                                                       start=(c == 0), stop=(c == 3)), [wq, cno], [pa])
                for c in range(4):
                    op("pe", lambda e, c=c, pb=pb, base=base, cno=cno: e.matmul(pb.ap, wq.ap[:, c, base + 128:base + 256], cno.ap[:, c, :],
                                                                       start=(c == 0), stop=(c == 3)), [wq, cno], [pb])
                a1, a2 = t1[pr], t2[pr]
                o = ob[st["oi"] % 4]
                st["oi"] += 1
                op("dve", lambda e, a1=a1, pa=pa, r=r: e.tensor_tensor(a1.ap, pa.ap, r.ap[:, 0, :], ALU.mult), [pa, r], [a1])
                op("dve", lambda e, a2=a2, pb=pb, r=r: e.tensor_tensor(a2.ap, pb.ap, r.ap[:, 1, :], ALU.mult), [pb, r], [a2])
                op("dve", lambda e, a1=a1, a2=a2, o=o: e.tensor_tensor(o.ap, a1.ap, a2.ap, ALU.add), [a1, a2], [o])
                dma("sp", QMR[pr].ap[:, tok0:tok0 + 512], o.ap, [o], [QMR[pr]])

    def attention(L, kind):
        LA = 1
        nq_chunks = 8 if L == 0 else 4
        lam_init = 0.8 - 0.6 * math.exp(-0.3 * L)
        kTb = [ar.alloc([128, S], BF16, "at_kT%d" % i) for i in range(2)]
        kT2 = ar.alloc([128, S], BF16, "at_kT2") if kind == "mla" else None
        Vb = [ar.alloc([128, 32, 128], BF16, "at_V%d" % i) for i in range(2)]
        qT = [ar.alloc([128, 512], BF16, "at_qT%d" % i) for i in range(2)]
        qT2 = [ar.alloc([128, 512], BF16, "at_qT2%d" % i) for i in range(2)] if kind == "mla" else None
        pT = [ar.alloc([128, 1024], BF16, "at_pT%d" % i) for i in range(3)]
        sb = [ar.alloc([128, 1024], F32, "at_sb%d" % i) for i in range(2)] if kind in ("na", "swa") else None
        accb = [ar.alloc([128, 1024], F32, "at_acc%d" % i) for i in range(2)]
        tab = [ar.alloc([128, 8, 512], BF16, "at_tab%d" % i) for i in range(2)] if kind in ("na", "swa") else None
        rl = ar.alloc([128, 512], F32, "at_rl")
        o1 = ar.alloc([128, 512], F32, "at_o1")
        o2 = ar.alloc([128, 512], F32, "at_o2")
        osq = ar.alloc([128, 512], F32, "at_osq")
        ob = [ar.alloc([128, 512], BF16, "at_ob%d" % i) for i in range(2)]
        small = ar.alloc([128, 16], F32, "at_small")
        lamt = ar.alloc([128, 4, 64], F32, "at_lamt")
        junk = ar.alloc([128, 64], F32, "at_junk")
        S_banks = ps[0:4]
        O_banks = ps[4:6]
        L_banks = ps[6:8]
        mix0 = dict(diff=0, na=4, swa=8, mla=12)[kind]
        scale = dict(diff=64 ** -0.5, na=128 ** -0.5, swa=128 ** -0.5, mla=192 ** -0.5)[kind]
        if kind == "diff":
            dma("sp", lamt.ap, lam_in.ap[L].unsqueeze(0).to_broadcast([128, 4, 64]), [lam_in], [lamt])
            op("dve", lambda e: e.tensor_tensor(junk.ap, lamt.ap[:, 0, :], lamt.ap[:, 1, :], ALU.mult), [lamt], [junk])
            op("dve", lambda e: e.reduce_sum(small.ap[:, 0:1], junk.ap, AX.X), [junk], [small])
            op("dve", lambda e: e.tensor_tensor(junk.ap, lamt.ap[:, 2, :], lamt.ap[:, 3, :], ALU.mult), [lamt, small], [junk])
            op("dve", lambda e: e.reduce_sum(small.ap[:, 1:2], junk.ap, AX.X), [junk], [small])
            op("act", lambda e: e.activation(out=small.ap[:, 2:4], in_=small.ap[:, 0:2], func=AF.Exp), [small], [small])
            op("dve", lambda e: e.tensor_tensor(small.ap[:, 4:5], small.ap[:, 3:4], small.ap[:, 2:3], ALU.subtract), [small], [small])
            op("dve", lambda e: e.tensor_scalar(small.ap[:, 4:5], small.ap[:, 4:5], -lam_init, None, ALU.add), [small], [small])
            dma("sp", small.ap[:, 5:6], subln.ap[L], [subln], [small])
            op("dve", lambda e: e.tensor_scalar(small.ap[:, 5:6], small.ap[:, 5:6], 1.0 - lam_init, None, ALU.mult), [small], [small])
        if kind == "swa":
            dma("sp", small.ap[:, 0:4], sinks.ap[L:L + 1, :].to_broadcast([128, 4]), [sinks], [small])
            op("act", lambda e: e.activation(out=small.ap[:, 4:8], in_=small.ap[:, 0:4], func=AF.Exp), [small], [small])
        st = dict(si=0, pi=0, oi=0, sbi=0)

        def kvbuf(h):
            return (h // 2) % 2 if kind == "swa" else h % 2

        def load_head(h):
            if kind == "swa" and h % 2 == 1:
                return
            if kind == "diff":
                ksrc, vsrc, vcol = FMS["KD%d" % h], TMS["VD"], h * 128
            elif kind == "na":
                ksrc, vsrc, vcol = FMS["KN%d" % h], TMS["VN"], h * 128
            elif kind == "swa":
                ksrc, vsrc, vcol = FMS["KS%d" % (h // 2)], TMS["VS"], (h // 2) * 128
            else:
                ksrc, vsrc, vcol = KMN[h], VM, h * 128
            kT, V = kTb[kvbuf(h)], Vb[kvbuf(h)]
            dma("sp", kT.ap, ksrc.ap, [ksrc], [kT])
            dma("sp", V.ap, vsrc.ap[:, vcol:vcol + 128].rearrange("(t p) d -> p t d", p=128), [vsrc], [V])

        groups = [(h, qc) for h in range(4) for qc in range(nq_chunks)]

        def load_group(gi):
            h, qc = groups[gi]
            qsrc = dict(diff=lambda: FMS["QD%d" % h], na=lambda: FMS["QN%d" % h], swa=lambda: FMS["QS%d" % h], mla=lambda: QMN[h])[kind]()
            q = qT[gi % 2]
            dma("sp", q.ap, qsrc.ap[:, qc * 512:(qc + 1) * 512], [qsrc], [q])
            if kind == "mla":
                q2 = qT2[gi % 2]
                dma("sp", q2.ap, QMR[h // 2].ap[:, qc * 512:(qc + 1) * 512], [QMR[h // 2]], [q2])
            if kind == "na":
                tb = tab[gi % 2]
                dma("sp", tb.ap, natab.ap[L, qc, h].rearrange("j p q -> p j q"), [natab], [tb])
            elif kind == "swa":
                tb = tab[gi % 2]
                dma("sp", tb.ap[:, 0:6, :], swatab.ap[qc].rearrange("j p q -> p j q"), [swatab], [tb])

        if kind == "mla":
            dma("sp", kT2.ap, FMS["KR"].ap, [FMS["KR"]], [kT2])
        load_head(0)
        load_group(0)

        dense = kind in ("diff", "mla")

        def emit_S(kT, q, q2, tb, h, comp, pair):
            pp = st["si"] % 2
            st["si"] += 1
            banks = (ps[2 * pp], ps[2 * pp + 1])
            big = P.psbig[pp]
            for (kt, j), Sb in zip(pair, banks):
                ks = slice(kt * 128, (kt + 1) * 128)
                if kind == "diff":
                    pr = slice(comp * 64, comp * 64 + 64)
                    op("pe", lambda e, Sb=Sb, ks=ks, pr=pr: e.matmul(Sb.ap, kT.ap[pr, ks], q.ap[pr, :], start=True, stop=True), [kT, q], [Sb])
                elif kind == "mla":
                    pr = slice((h % 2) * 64, (h % 2) * 64 + 64)
                    op("pe", lambda e, Sb=Sb, ks=ks: e.matmul(Sb.ap, kT.ap[:, ks], q.ap, start=True, stop=False), [kT, q], [Sb])
                    op("pe", lambda e, Sb=Sb, ks=ks, pr=pr: e.matmul(Sb.ap, kT2.ap[pr, ks], q2.ap[pr, :], start=False, stop=True), [kT2, q2], [Sb])
                else:
                    op("pe", lambda e, Sb=Sb, ks=ks: e.matmul(Sb.ap, kT.ap[:, ks], q.ap, start=True, stop=True), [kT, q], [Sb])
            p = pT[st["pi"] % 3]
            st["pi"] += 1
            j0 = pair[0][1]
            if j0 is None:
                op("act", lambda e: e.activation(out=p.ap, in_=big, func=AF.Exp, scale=scale), list(banks), [p])
            else:
                s_ = sb[st["sbi"] % 2]
                st["sbi"] += 1
                tbv = tb.ap[:, j0:j0 + 2, :].rearrange("p a b -> p (a b)")
                op("dve", lambda e: e.scalar_tensor_tensor(s_.ap, big, scale, tbv, ALU.mult, ALU.add), list(banks) + [tb], [s_])
                op("act", lambda e: e.activation(out=p.ap, in_=s_.ap, func=AF.Exp), [s_], [p])
            return p

        def emit_OL(V, Ob, Lb, pair, p, pi_, npairs, acc):
            for t, (kt, j) in enumerate(pair):
                first = (pi_ == 0 and t == 0)
                last = (pi_ == npairs - 1 and t == 1)
                pv = p.ap[:, t * 512:(t + 1) * 512]
                op("pe", lambda e, kt=kt, pv=pv, first=first, last=last: e.matmul(Ob.ap, V.ap[:, kt, :], pv, start=first, stop=last), [V, p], [Ob])
                if not dense:
                    op("pe", lambda e, pv=pv, first=first, last=last: e.matmul(Lb.ap, ones_b.ap, pv, start=first, stop=last), [ones_b, p], [Lb])
            if dense:
                if pi_ == 0:
                    op("dve", lambda e: e.tensor_copy(acc.ap, p.ap), [p], [acc])
                else:
                    op("dve", lambda e: e.tensor_tensor(acc.ap, acc.ap, p.ap, ALU.add), [p, acc], [acc])

        def finalize(h, qc, comp, Ob, Lb, acc):
            if dense:
                op("dve", lambda e: e.tensor_tensor(acc.ap[:, 0:512], acc.ap[:, 0:512], acc.ap[:, 512:1024], ALU.add), [acc], [acc])
                op("pe", lambda e: e.matmul(Lb.ap, ones_f.ap, acc.ap[:, 0:512], start=True, stop=True), [ones_f, acc], [Lb])
            if kind == "swa":
                op("dve", lambda e: e.tensor_scalar(rl.ap, Lb.ap, small.ap[:, 4 + h:5 + h], None, ALU.add), [Lb, small], [rl])
                op("dve", lambda e: e.reciprocal(rl.ap, rl.ap), [rl], [rl])
            else:
                op("dve", lambda e: e.reciprocal(rl.ap, Lb.ap), [Lb], [rl])
            o = ob[st["oi"] % 2]
            dst = MIXT.ap[mix0 + h, :, qc * 512:(qc + 1) * 512]
            if kind != "diff":
                op("dve", lambda e: e.tensor_tensor(o.ap, Ob.ap, rl.ap, ALU.mult), [Ob, rl], [o])
                dma("sp", dst, o.ap, [o], [MIXT])
            elif comp == 0:
                op("dve", lambda e: e.tensor_tensor(o1.ap, Ob.ap, rl.ap, ALU.mult), [Ob, rl], [o1])
            else:
                op("dve", lambda e: e.tensor_tensor(o2.ap, Ob.ap, rl.ap, ALU.mult), [Ob, rl], [o2])
                op("dve", lambda e: e.scalar_tensor_tensor(o1.ap, o2.ap, small.ap[:, 4:5], o1.ap, ALU.mult, ALU.add), [o2, small, o1], [o1])
                op("act", lambda e: e.activation(out=osq.ap, in_=o1.ap, func=AF.Square), [o1], [osq])
                Mb = ps[2 * (st["si"] % 2)]
                op("pe", lambda e: e.matmul(Mb.ap, ones_f.ap, osq.ap, start=True, stop=True), [ones_f, osq], [Mb])
                op("act", lambda e: e.activation(out=rl.ap, in_=Mb.ap, func=AF.Sqrt, scale=1.0 / 128, bias=eps_t.ap), [Mb, eps_t], [rl])
                op("dve", lambda e: e.reciprocal(rl.ap, rl.ap), [rl], [rl])
                op("dve", lambda e: e.scalar_tensor_tensor(o.ap, o1.ap, small.ap[:, 5:6], rl.ap, ALU.mult, ALU.mult), [o1, small, rl], [o])
                dma("sp", dst, o.ap, [o], [MIXT])

        FINLAG = 2
        steps = []
        for gi, (h, qc) in enumerate(groups):
            if kind == "na":
                klist = [((4 * qc - 2 + j) % 32, j) for j in range(8)]
            elif kind == "swa":
                klist = [((4 * qc - 1 + j) % 32, j) for j in range(6)]
            else:
                klist = [(kt, None) for kt in range(32)]
            pairs = [(klist[2 * i], klist[2 * i + 1]) for i in range(len(klist) // 2)]
            for comp in range(2 if kind == "diff" else 1):
                sub = dict(gi=gi, h=h, qc=qc, comp=comp, npairs=len(pairs), first_of_group=(comp == 0))
                for pi_, pair in enumerate(pairs):
                    steps.append((sub, pi_, pair))
        pend = []
        fins = []
        subidx = dict(n=0)

        def do_OL(item):
            sub, pi_, pair, p = item
            if pi_ == 0:
                sub["Ob"] = O_banks[subidx["n"] % 2]
                sub["Lb"] = L_banks[subidx["n"] % 2]
                sub["acc"] = accb[subidx["n"] % 2]
                subidx["n"] += 1
            h = sub["h"]
            if pi_ == 0 and sub["first_of_group"] and sub["qc"] == 0 and h + 1 < 4:
                load_head(h + 1)
            emit_OL(Vb[kvbuf(h)], sub["Ob"], sub["Lb"], pair, p, pi_, sub["npairs"], sub["acc"])
            if pi_ == sub["npairs"] - 1:
                fins.append([sub, FINLAG])

        def tick_fins(force=False):
            for f in list(fins):
                f[1] -= 1
                if f[1] <= 0 or force:
                    sub = f[0]
                    finalize(sub["h"], sub["qc"], sub["comp"], sub["Ob"], sub["Lb"], sub["acc"])
                    fins.remove(f)

        for (sub, pi_, pair) in steps:
            gi, h, qc, comp = sub["gi"], sub["h"], sub["qc"], sub["comp"]
            if pi_ == 0 and sub["first_of_group"]:
                if gi + 1 < len(groups):
                    load_group(gi + 1)
            kT = kTb[kvbuf(h)]
            q = qT[gi % 2]
            q2 = qT2[gi % 2] if kind == "mla" else None
            tb = tab[gi % 2] if kind in ("na", "swa") else None
            p = emit_S(kT, q, q2, tb, h, comp, pair)
            pend.append((sub, pi_, pair, p))
            if len(pend) > LA:
                do_OL(pend.pop(0))
                tick_fins()
        while pend:
            do_OL(pend.pop(0))
            tick_fins()
        while fins:
            tick_fins(force=True)

    def gates_for(lt, tt):
        L8, A8, B8 = lt.ap[:, 0:8], lt.ap[:, 8:16], lt.ap[:, 16:24]
        m1, m2, nm1, den = lt.ap[:, 24:25], lt.ap[:, 25:26], lt.ap[:, 26:27], lt.ap[:, 27:28]
        op("dve", lambda e: e.reduce_max(m1, L8, AX.X), [lt], [lt])
        op("dve", lambda e: e.tensor_scalar(A8, L8, m1, None, ALU.is_equal), [lt], [lt])
        op("dve", lambda e: e.scalar_tensor_tensor(A8, A8, -1e30, L8, ALU.mult, ALU.add), [lt], [lt])
        op("dve", lambda e: e.reduce_max(m2, A8, AX.X), [lt], [lt])
        op("dve", lambda e: e.tensor_scalar(B8, L8, m2, None, ALU.is_ge), [lt], [lt])
        op("dve", lambda e: e.tensor_scalar(nm1, m1, -1.0, None, ALU.mult), [lt], [lt])
        op("act", lambda e: e.activation(out=A8, in_=L8, func=AF.Exp, bias=nm1, scale=1.0), [lt], [lt])
        op("dve", lambda e: e.tensor_tensor(B8, B8, A8, ALU.mult), [lt], [lt])
        op("dve", lambda e: e.reduce_sum(den, B8, AX.X), [lt], [lt])
        op("dve", lambda e: e.reciprocal(den, den), [lt], [lt])
        op("dve", lambda e: e.tensor_scalar(B8, B8, den, None, ALU.mult), [lt], [lt])
        dma("sp", GATES.ap[tt * 128:(tt + 1) * 128, :], B8, [lt], [GATES])

    def phase_outproj(L):
        ntt = 32 if L == 0 else 16
        w = ar.alloc([128, 16, D], BF16, "op_w")
        for c4 in range(4):
            dma("pool", w.ap[:, c4 * 4:(c4 + 1) * 4, :], wout.ap[L, c4 * 512:(c4 + 1) * 512, :].rearrange("(c p) n -> p c n", p=128), [wout], [w])
        mx = [ar.alloc([128, 16, 128], BF16, "op_mx%d" % i) for i in range(2)]
        hb = [ar.alloc([128, D], F32, "op_h%d" % i) for i in range(3)]
        nctx = NormCtx()
        moe = (L == 1)
        if moe:
            rTg = ar.alloc([128, 8, D], F32, "op_rT")
            dma("sp", rTg.ap, routerT.ap.unsqueeze(0).to_broadcast([128, 8, D]), [routerT], [rTg])
            grow = ar.alloc([128, D], F32, "op_grow")
            dma("sp", grow.ap, ffng1.ap.to_broadcast([128, D]), [ffng1], [grow])
            for e_ in range(8):
                op("dve", lambda e, e_=e_: e.tensor_tensor(rTg.ap[:, e_, :], rTg.ap[:, e_, :], grow.ap, ALU.mult), [rTg, grow], [rTg])
            junk = ar.alloc([128, D], F32, "op_junk")
            lg = [ar.alloc([128, 32], F32, "op_lg%d" % i) for i in range(2)]
        def op_loads(tt):
            m, h = mx[tt % 2], hb[tt % 3]
            dma("sp", m.ap, MIXT.ap[:, :, tt * 128:(tt + 1) * 128].rearrange("c p t -> p c t"), [MIXT], [m])
            dma("sp", h.ap, Hres.ap[tt * 128:(tt + 1) * 128, :], [H_b[tt]], [h])

        op_loads(0)
        prev = None
        for tt in range(ntt):
            m, h = mx[tt % 2], hb[tt % 3]
            if tt + 1 < ntt:
                op_loads(tt + 1)
            for db in range(4):
                pb = ps[(tt * 4 + db) % 4]
                for c in range(16):
                    op("pe", lambda e, c=c, pb=pb, m=m, db=db: e.matmul(pb.ap, m.ap[:, c, :], w.ap[:, c, db * 512:(db + 1) * 512],
                                                                        start=(c == 0), stop=(c == 15)), [m, w], [pb])
                op("dve", lambda e, pb=pb, h=h, db=db: e.tensor_tensor(h.ap[:, db * 512:(db + 1) * 512], h.ap[:, db * 512:(db + 1) * 512], pb.ap, ALU.add),
                   [pb, h], [h])
            dma("sp", Hres.ap[tt * 128:(tt + 1) * 128, :], h.ap, [h], [H_b[tt]])
            ctx = nctx.stats(h)
            rstd = ctx["rstd"]
            if prev is not None:
                nctx.trans(prev[0], 3 * L + 1, prev[1], (ps[4 + (prev[1] % 2) * 2], ps[5 + (prev[1] % 2) * 2]))
            prev = (ctx, tt)
            if moe:
                lt = lg[tt % 2]
                for e_ in range(8):
                    op("dve", lambda e, e_=e_, lt=lt, h=h, rstd=rstd: e.scalar_tensor_tensor(
                        junk.ap, h.ap, rstd.ap, rTg.ap[:, e_, :], ALU.mult, ALU.mult, accum_out=lt.ap[:, e_:e_ + 1]),
                       [h, rstd, rTg], [junk, lt])
                gates_for(lt, tt)
        nctx.trans(prev[0], 3 * L + 1, prev[1], (ps[4 + (prev[1] % 2) * 2], ps[5 + (prev[1] % 2) * 2]))

    def ffn_prefix():
        wd0 = ar.alloc([128, NFT, 512], BF16, "f_wd0")
        hnc0 = ar.alloc([128, 16, 512], BF16, "f_hnc0")
        wgA = ar.alloc([128, 16, 256], BF16, "f_wgA")
        wuA = ar.alloc([128, 16, 256], BF16, "f_wuA")
        return wd0, hnc0, wgA, wuA

    def load_hn_chunk(dst, sci, q4):
        t0 = sci * 2048
        dma("sp", dst.ap, HNT.ap[:, :, t0 + q4 * 512:t0 + (q4 + 1) * 512].rearrange("c p t -> p c t"),
            [HNT_b[sci * 16 + q4 * 4 + j] for j in range(4)], [dst])

    def load_wd_block(w, wd_ap, wsrc, db):
        for q4 in range(4):
            dma("pool", w.ap[:, q4 * 14:(q4 + 1) * 14, :],
                wd_ap[q4 * 14 * 128:(q4 + 1) * 14 * 128, db * 512:(db + 1) * 512].rearrange("(c p) n -> p c n", p=128), [wsrc], [w])

    def load_wgu_block(wg_, wu_, wg_ap, wu_ap, wsrc, fb):
        dma("pool", wg_.ap, wg_ap[:, fb * 256:(fb + 1) * 256].rearrange("(c p) n -> p c n", p=128), [wsrc], [wg_])
        dma("pool", wu_.ap, wu_ap[:, fb * 256:(fb + 1) * 256].rearrange("(c p) n -> p c n", p=128), [wsrc], [wu_])

    def ffn_up(sci, wg_ap, wu_ap, wsrc, pre_loaded=False, next_down=None):
        wd0, hnc0, wgA, wuA = ffn_prefix()
        hnc = [hnc0] + [ar.alloc([128, 16, 512], BF16, "fu_hn%d" % i) for i in range(1, 4)]
        wgb = [wgA] + [ar.alloc([128, 16, 256], BF16, "fu_wg%d" % i) for i in range(1, 3)]
        wub = [wuA] + [ar.alloc([128, 16, 256], BF16, "fu_wu%d" % i) for i in range(1, 3)]
        sg = [ar.alloc([128, 512], F32, "fu_sg%d" % i) for i in range(2)]
        ab = [ar.alloc([128, 512], BF16, "fu_ab%d" % i) for i in range(4)]
        for q4 in range(4):
            if q4 == 0 and pre_loaded:
                continue
            load_hn_chunk(hnc[q4], sci, q4)
        k = 0
        nfb = NFT // 2
        for fb in range(nfb):
            wg_, wu_ = wgb[fb % 3], wub[fb % 3]
            if not (fb == 0 and pre_loaded):
                load_wgu_block(wg_, wu_, wg_ap, wu_ap, wsrc, fb)
            if fb == nfb - 1 and next_down is not None:
                load_wd_block(wd0, next_down[0], next_down[1], 0)
            for f2 in range(2):
                ft = fb * 2 + f2
                for ch in range(4):
                    pg, pu = ps[(2 * k) % 8], ps[(2 * k + 1) % 8]
                    s_, a_ = sg[k % 2], ab[k % 4]
                    hn = hnc[ch]
                    k += 1
                    for c in range(16):
                        op("pe", lambda e, c=c, pg=pg, wg_=wg_, f2=f2, hn=hn: e.matmul(
                            pg.ap, wg_.ap[:, c, f2 * 128:(f2 + 1) * 128], hn.ap[:, c, :], start=(c == 0), stop=(c == 15)),
                           [wg_, hn], [pg])
                    for c in range(16):
                        op("pe", lambda e, c=c, pu=pu, wu_=wu_, f2=f2, hn=hn: e.matmul(
                            pu.ap, wu_.ap[:, c, f2 * 128:(f2 + 1) * 128], hn.ap[:, c, :], start=(c == 0), stop=(c == 15)),
                           [wu_, hn], [pu])
                    op("act", lambda e, s_=s_, pg=pg: e.activation(out=s_.ap, in_=pg.ap, func=AF.Silu), [pg], [s_])
                    op("dve", lambda e, s_=s_, pu=pu, a_=a_: e.tensor_tensor(a_.ap, s_.ap, pu.ap, ALU.mult), [s_, pu], [a_])
                    dma("sp", ACTT.ap[ch * 4:(ch + 1) * 4, :, ft, :].rearrange("t p k -> p t k"),
                        a_.ap.rearrange("p (t k) -> p t k", t=4), [a_], [ACTT])

    def ffn_down(sci, wd_ap, wsrc, gate_e, pre_loaded=False, next_up=None):
        wd0, hnc0, wgA, wuA = ffn_prefix()
        wd = [wd0, ar.alloc([128, NFT, 512], BF16, "fd_w1")]
        at = [ar.alloc([128, NFT, 128], BF16, "fd_a%d" % i) for i in range(2)]
        hb = [ar.alloc([128, 512], F32, "fd_h%d" % i) for i in range(3)]
        gt = None
        if gate_e is not None:
            gt = ar.alloc([128, 16, 8], F32, "fd_g")
            dma("sp", gt.ap, GATES.ap.rearrange("(t p) e -> p t e", p=128), [GATES], [gt])

        its = [(db, tt) for db in range(4) for tt in range(16)]

        def loads(k):
            db, tt = its[k]
            gtt = sci * 16 + tt
            dma("sp", at[k % 2].ap, ACTT.ap[tt], [ACTT], [at[k % 2]])
            dma("sp", hb[k % 3].ap, Hres.ap[gtt * 128:(gtt + 1) * 128, db * 512:(db + 1) * 512], [H_b[gtt]], [hb[k % 3]])

        def compute(k):
            db, tt = its[k]
            gtt = sci * 16 + tt
            a_, h, pb, w = at[k % 2], hb[k % 3], ps[k % 8], wd[db % 2]
            for ft in range(NFT):
                op("pe", lambda e, ft=ft: e.matmul(pb.ap, a_.ap[:, ft, :], w.ap[:, ft, :], start=(ft == 0), stop=(ft == NFT - 1)),
                   [a_, w], [pb])
            if gate_e is None:
                op("dve", lambda e: e.tensor_tensor(h.ap, h.ap, pb.ap, ALU.add), [h, pb], [h])
            else:
                op("dve", lambda e: e.scalar_tensor_tensor(h.ap, pb.ap, gt.ap[:, tt, gate_e:gate_e + 1], h.ap, ALU.mult, ALU.add),
                   [h, pb, gt], [h])
            dma("sp", Hres.ap[gtt * 128:(gtt + 1) * 128, db * 512:(db + 1) * 512], h.ap, [h], [H_b[gtt]])

        if not pre_loaded:
            load_wd_block(wd[0], wd_ap, wsrc, 0)
        load_wd_block(wd[1], wd_ap, wsrc, 1)
        loads(0)
        for k, (db, tt) in enumerate(its):
            if k + 1 < len(its):
                loads(k + 1)
            compute(k)
            if tt == 15 and db + 2 < 4:
                load_wd_block(wd[db % 2], wd_ap, wsrc, db + 2)
                if db == 1 and next_up is not None:
                    sci2, wg2, wu2, wsrc2 = next_up
                    load_wgu_block(wgA, wuA, wg2, wu2, wsrc2, 0)
                    load_hn_chunk(hnc0, sci2, 0)

    def phase_ple(L):
        ntt = 32 if L == 0 else 16
        pg = ar.alloc([128, 16, D], BF16, "pl_pg")
        for c4 in range(4):
            dma("pool", pg.ap[:, c4 * 4:(c4 + 1) * 4, :], plegate.ap[L, c4 * 512:(c4 + 1) * 512, :].rearrange("(c p) n -> p c n", p=128), [plegate], [pg])
        pp = ar.alloc([128, 2, D], BF16, "pl_pp")
        dma("pool", pp.ap, pleproj.ap[L].rearrange("(c p) n -> p c n", p=128), [pleproj], [pp])
        nc1 = NormCtx()
        nc2 = NormCtx() if L == 0 else None
        hb = [ar.alloc([128, D], F32, "pl_h%d" % i) for i in range(3)]
        ptb = [ar.alloc([128, 2, 128], BF16, "pl_pt%d" % i) for i in range(3)]
        sg = [ar.alloc([128, 512], F32, "pl_sg%d" % i) for i in range(2)]
        if L == 1:
            gfin = ar.alloc([128, D], F32, "pl_gfin")
            dma("sp", gfin.ap, finalg.ap.to_broadcast([128, D]), [finalg], [gfin])
            ob = [ar.alloc([128, D], F32, "pl_ob%d" % i) for i in range(2)]
            ss = [ar.alloc([128, 2], F32, "pl_ss%d" % i) for i in range(2)]
            junk = ar.alloc([128, D], BF16, "pl_junk")
        k = 0
        def pl_loads(tt):
            h, pt = hb[tt % 3], ptb[tt % 3]
            dma("sp", h.ap, Hres.ap[tt * 128:(tt + 1) * 128, :], [H_b[tt]], [h])
            dma("pool", pt.ap, pT_in.ap[L, :, tt * 128:(tt + 1) * 128].rearrange("(c p) t -> p c t", p=128), [pT_in], [pt])

        pl_loads(0)
        pl_loads(1)
        hT_next = nc1.trans(nc1.stats(hb[0]), 3 * L + 2, 0, (ps[6], ps[7]), to_hnt=False)
        prev2 = None
        for tt in range(ntt):
            h, pt = hb[tt % 3], ptb[tt % 3]
            hT = hT_next
            if tt + 1 < ntt:
                hT_next = nc1.trans(nc1.stats(hb[(tt + 1) % 3]), 3 * L + 2, tt + 1, (ps[6], ps[7]), to_hnt=False)
            for db in range(4):
                pG, pE = ps[(2 * k) % 6], ps[(2 * k + 1) % 6]
                s_ = sg[k % 2]
                k += 1
                for c in range(16):
                    op("pe", lambda e, c=c, pG=pG, hT=hT, db=db: e.matmul(pG.ap, hT.ap[:, c, :], pg.ap[:, c, db * 512:(db + 1) * 512],
                                                                          start=(c == 0), stop=(c == 15)), [hT, pg], [pG])
                for c in range(2):
                    op("pe", lambda e, c=c, pE=pE, pt=pt, db=db: e.matmul(pE.ap, pt.ap[:, c, :], pp.ap[:, c, db * 512:(db + 1) * 512],
                                                                          start=(c == 0), stop=(c == 1)), [pt, pp], [pE])
                op("act", lambda e, s_=s_, pG=pG: e.activation(out=s_.ap, in_=pG.ap, func=AF.Sigmoid), [pG], [s_])
                op("dve", lambda e, s_=s_, pE=pE: e.tensor_tensor(s_.ap, s_.ap, pE.ap, ALU.mult), [s_, pE], [s_])
                op("dve", lambda e, s_=s_, h=h, db=db: e.tensor_tensor(h.ap[:, db * 512:(db + 1) * 512], h.ap[:, db * 512:(db + 1) * 512], s_.ap, ALU.add),
                   [s_, h], [h])
            if tt + 2 < ntt:
                pl_loads(tt + 2)
            if L == 0:
                dma("sp", Hres.ap[tt * 128:(tt + 1) * 128, :], h.ap, [h], [H_b[tt]])
                ctx2 = nc2.stats(h)
                if prev2 is not None:
                    nc2.trans(prev2[0], 3, prev2[1], (ps[6], ps[7]))
                prev2 = (ctx2, tt)
            else:
                s2, o = ss[tt % 2], ob[tt % 2]
                op("act", lambda e, s2=s2, h=h: e.activation(out=junk.ap, in_=h.ap, func=AF.Square, accum_out=s2.ap[:, 0:1]), [h], [junk, s2])
                op("act", lambda e, s2=s2: e.activation(out=s2.ap[:, 1:2], in_=s2.ap[:, 0:1], func=AF.Sqrt, scale=1.0 / D, bias=eps_t.ap), [s2, eps_t], [s2])
                op("dve", lambda e, s2=s2: e.reciprocal(s2.ap[:, 1:2], s2.ap[:, 1:2]), [s2], [s2])
                op("dve", lambda e, s2=s2, o=o, h=h: e.scalar_tensor_tensor(o.ap, h.ap, s2.ap[:, 1:2], gfin.ap, ALU.mult, ALU.mult), [h, s2, gfin], [o])
                dma("sp", out_ap.ap[tt * 128:(tt + 1) * 128, :], o.ap, [o], [out_ap])
        if prev2 is not None:
            nc2.trans(prev2[0], 3, prev2[1], (ps[6], ps[7]))

    def run_all():
        phase_prologue()
        if P.phase_end("prologue"):
            return
        for L in range(2):
            phase_inproj(L)
            if P.phase_end("inproj%d" % L):
                return
            phase_mla_prep(L)
            if P.phase_end("mlaprep%d" % L):
                return
            for kind in ("diff", "na", "swa", "mla"):
                attention(L, kind)
                if P.phase_end("%s%d" % (kind, L)):
                    return
            phase_outproj(L)
            if P.phase_end("outproj%d" % L):
                return
            if L == 0:
                for sci in range(2):
                    ffn_up(sci, dwg.ap, dwu.ap, dwg, pre_loaded=(sci == 1), next_down=(dwd.ap, dwd))
                    if P.phase_end("ffnup%d_%d" % (L, sci)):
                        return
                    ffn_down(sci, dwd.ap, dwd, None, pre_loaded=True,
                             next_up=((1, dwg.ap, dwu.ap, dwg) if sci == 0 else None))
                    if P.phase_end("ffndown%d_%d" % (L, sci)):
                        return
            else:
                for e_ in range(8):
                    ffn_up(0, mwg.ap[e_], mwu.ap[e_], mwg, pre_loaded=(e_ > 0), next_down=(mwd.ap[e_], mwd))
                    if P.phase_end("moeup%d" % e_):
                        return
                    ffn_down(0, mwd.ap[e_], mwd, e_, pre_loaded=True,
                             next_up=((0, mwg.ap[e_ + 1], mwu.ap[e_ + 1], mwg) if e_ < 7 else None))
                    if P.phase_end("moedown%d" % e_):
                        return
            phase_ple(L)
            if P.phase_end("ple%d" % L):
                return

    run_all()
    sc.barrier()
    sc.emit()
    return P


def _rope_tables(pos):
    pos = pos.astype(np.float32)
    f = np.arange(128)

    def tab(dim, fidx, sign):
        inv = (10000.0 ** (-np.arange(0, dim, 2, dtype=np.float32) / dim)).astype(np.float32)
        ang = pos[None, :] * inv[fidx][:, None]
        return np.stack([np.cos(ang), np.sin(ang) * sign[:, None]]).astype(np.float32)

    ropeD = tab(64, (f % 64) % 32, np.where((f % 64) < 32, -1.0, 1.0).astype(np.float32))
    ropeH = tab(128, f % 64, np.where(f < 64, -1.0, 1.0).astype(np.float32))
    return ropeD, ropeH


def _na_tables(rpb, half):
    out = np.full((8, 4, 8, 128, 512), NEG, dtype=np.float32)
    ki = np.arange(128)
    qi = np.arange(512)
    kc = ki % 64
    qr_l = qi // 64
    qcol = qi % 64
    ws = np.clip(qcol - 8, 0, 48)
    colok = (kc[:, None] >= ws[None, :]) & (kc[:, None] < ws[None, :] + 16)
    cidx = np.clip(kc[:, None] - qcol[None, :], -15, 15) + 15
    for qc in range(8):
        R0 = (8 * qc + 32 * half) % 64
        r = R0 + qr_l
        rs = np.clip(r - 4, 0, 56)
        for j in range(8):
            kr0 = R0 - 4 + 2 * j
            if kr0 < 0 or kr0 > 62:
                continue
            kr = kr0 + ki // 64
            rowok = (kr[:, None] >= rs[None, :]) & (kr[:, None] < rs[None, :] + 8)
            ridx = np.clip(kr[:, None] - r[None, :] + 7, 0, 14)
            ok = rowok & colok
            for h in range(4):
                out[qc, h, j] = np.where(ok, rpb[h][ridx, cidx], NEG)
    return out.astype(ml_dtypes.bfloat16)


def _swa_tables(half):
    out = np.full((8, 6, 128, 512), NEG, dtype=np.float32)
    ki = np.arange(128)
    qi = np.arange(512)
    for qc in range(8):
        Q0 = (512 * qc + 2048 * half) % 4096
        for j in range(6):
            k0 = Q0 - 128 + 128 * j
            if k0 < 0 or k0 >= 4096:
                continue
            ok = np.abs((Q0 + qi)[None, :] - (k0 + ki)[:, None]) <= 128
            out[qc, j] = np.where(ok, 0.0, NEG)
    return out.astype(ml_dtypes.bfloat16)


def _fm_cols(g):
    return np.ascontiguousarray(g.reshape(16, 128).T)


def make_in_maps(inputs, cores=range(8)):
    f32 = lambda a: np.ascontiguousarray(a, dtype=np.float32)
    I = inputs
    shared = {}
    shared["wext"] = f32(I["w_in"][:, :, WIN_COLS])
    shared["wout"] = f32(I["w_out"])
    shared["wuq"] = f32(I["mla_w_uq"][:, :, UQ_COLS])
    shared["wukv"] = f32(I["mla_w_ukv"][:, :, UKV_COLS])
    shared["dwg"] = f32(I["dense_w_gate"][0])
    shared["dwu"] = f32(I["dense_w_up"][0])
    shared["dwd"] = f32(I["dense_w_down"][0])
    shared["mwg"] = f32(I["moe_w_gate"][0])
    shared["mwu"] = f32(I["moe_w_up"][0])
    shared["mwd"] = f32(I["moe_w_down"][0])
    shared["routerT"] = f32(I["moe_router"][0].T)
    shared["plegate"] = f32(I["ple_gate"])
    shared["pleproj"] = f32(I["ple_proj"])
    gl = []
    for L in range(2):
        gl += [_fm_cols(I["attn_norm"][L]), _fm_cols(I["ffn_norm"][L]), _fm_cols(I["ple_norm"][L])]
    shared["gains"] = f32(np.stack(gl))
    shared["ffng1"] = f32(I["ffn_norm"][1][None, :])
    shared["finalg"] = f32(I["final_norm"][None, :])
    shared["mlan"] = f32(np.stack([np.concatenate([I["mla_q_norm"][L].reshape(4, 128).T, I["mla_kv_norm"][L].reshape(4, 128).T], axis=1)
                                   for L in range(2)]))
    shared["subln"] = f32(I["diff_subln"][:, :, None])
    shared["lamv"] = f32(np.stack([I["diff_lq1"], I["diff_lk1"], I["diff_lq2"], I["diff_lk2"]], axis=1))
    shared["sinks"] = f32(I["swa_sinks"])
    shared["ident"] = np.eye(128, dtype=np.float32).astype(ml_dtypes.bfloat16)
    per_half = {}
    for half in range(2):
        pos = (np.arange(S) + 2048 * half) % S
        ropeD, ropeH = _rope_tables(pos)
        per_half[half] = dict(
            ropeD=ropeD, ropeH=ropeH,
            natab=np.stack([_na_tables(np.asarray(I["na_rpb"][L], dtype=np.float32), half) for L in range(2)]),
            swatab=_swa_tables(half),
        )
    maps = []
    for c in cores:
        b, half = c // 2, c % 2
        m = dict(shared)
        m.update(per_half[half])
        m["x"] = f32(np.roll(I["x"][b], -2048 * half, axis=0))
        m["pT"] = f32(np.stack([np.roll(I["p"][L, b], -2048 * half, axis=0).T for L in range(2)]))
        maps.append(m)
    return maps


_PROG = None


def kernel(**inputs):
    global _PROG
    if _PROG is None:
        _PROG = build_program()
    P = _PROG
    maps = make_in_maps(inputs)
    maps = [{k: v for k, v in m.items() if k in P.inputs} for m in maps]
    res = run_bass_kernel_spmd(P.nc, maps, core_ids=list(range(8)))
    out = np.empty((4, S, D), dtype=np.float32)
    for c in range(8):
        b, half = c // 2, c % 2
        out[b, 2048 * half:2048 * (half + 1)] = np.asarray(res.results[c]["out"], dtype=np.float32)
    return out
```

```python
import math
from contextlib import ExitStack
import numpy as np
import ml_dtypes
import concourse.bass as bass
import concourse.mybir as mybir
from concourse.bass_utils import run_bass_kernel_spmd

F32 = mybir.dt.float32
BF16 = mybir.dt.bfloat16
U8 = mybir.dt.uint8
AF = mybir.ActivationFunctionType
ALU = mybir.AluOpType
AX = mybir.AxisListType

D = 2048
S = 4096
NC16 = 16
FFN = 7168
NFT = FFN // 128
EPS = 1e-6
NEG = -30000.0
SAME_ENGINE_SYNC = True
KSLOT = 8


class Buf:
    __slots__ = ("name", "w", "r")

    def __init__(self, name=""):
        self.name = name
        self.w = None
        self.r = {}


class T:
    __slots__ = ("ap", "buf")

    def __init__(self, ap, buf=None, name=""):
        self.ap = ap
        self.buf = buf if buf is not None else Buf(name)

    def __getitem__(self, k):
        return self.ap[k]


class LazyIn:
    def __init__(self, P, name, shape, dtype):
        self.P, self.name, self.shape, self.dtype = P, name, list(shape), dtype
        self._t = None

    def _get(self):
        if self._t is None:
            t = self.P.nc.dram_tensor(self.name, self.shape, self.dtype, kind="ExternalInput")
            self.P.inputs[self.name] = (tuple(self.shape), self.dtype)
            self._t = T(t.ap(), name=self.name)
        return self._t

    @property
    def ap(self):
        return self._get().ap

    @property
    def buf(self):
        return self._get().buf


def _bufs(xs):
    out = []
    for x in xs:
        if x is None:
            continue
        out.append(x.buf if isinstance(x, (T, LazyIn)) else x)
    return out


class Sched:
    def __init__(self, nc, es):
        self.nc = nc
        self.names = ["pe", "act", "dve", "pool", "sp"]
        self.ops = {e: [] for e in self.names}
        self.cnt = {e: 0 for e in self.names}
        self.seen = {e: {} for e in self.names}
        self.sem = {e: es.enter_context(nc.semaphore("c_" + e)) for e in self.names}
        self.dq = {}
        for q in ("sp", "pool", "act"):
            self.dq[q] = {"n": 0, "sems": [es.enter_context(nc.semaphore("d_%s%d" % (q, i))) for i in range(KSLOT)]}

    def _wait(self, E, tok):
        if tok[0] == "c":
            _, F, idx = tok
            if F == E and (E == "pe" or not SAME_ENGINE_SYNC):
                return
            key, val, sem = F, idx, self.sem[F]
        else:
            _, q, slot, c = tok
            key, val, sem = (q, slot), 16 * c, self.dq[q]["sems"][slot]
        if self.seen[E].get(key, 0) >= val:
            return
        self.seen[E][key] = val
        self.ops[E].append(lambda e, sem=sem, val=val: e.wait_ge(sem, val))

    @staticmethod
    def _key(tok):
        return tok[1] if tok[0] == "c" else (tok[1], tok[2])

    def _deps(self, reads, writes):
        toks = []
        for b in reads:
            if b.w is not None:
                toks.append(b.w)
        for b in writes:
            if b.w is not None:
                toks.append(b.w)
            toks.extend(b.r.values())
        return toks

    def _mark(self, tok, reads, writes):
        k = self._key(tok)
        for b in reads:
            b.r[k] = tok
        for b in writes:
            b.w = tok
            b.r = {}

    def op(self, E, fn, reads=(), writes=()):
        reads = _bufs(reads)
        writes = _bufs(writes)
        for t in self._deps(reads, writes):
            self._wait(E, t)
        self.cnt[E] += 1
        n = self.cnt[E]
        sem = self.sem[E]
        self.ops[E].append(lambda e, fn=fn, sem=sem: fn(e).then_inc(sem, 1))
        self._mark(("c", E, n), reads, writes)

    def dma(self, q, out, in_, reads=(), writes=()):
        reads = _bufs(reads)
        writes = _bufs(writes)
        dq = self.dq[q]
        n = dq["n"]
        dq["n"] += 1
        slot, c = n % KSLOT, n // KSLOT + 1
        toks = self._deps(reads, writes)
        if c > 1:
            toks.append(("d", q, slot, c - 1))
        for t in toks:
            self._wait(q, t)
        sem = dq["sems"][slot]
        self.ops[q].append(lambda e, out=out, in_=in_, sem=sem: e.dma_start(out=out, in_=in_).then_inc(sem, 16))
        self._mark(("d", q, slot, c), reads, writes)

    def barrier(self):
        toks = [("c", e, self.cnt[e]) for e in self.names if self.cnt[e] > 0]
        for q, dq in self.dq.items():
            n = dq["n"]
            for slot in range(KSLOT):
                c = (n - slot + KSLOT - 1) // KSLOT
                if c > 0:
                    toks.append(("d", q, slot, c))
        save = SAME_ENGINE_SYNC
        for E in self.names:
            for t in toks:
                if t[0] == "c" and t[1] == E:
                    continue
                self._wait(E, t)

    def emit(self):
        nc = self.nc
        with nc.Block() as block:
            @block.tensor
            def _(e):
                for f in self.ops["pe"]:
                    f(e)

            @block.scalar
            def _(e):
                for f in self.ops["act"]:
                    f(e)

            @block.vector
            def _(e):
                for f in self.ops["dve"]:
                    f(e)

            @block.gpsimd
            def _(e):
                for f in self.ops["pool"]:
                    f(e)

            @block.sync
            def _(e):
                for f in self.ops["sp"]:
                    f(e)


class Arena:
    def __init__(self, nc, nbytes):
        self.t = nc.alloc_sbuf_tensor("arena", [128, nbytes], U8)
        self.size = nbytes
        self.off = 0
        self.top = nbytes

    def reset(self):
        self.off = 0

    def alloc(self, shape, dtype, name="", persist=False):
        esz = 2 if dtype == BF16 else 4
        n = 1
        for s in shape[1:]:
            n *= s
        nb = (n * esz + 63) // 64 * 64
        if persist:
            self.top -= nb
            o = self.top
        else:
            o = self.off
            self.off += nb
        assert self.off <= self.top, "SBUF arena overflow %s %d %d" % (name, self.off, self.top)
        ap = self.t[:, o:o + n * esz].bitcast(dtype)
        if len(shape) == 3:
            ap = ap.rearrange("p (a b) -> p a b", a=shape[1])
        elif len(shape) == 4:
            ap = ap.rearrange("p (a b c) -> p a b c", a=shape[1], b=shape[2])
        if shape[0] < 128:
            ap = ap[0:shape[0]]
        return T(ap, name=name)


def _win_layout():
    cols = []
    tiles = []

    def add(name, src, rope=None, q_only=False, partner=None):
        tiles.append(dict(name=name, off=len(cols), rope=rope, q_only=q_only))
        cols.extend(src)
        if rope is not None:
            tiles.append(dict(name=name + "p", off=len(cols), rope="partner", q_only=q_only))
            cols.extend(partner)

    def swap64x2(base):
        return [base + blk * 64 + (i + 32) % 64 for blk in range(2) for i in range(64)]

    def swap128(base):
        return [base + (i + 64) % 128 for i in range(128)]

    for h in range(4):
        add("QD%d" % h, list(range(h * 128, h * 128 + 128)), "D", True, swap64x2(h * 128))
    for h in range(4):
        add("KD%d" % h, list(range(512 + h * 128, 512 + h * 128 + 128)), "D", False, swap64x2(512 + h * 128))
    for h in range(4):
        add("QS%d" % h, list(range(3072 + h * 128, 3072 + h * 128 + 128)), "H", True, swap128(3072 + h * 128))
    for g in range(2):
        add("KS%d" % g, list(range(3584 + g * 128, 3584 + g * 128 + 128)), "H", False, swap128(3584 + g * 128))
    kr = list(range(5120, 5184))
    krp = [5120 + (i + 32) % 64 for i in range(64)]
    add("KR", kr + kr, "D", False, krp + krp)
    assert len(tiles) % 2 == 0
    for h in range(4):
        add("QN%d" % h, list(range(1536 + h * 128, 1536 + h * 128 + 128)), None, True)
    for h in range(4):
        add("CQ%d" % h, list(range(4096 + h * 128, 4096 + h * 128 + 128)), None, True)
    for h in range(4):
        add("KN%d" % h, list(range(2048 + h * 128, 2048 + h * 128 + 128)), None, False)
    for h in range(4):
        add("CKV%d" % h, list(range(4608 + h * 128, 4608 + h * 128 + 128)), None, False)
    tm = []
    for name, lo, n in (("VD", 1024, 512), ("VN", 2560, 512), ("VS", 3840, 256)):
        tm.append(dict(name=name, off=len(cols), n=n))
        cols.extend(range(lo, lo + n))
    return np.array(cols, dtype=np.int64), tiles, tm


WIN_COLS, FM_TILES, TM_BLOCKS = _win_layout()
NWEXT = len(WIN_COLS)


def _uq_cols():
    nope = [192 * h + i for h in range(4) for i in range(128)]
    rope = [192 * h + 128 + i for h in range(4) for i in range(64)]
    ropep = [192 * h + 128 + (i + 32) % 64 for h in range(4) for i in range(64)]
    return np.array(nope + rope[0:128] + ropep[0:128] + rope[128:256] + ropep[128:256], dtype=np.int64)


def _ukv_cols():
    kn = [256 * h + i for h in range(4) for i in range(128)]
    v = [256 * h + 128 + i for h in range(4) for i in range(128)]
    return np.array(kn + v, dtype=np.int64)


UQ_COLS = _uq_cols()
UKV_COLS = _ukv_cols()


class Prog:
    def __init__(self, stop_after=None, dump=()):
        self.stop_after = stop_after
        self.dump = set(dump)
        self.es = ExitStack()
        nc = bass.Bass("TRN2", target_bir_lowering=False)
        self.nc = nc
        self.sc = Sched(nc, self.es)
        self.ar = Arena(nc, 206 * 1024)
        self.psbig = [nc.alloc_psum_tensor("psb%d" % i, [128, 1024], F32)[:] for i in range(4)]
        self.ps = [T(self.psbig[i // 2][:, (i % 2) * 512:(i % 2 + 1) * 512], name="ps%d" % i) for i in range(8)]
        self.inputs = {}
        self.scr = {}
        self.done = False

    def inp(self, name, shape, dtype=F32):
        return LazyIn(self, name, shape, dtype)

    def scratch(self, name, shape, dtype):
        kind = "ExternalOutput" if name in self.dump else "Internal"
        t = self.nc.dram_tensor(name, list(shape), dtype, kind=kind)
        r = T(t.ap(), name=name)
        self.scr[name] = r
        return r

    def phase_end(self, name):
        self.sc.barrier()
        self.ar.reset()
        if self.stop_after == name:
            self.done = True
        return self.done


def build_program(stop_after=None, dump=()):
    P = Prog(stop_after, dump)
    nc, sc, ar, ps = P.nc, P.sc, P.ar, P.ps
    op, dma = sc.op, sc.dma

    x_in = P.inp("x", [S, D])
    pT_in = P.inp("pT", [2, 256, S])
    wext = P.inp("wext", [2, D, NWEXT])
    wout = P.inp("wout", [2, D, D])
    wuq = P.inp("wuq", [2, 512, 1024])
    wukv = P.inp("wukv", [2, 512, 1024])
    dwg = P.inp("dwg", [D, FFN])
    dwu = P.inp("dwu", [D, FFN])
    dwd = P.inp("dwd", [FFN, D])
    mwg = P.inp("mwg", [8, D, FFN])
    mwu = P.inp("mwu", [8, D, FFN])
    mwd = P.inp("mwd", [8, FFN, D])
    routerT = P.inp("routerT", [8, D])
    plegate = P.inp("plegate", [2, D, D])
    pleproj = P.inp("pleproj", [2, 256, D])
    gains = P.inp("gains", [6, 128, 16])
    finalg = P.inp("finalg", [1, D])
    ffng1 = P.inp("ffng1", [1, D])
    mlan = P.inp("mlan", [2, 128, 8])
    subln = P.inp("subln", [2, 128, 1])
    lam_in = P.inp("lamv", [2, 4, 64])
    sinks = P.inp("sinks", [2, 4])
    natab = P.inp("natab", [2, 8, 4, 8, 128, 512], BF16)
    swatab = P.inp("swatab", [8, 6, 128, 512], BF16)
    ropeD = P.inp("ropeD", [2, 128, S])
    ropeH = P.inp("ropeH", [2, 128, S])
    ident_in = P.inp("ident", [128, 128], BF16)
    out_t = P.nc.dram_tensor("out", [2048, D], F32, kind="ExternalOutput")
    out_ap = T(out_t.ap(), name="out")

    Hres = P.scratch("Hres", [S, D], F32)
    HNT = P.scratch("HNT", [16, 128, S], BF16)
    fm_names = [t["name"] for t in FM_TILES if t["rope"] != "partner"]
    FMS = {n: P.scratch("z_" + n, [128, S], BF16) for n in fm_names}
    TMS = {b["name"]: P.scratch("z_" + b["name"], [S, b["n"]], BF16) for b in TM_BLOCKS}
    QMN = [P.scratch("QMN%d" % h, [128, S], BF16) for h in range(4)]
    QMR = [P.scratch("QMR%d" % h, [128, S], BF16) for h in range(2)]
    KMN = [P.scratch("KMN%d" % h, [128, S], BF16) for h in range(4)]
    VM = P.scratch("VM", [S, 512], BF16)
    MIXT = P.scratch("MIXT", [16, 128, S], BF16)
    ACTT = P.scratch("ACTT", [16, 128, NFT, 128], BF16)
    GATES = P.scratch("GATES", [2048, 8], F32)

    H_b = [Buf("H%d" % i) for i in range(32)]
    HNT_b = [Buf("HNT%d" % i) for i in range(32)]

    ident = ar.alloc([128, 128], BF16, "ident", persist=True)
    ones_f = ar.alloc([128, 128], F32, "ones_f", persist=True)
    ones_b = ar.alloc([128, 128], BF16, "ones_b", persist=True)
    gains_sb = ar.alloc([128, 6, 16], F32, "gains", persist=True)
    dma("sp", ident.ap, ident_in.ap, [ident_in], [ident])
    dma("sp", gains_sb.ap, gains.ap.rearrange("g p c -> p g c"), [gains], [gains_sb])
    op("dve", lambda e: e.memset(ones_f.ap, 1.0), [], [ones_f])
    op("dve", lambda e: e.memset(ones_b.ap, 1.0), [], [ones_b])
    eps_t = ar.alloc([128, 1], F32, "eps", persist=True)
    op("dve", lambda e: e.memset(eps_t.ap, EPS), [], [eps_t])

    def norm_transpose(h_sb, gidx, tt, pbank, extra_hn_f32=None):
        raise NotImplementedError

    class NormCtx:
        def __init__(self):
            self.junk = ar.alloc([128, D], BF16, "nt_junk")
            self.ss = [ar.alloc([128, 1], F32, "nt_ss%d" % i) for i in range(2)]
            self.rstd = [ar.alloc([128, 1], F32, "nt_rstd%d" % i) for i in range(2)]
            self.hs = [ar.alloc([128, D], BF16, "nt_hs%d" % i) for i in range(2)]
            self.hT = [ar.alloc([128, 16, 128], BF16, "nt_hT%d" % i) for i in range(2)]
            self.i = 0

        def stats(self, h_sb):
            k = self.i % 2
            self.i += 1
            ss, rstd, hs = self.ss[k], self.rstd[k], self.hs[k]
            op("act", lambda e: e.activation(out=self.junk.ap, in_=h_sb.ap, func=AF.Square, accum_out=ss.ap),
               [h_sb], [self.junk, ss])
            op("act", lambda e: e.activation(out=rstd.ap, in_=ss.ap, func=AF.Sqrt, scale=1.0 / D, bias=eps_t.ap), [ss, eps_t], [rstd])
            op("dve", lambda e: e.reciprocal(rstd.ap, rstd.ap), [rstd], [rstd])
            op("act", lambda e: e.activation(out=hs.ap, in_=h_sb.ap, func=AF.Copy, scale=rstd.ap), [h_sb, rstd], [hs])
            return dict(k=k, rstd=rstd, hs=hs)

        def trans(self, ctx, gidx, tt, pbanks, to_hnt=True):
            hs, hT = ctx["hs"], self.hT[ctx["k"]]
            for half in range(2):
                pb = pbanks[half]
                pv = pb.ap.bitcast(BF16).rearrange("p (a b) -> p a b", a=8)
                for c in range(8):
                    cc = half * 8 + c
                    op("pe", lambda e, c=c, cc=cc, pv=pv: e.transpose(pv[:, c, :], hs.ap[:, cc * 128:(cc + 1) * 128], ident.ap),
                       [hs, ident], [pb])
                g = gains_sb.ap[:, gidx, half * 8:(half + 1) * 8]
                gb = g.unsqueeze(2).to_broadcast([128, 8, 128])
                op("dve", lambda e, pv=pv, gb=gb, half=half: e.tensor_tensor(hT.ap[:, half * 8:(half + 1) * 8, :], pv, gb, ALU.mult),
                   [pb, gains_sb], [hT])
            if to_hnt:
                dma("sp", HNT.ap[:, :, tt * 128:(tt + 1) * 128].rearrange("c p t -> p c t"), hT.ap, [hT], [HNT_b[tt]])
            return hT

        def run(self, h_sb, gidx, tt, pbanks, hn_f32=None, to_hnt=True):
            ctx = self.stats(h_sb)
            hT = self.trans(ctx, gidx, tt, pbanks, to_hnt)
            return ctx["rstd"] if to_hnt else hT

    def phase_prologue():
        nctx = NormCtx()
        hb = [ar.alloc([128, D], F32, "pro_h%d" % i) for i in range(3)]
        prev = None
        for tt in range(32):
            h = hb[tt % 3]
            dma("sp", h.ap, x_in.ap[tt * 128:(tt + 1) * 128, :], [x_in], [h])
            dma("sp", Hres.ap[tt * 128:(tt + 1) * 128, :], h.ap, [h], [H_b[tt]])
            ctx = nctx.stats(h)
            if prev is not None:
                nctx.trans(prev[0], 0, prev[1], (ps[(2 * prev[1]) % 8], ps[(2 * prev[1] + 1) % 8]))
            prev = (ctx, tt)
        nctx.trans(prev[0], 0, prev[1], (ps[(2 * prev[1]) % 8], ps[(2 * prev[1] + 1) % 8]))

    def phase_inproj(L):
        nq_chunks = 8 if L == 0 else 4
        hn = [ar.alloc([128, 16, 1024], BF16, "ip_hn%d" % i) for i in range(2)]
        wu = [ar.alloc([128, 16, 256], BF16, "ip_w%d" % i) for i in range(3)]
        wb = [ar.alloc([128, 16, 512], BF16, "ip_wb%d" % i) for i in range(2)]
        rt = [ar.alloc([128, 4, 512], F32, "ip_rt%d" % i) for i in range(2)]
        t1 = [ar.alloc([128, 512], F32, "ip_t1%d" % i) for i in range(2)]
        t2 = [ar.alloc([128, 512], F32, "ip_t2%d" % i) for i in range(2)]
        ob = [ar.alloc([128, 512], BF16, "ip_ob%d" % i) for i in range(4)]
        units = [(FM_TILES[i], FM_TILES[i + 1]) for i in range(0, len(FM_TILES), 2)]
        wi = 0
        oi = 0
        pi = 0
        def load_hn(sc_j):
            dma("sp", hn[sc_j % 2].ap, HNT.ap[:, :, sc_j * 1024:(sc_j + 1) * 1024].rearrange("c p t -> p c t"),
                [HNT_b[sc_j * 8 + j] for j in range(8)], [hn[sc_j % 2]])

        load_hn(0)
        for sc_i in range(4):
            hnb = hn[sc_i % 2]
            t0 = sc_i * 1024
            for ch in range(2):
                tok0 = t0 + ch * 512
                dma("sp", rt[ch].ap[:, 0:2, :], ropeD.ap[:, :, tok0:tok0 + 512].rearrange("a p t -> p a t"), [ropeD], [rt[ch]])
                dma("sp", rt[ch].ap[:, 2:4, :], ropeH.ap[:, :, tok0:tok0 + 512].rearrange("a p t -> p a t"), [ropeH], [rt[ch]])
            if sc_i + 1 < 4:
                load_hn(sc_i + 1)
            for (ta, tb) in units:
                chunks = [ch for ch in range(2) if not (ta["q_only"] and (sc_i * 2 + ch) >= nq_chunks)]
                if not chunks:
                    continue
                w = wu[wi % 3]
                wi += 1
                dma("pool", w.ap, wext.ap[L, :, ta["off"]:ta["off"] + 256].rearrange("(c p) n -> p c n", p=128), [wext], [w])
                for ch in chunks:
                    tok0 = t0 + ch * 512
                    pa, pb = ps[pi % 8], ps[(pi + 1) % 8]
                    pi += 2
                    for c in range(16):
                        op("pe", lambda e, c=c, pa=pa, w=w, hnb=hnb, ch=ch: e.matmul(
                            pa.ap, w.ap[:, c, 0:128], hnb.ap[:, c, ch * 512:(ch + 1) * 512], start=(c == 0), stop=(c == 15)),
                           [w, hnb], [pa])
                    for c in range(16):
                        op("pe", lambda e, c=c, pb=pb, w=w, hnb=hnb, ch=ch: e.matmul(
                            pb.ap, w.ap[:, c, 128:256], hnb.ap[:, c, ch * 512:(ch + 1) * 512], start=(c == 0), stop=(c == 15)),
                           [w, hnb], [pb])
                    if ta["rope"] is not None:
                        r = rt[ch]
                        ro = 0 if ta["rope"] == "D" else 2
                        a1, a2 = t1[oi % 2], t2[oi % 2]
                        o = ob[oi % 4]
                        oi += 1
                        op("dve", lambda e, a1=a1, pa=pa, r=r, ro=ro: e.tensor_tensor(a1.ap, pa.ap, r.ap[:, ro, :], ALU.mult), [pa, r], [a1])
                        op("dve", lambda e, a2=a2, pb=pb, r=r, ro=ro: e.tensor_tensor(a2.ap, pb.ap, r.ap[:, ro + 1, :], ALU.mult), [pb, r], [a2])
                        op("dve", lambda e, a1=a1, a2=a2, o=o: e.tensor_tensor(o.ap, a1.ap, a2.ap, ALU.add), [a1, a2], [o])
                        dst = FMS[ta["name"]]
                        dma("sp", dst.ap[:, tok0:tok0 + 512], o.ap, [o], [dst])
                    else:
                        for (tl, pp) in ((ta, pa), (tb, pb)):
                            o = ob[oi % 4]
                            oi += 1
                            op("act", lambda e, o=o, pp=pp: e.activation(out=o.ap, in_=pp.ap, func=AF.Copy), [pp], [o])
                            dst = FMS[tl["name"]]
                            dma("sp", dst.ap[:, tok0:tok0 + 512], o.ap, [o], [dst])
            for bi, blk in enumerate(TM_BLOCKS):
                w = wb[bi % 2]
                n = blk["n"]
                dma("pool", w.ap[:, :, 0:n], wext.ap[L, :, blk["off"]:blk["off"] + n].rearrange("(c p) n -> p c n", p=128), [wext], [w])
                for tt in range(8):
                    pa = ps[pi % 8]
                    pi += 1
                    for c in range(16):
                        op("pe", lambda e, c=c, pa=pa, w=w, hnb=hnb, tt=tt, n=n: e.matmul(
                            pa.ap[:, 0:n], hnb.ap[:, c, tt * 128:(tt + 1) * 128], w.ap[:, c, 0:n], start=(c == 0), stop=(c == 15)),
                           [w, hnb], [pa])
                    o = ob[oi % 4]
                    oi += 1
                    op("act", lambda e, o=o, pa=pa, n=n: e.activation(out=o.ap[:, 0:n], in_=pa.ap[:, 0:n], func=AF.Copy), [pa], [o])
                    dst = TMS[blk["name"]]
                    dma("sp", dst.ap[t0 + tt * 128:t0 + (tt + 1) * 128, :], o.ap[:, 0:n], [o], [dst])

    def phase_mla_prep(L):
        nq_chunks = 8 if L == 0 else 4
        wq = ar.alloc([128, 4, 1024], BF16, "mp_wq")
        wk = ar.alloc([128, 4, 1024], BF16, "mp_wk")
        nrm = ar.alloc([128, 8], F32, "mp_nrm")
        dma("pool", wq.ap, wuq.ap[L].rearrange("(c p) n -> p c n", p=128), [wuq], [wq])
        dma("pool", wk.ap, wukv.ap[L].rearrange("(c p) n -> p c n", p=128), [wukv], [wk])
        dma("sp", nrm.ap, mlan.ap[L], [mlan], [nrm])
        cin = [ar.alloc([128, 4, 512], BF16, "mp_cin%d" % i) for i in range(2)]
        sq = [ar.alloc([128, 4, 512], F32, "mp_sq%d" % i) for i in range(2)]
        rstd = [ar.alloc([128, 512], F32, "mp_rstd%d" % i) for i in range(2)]
        cn = [ar.alloc([128, 4, 512], BF16, "mp_cn%d" % i) for i in range(2)]
        rt = [ar.alloc([128, 2, 512], F32, "mp_rt%d" % i) for i in range(2)]
        t1 = [ar.alloc([128, 512], F32, "mp_t1%d" % i) for i in range(2)]
        t2 = [ar.alloc([128, 512], F32, "mp_t2%d" % i) for i in range(2)]
        ob = [ar.alloc([128, 512], BF16, "mp_ob%d" % i) for i in range(4)]
        st = dict(i=0, oi=0, pi=0)

        def normed(src_names, noff, tok0):
            k = st["i"] % 2
            st["i"] += 1
            ci, s2, rs, cno = cin[k], sq[k], rstd[k], cn[k]
            for t in range(4):
                src = FMS[src_names[t]]
                dma("sp", ci.ap[:, t, :], src.ap[:, tok0:tok0 + 512], [src], [ci])
            op("act", lambda e: e.activation(out=s2.ap, in_=ci.ap, func=AF.Square), [ci], [s2])
            pm = ps[st["pi"] % 8]
            st["pi"] += 1
            for t in range(4):
                op("pe", lambda e, t=t: e.matmul(pm.ap, ones_f.ap, s2.ap[:, t, :], start=(t == 0), stop=(t == 3)), [ones_f, s2], [pm])
            op("act", lambda e: e.activation(out=rs.ap, in_=pm.ap, func=AF.Sqrt, scale=1.0 / 512, bias=eps_t.ap), [pm, eps_t], [rs])
            op("dve", lambda e: e.reciprocal(rs.ap, rs.ap), [rs], [rs])
            for t in range(4):
                op("dve", lambda e, t=t: e.scalar_tensor_tensor(cno.ap[:, t, :], ci.ap[:, t, :], nrm.ap[:, noff + t:noff + t + 1],
                                                                  rs.ap, ALU.mult, ALU.mult), [ci, nrm, rs], [cno])
            return cno

        def evac_plain(pp, dst, tok0, n=512):
            o = ob[st["oi"] % 4]
            st["oi"] += 1
            op("act", lambda e: e.activation(out=o.ap[:, 0:n], in_=pp.ap[:, 0:n], func=AF.Copy), [pp], [o])
            return o

        for ch in range(8):
            tok0 = ch * 512
            cno = normed(["CKV%d" % t for t in range(4)], 4, tok0)
            for h in range(4):
                pp = ps[st["pi"] % 8]
                st["pi"] += 1
                for c in range(4):
                    op("pe", lambda e, c=c, h=h, pp=pp, cno=cno: e.matmul(pp.ap, wk.ap[:, c, h * 128:(h + 1) * 128], cno.ap[:, c, :],
                                                                start=(c == 0), stop=(c == 3)), [wk, cno], [pp])
                o = evac_plain(pp, None, tok0)
                dma("sp", KMN[h].ap[:, tok0:tok0 + 512], o.ap, [o], [KMN[h]])
            for tt in range(4):
                pp = ps[st["pi"] % 8]
                st["pi"] += 1
                for c in range(4):
                    op("pe", lambda e, c=c, tt=tt, pp=pp, cno=cno: e.matmul(pp.ap, cno.ap[:, c, tt * 128:(tt + 1) * 128], wk.ap[:, c, 512:1024],
                                                                 start=(c == 0), stop=(c == 3)), [wk, cno], [pp])
                o = evac_plain(pp, None, tok0)
                dma("sp", VM.ap[tok0 + tt * 128:tok0 + (tt + 1) * 128, :], o.ap, [o], [VM])
            if ch >= nq_chunks:
                continue
            cno = normed(["CQ%d" % t for t in range(4)], 0, tok0)
            for h in range(4):
                pp = ps[st["pi"] % 8]
                st["pi"] += 1
                for c in range(4):
                    op("pe", lambda e, c=c, h=h, pp=pp, cno=cno: e.matmul(pp.ap, wq.ap[:, c, h * 128:(h + 1) * 128], cno.ap[:, c, :],
                                                                start=(c == 0), stop=(c == 3)), [wq, cno], [pp])
                o = evac_plain(pp, None, tok0)
                dma("sp", QMN[h].ap[:, tok0:tok0 + 512], o.ap, [o], [QMN[h]])
            r = rt[ch % 2]
            dma("sp", r.ap, ropeD.ap[:, :, tok0:tok0 + 512].rearrange("a p t -> p a t"), [ropeD], [r])
            for pr in range(2):
                pa, pb = ps[st["pi"] % 8], ps[(st["pi"] + 1) % 8]
                st["pi"] += 2
                base = 512 + pr * 256
                for c in range(4):
                    op("pe", lambda e, c=c, pa=pa, base=base, cno=cno: e.matmul(pa.ap, wq.ap[:, c, base:base + 128], cno.ap[:, c, :],
                                                                       start=(c == 0), stop=(c == 3)), [wq, cno], [pa])
                for c in range(4):
                    op("pe", lambda e, c=c, pb=pb, base=base, cno=cno: e.matmul(pb.ap, wq.ap[:, c, base + 128:base + 256], cno.ap[:, c, :],
                                                                       start=(c == 0), stop=(c == 3)), [wq, cno], [pb])
                a1, a2 = t1[pr], t2[pr]
                o = ob[st["oi"] % 4]
                st["oi"] += 1
                op("dve", lambda e, a1=a1, pa=pa, r=r: e.tensor_tensor(a1.ap, pa.ap, r.ap[:, 0, :], ALU.mult), [pa, r], [a1])
                op("dve", lambda e, a2=a2, pb=pb, r=r: e.tensor_tensor(a2.ap, pb.ap, r.ap[:, 1, :], ALU.mult), [pb, r], [a2])
                op("dve", lambda e, a1=a1, a2=a2, o=o: e.tensor_tensor(o.ap, a1.ap, a2.ap, ALU.add), [a1, a2], [o])
                dma("sp", QMR[pr].ap[:, tok0:tok0 + 512], o.ap, [o], [QMR[pr]])

    def attention(L, kind):
        LA = 1
        nq_chunks = 8 if L == 0 else 4
        lam_init = 0.8 - 0.6 * math.exp(-0.3 * L)
        if kind == "mla":
            wpre = ar.alloc([128, 16, D], BF16, "at_wpre")
            for c4 in range(4):
                dma("pool", wpre.ap[:, c4 * 4:(c4 + 1) * 4, :],
                    wout.ap[L, c4 * 512:(c4 + 1) * 512, :].rearrange("(c p) n -> p c n", p=128), [wout], [wpre])
        kTb = [ar.alloc([128, S], BF16, "at_kT%d" % i) for i in range(2)]
        kT2 = ar.alloc([128, S], BF16, "at_kT2") if kind == "mla" else None
        Vb = [ar.alloc([128, 32, 128], BF16, "at_V%d" % i) for i in range(2)]
        qT = [ar.alloc([128, 512], BF16, "at_qT%d" % i) for i in range(2)]
        qT2 = [ar.alloc([128, 512], BF16, "at_qT2%d" % i) for i in range(2)] if kind == "mla" else None
        pT = [ar.alloc([128, 1024], BF16, "at_pT%d" % i) for i in range(3)]
        sb = [ar.alloc([128, 1024], F32, "at_sb%d" % i) for i in range(2)] if kind in ("na", "swa") else None
        accb = [ar.alloc([128, 1024], F32, "at_acc%d" % i) for i in range(2)]
        tab = [ar.alloc([128, 8, 512], BF16, "at_tab%d" % i) for i in range(2)] if kind in ("na", "swa") else None
        rl = ar.alloc([128, 512], F32, "at_rl")
        o1 = ar.alloc([128, 512], F32, "at_o1")
        o2 = ar.alloc([128, 512], F32, "at_o2")
        osq = ar.alloc([128, 512], F32, "at_osq")
        ob = [ar.alloc([128, 512], BF16, "at_ob%d" % i) for i in range(2)]
        small = ar.alloc([128, 16], F32, "at_small")
        lamt = ar.alloc([128, 4, 64], F32, "at_lamt")
        junk = ar.alloc([128, 64], F32, "at_junk")
        S_banks = ps[0:4]
        O_banks = ps[4:6]
        L_banks = ps[6:8]
        mix0 = dict(diff=0, na=4, swa=8, mla=12)[kind]
        scale = dict(diff=64 ** -0.5, na=128 ** -0.5, swa=128 ** -0.5, mla=192 ** -0.5)[kind]
        if kind == "diff":
            dma("sp", lamt.ap, lam_in.ap[L].unsqueeze(0).to_broadcast([128, 4, 64]), [lam_in], [lamt])
            op("dve", lambda e: e.tensor_tensor(junk.ap, lamt.ap[:, 0, :], lamt.ap[:, 1, :], ALU.mult), [lamt], [junk])
            op("dve", lambda e: e.reduce_sum(small.ap[:, 0:1], junk.ap, AX.X), [junk], [small])
            op("dve", lambda e: e.tensor_tensor(junk.ap, lamt.ap[:, 2, :], lamt.ap[:, 3, :], ALU.mult), [lamt, small], [junk])
            op("dve", lambda e: e.reduce_sum(small.ap[:, 1:2], junk.ap, AX.X), [junk], [small])
            op("act", lambda e: e.activation(out=small.ap[:, 2:4], in_=small.ap[:, 0:2], func=AF.Exp), [small], [small])
            op("dve", lambda e: e.tensor_tensor(small.ap[:, 4:5], small.ap[:, 3:4], small.ap[:, 2:3], ALU.subtract), [small], [small])
            op("dve", lambda e: e.tensor_scalar(small.ap[:, 4:5], small.ap[:, 4:5], -lam_init, None, ALU.add), [small], [small])
            dma("sp", small.ap[:, 5:6], subln.ap[L], [subln], [small])
            op("dve", lambda e: e.tensor_scalar(small.ap[:, 5:6], small.ap[:, 5:6], 1.0 - lam_init, None, ALU.mult), [small], [small])
        if kind == "swa":
            dma("sp", small.ap[:, 0:4], sinks.ap[L:L + 1, :].to_broadcast([128, 4]), [sinks], [small])
            op("act", lambda e: e.activation(out=small.ap[:, 4:8], in_=small.ap[:, 0:4], func=AF.Exp), [small], [small])
        st = dict(si=0, pi=0, oi=0, sbi=0)

        def kvbuf(h):
            return (h // 2) % 2 if kind == "swa" else h % 2

        def load_head(h):
            if kind == "swa" and h % 2 == 1:
                return
            if kind == "diff":
                ksrc, vsrc, vcol = FMS["KD%d" % h], TMS["VD"], h * 128
            elif kind == "na":
                ksrc, vsrc, vcol = FMS["KN%d" % h], TMS["VN"], h * 128
            elif kind == "swa":
                ksrc, vsrc, vcol = FMS["KS%d" % (h // 2)], TMS["VS"], (h // 2) * 128
            else:
                ksrc, vsrc, vcol = KMN[h], VM, h * 128
            kT, V = kTb[kvbuf(h)], Vb[kvbuf(h)]
            dma("sp", kT.ap, ksrc.ap, [ksrc], [kT])
            dma("sp", V.ap, vsrc.ap[:, vcol:vcol + 128].rearrange("(t p) d -> p t d", p=128), [vsrc], [V])

        groups = [(h, qc) for h in range(4) for qc in range(nq_chunks)]

        def load_group(gi):
            h, qc = groups[gi]
            qsrc = dict(diff=lambda: FMS["QD%d" % h], na=lambda: FMS["QN%d" % h], swa=lambda: FMS["QS%d" % h], mla=lambda: QMN[h])[kind]()
            q = qT[gi % 2]
            dma("sp", q.ap, qsrc.ap[:, qc * 512:(qc + 1) * 512], [qsrc], [q])
            if kind == "mla":
                q2 = qT2[gi % 2]
                dma("sp", q2.ap, QMR[h // 2].ap[:, qc * 512:(qc + 1) * 512], [QMR[h // 2]], [q2])
            if kind == "na":
                tb = tab[gi % 2]
                dma("sp", tb.ap, natab.ap[L, qc, h].rearrange("j p q -> p j q"), [natab], [tb])
            elif kind == "swa":
                tb = tab[gi % 2]
                dma("sp", tb.ap[:, 0:6, :], swatab.ap[qc].rearrange("j p q -> p j q"), [swatab], [tb])

        if kind == "mla":
            dma("sp", kT2.ap, FMS["KR"].ap, [FMS["KR"]], [kT2])
        load_head(0)
        load_group(0)

        dense = kind in ("diff", "mla")

        def emit_S(kT, q, q2, tb, h, comp, pair):
            pp = st["si"] % 2
            st["si"] += 1
            banks = (ps[2 * pp], ps[2 * pp + 1])
            big = P.psbig[pp]
            for (kt, j), Sb in zip(pair, banks):
                ks = slice(kt * 128, (kt + 1) * 128)
                if kind == "diff":
                    pr = slice(comp * 64, comp * 64 + 64)
                    op("pe", lambda e, Sb=Sb, ks=ks, pr=pr: e.matmul(Sb.ap, kT.ap[pr, ks], q.ap[pr, :], start=True, stop=True), [kT, q], [Sb])
                elif kind == "mla":
                    pr = slice((h % 2) * 64, (h % 2) * 64 + 64)
                    op("pe", lambda e, Sb=Sb, ks=ks: e.matmul(Sb.ap, kT.ap[:, ks], q.ap, start=True, stop=False), [kT, q], [Sb])
                    op("pe", lambda e, Sb=Sb, ks=ks, pr=pr: e.matmul(Sb.ap, kT2.ap[pr, ks], q2.ap[pr, :], start=False, stop=True), [kT2, q2], [Sb])
                else:
                    op("pe", lambda e, Sb=Sb, ks=ks: e.matmul(Sb.ap, kT.ap[:, ks], q.ap, start=True, stop=True), [kT, q], [Sb])
            p = pT[st["pi"] % 3]
            st["pi"] += 1
            j0 = pair[0][1]
            if j0 is None:
                op("act", lambda e: e.activation(out=p.ap, in_=big, func=AF.Exp, scale=scale), list(banks), [p])
            else:
                s_ = sb[st["sbi"] % 2]
                st["sbi"] += 1
                tbv = tb.ap[:, j0:j0 + 2, :].rearrange("p a b -> p (a b)")
                op("dve", lambda e: e.scalar_tensor_tensor(s_.ap, big, scale, tbv, ALU.mult, ALU.add), list(banks) + [tb], [s_])
                op("act", lambda e: e.activation(out=p.ap, in_=s_.ap, func=AF.Exp), [s_], [p])
            return p

        def emit_OL(V, Ob, Lb, pair, p, pi_, npairs, acc):
            for t, (kt, j) in enumerate(pair):
                first = (pi_ == 0 and t == 0)
                last = (pi_ == npairs - 1 and t == 1)
                pv = p.ap[:, t * 512:(t + 1) * 512]
                op("pe", lambda e, kt=kt, pv=pv, first=first, last=last: e.matmul(Ob.ap, V.ap[:, kt, :], pv, start=first, stop=last), [V, p], [Ob])
                if not dense:
                    op("pe", lambda e, pv=pv, first=first, last=last: e.matmul(Lb.ap, ones_b.ap, pv, start=first, stop=last), [ones_b, p], [Lb])
            if dense:
                if pi_ == 0:
                    op("dve", lambda e: e.tensor_copy(acc.ap, p.ap), [p], [acc])
                else:
                    op("dve", lambda e: e.tensor_tensor(acc.ap, acc.ap, p.ap, ALU.add), [p, acc], [acc])

        def finalize(h, qc, comp, Ob, Lb, acc):
            if dense:
                op("dve", lambda e: e.tensor_tensor(acc.ap[:, 0:512], acc.ap[:, 0:512], acc.ap[:, 512:1024], ALU.add), [acc], [acc])
                op("pe", lambda e: e.matmul(Lb.ap, ones_f.ap, acc.ap[:, 0:512], start=True, stop=True), [ones_f, acc], [Lb])
            if kind == "swa":
                op("dve", lambda e: e.tensor_scalar(rl.ap, Lb.ap, small.ap[:, 4 + h:5 + h], None, ALU.add), [Lb, small], [rl])
                op("dve", lambda e: e.reciprocal(rl.ap, rl.ap), [rl], [rl])
            else:
                op("dve", lambda e: e.reciprocal(rl.ap, Lb.ap), [Lb], [rl])
            o = ob[st["oi"] % 2]
            dst = MIXT.ap[mix0 + h, :, qc * 512:(qc + 1) * 512]
            if kind != "diff":
                op("dve", lambda e: e.tensor_tensor(o.ap, Ob.ap, rl.ap, ALU.mult), [Ob, rl], [o])
                dma("sp", dst, o.ap, [o], [MIXT])
            elif comp == 0:
                op("dve", lambda e: e.tensor_tensor(o1.ap, Ob.ap, rl.ap, ALU.mult), [Ob, rl], [o1])
            else:
                op("dve", lambda e: e.tensor_tensor(o2.ap, Ob.ap, rl.ap, ALU.mult), [Ob, rl], [o2])
                op("dve", lambda e: e.scalar_tensor_tensor(o1.ap, o2.ap, small.ap[:, 4:5], o1.ap, ALU.mult, ALU.add), [o2, small, o1], [o1])
                op("act", lambda e: e.activation(out=osq.ap, in_=o1.ap, func=AF.Square), [o1], [osq])
                Mb = ps[2 * (st["si"] % 2)]
                op("pe", lambda e: e.matmul(Mb.ap, ones_f.ap, osq.ap, start=True, stop=True), [ones_f, osq], [Mb])
                op("act", lambda e: e.activation(out=rl.ap, in_=Mb.ap, func=AF.Sqrt, scale=1.0 / 128, bias=eps_t.ap), [Mb, eps_t], [rl])
                op("dve", lambda e: e.reciprocal(rl.ap, rl.ap), [rl], [rl])
                op("dve", lambda e: e.scalar_tensor_tensor(o.ap, o1.ap, small.ap[:, 5:6], rl.ap, ALU.mult, ALU.mult), [o1, small, rl], [o])
                dma("sp", dst, o.ap, [o], [MIXT])

        FINLAG = 2
        steps = []
        for gi, (h, qc) in enumerate(groups):
            if kind == "na":
                klist = [((4 * qc - 2 + j) % 32, j) for j in range(8)]
            elif kind == "swa":
                klist = [((4 * qc - 1 + j) % 32, j) for j in range(6)]
            else:
                klist = [(kt, None) for kt in range(32)]
            pairs = [(klist[2 * i], klist[2 * i + 1]) for i in range(len(klist) // 2)]
            for comp in range(2 if kind == "diff" else 1):
                sub = dict(gi=gi, h=h, qc=qc, comp=comp, npairs=len(pairs), first_of_group=(comp == 0))
                for pi_, pair in enumerate(pairs):
                    steps.append((sub, pi_, pair))
        pend = []
        fins = []
        subidx = dict(n=0)

        def do_OL(item):
            sub, pi_, pair, p = item
            if pi_ == 0:
                sub["Ob"] = O_banks[subidx["n"] % 2]
                sub["Lb"] = L_banks[subidx["n"] % 2]
                sub["acc"] = accb[subidx["n"] % 2]
                subidx["n"] += 1
            h = sub["h"]
            if pi_ == 0 and sub["first_of_group"] and sub["qc"] == 0 and h + 1 < 4:
                load_head(h + 1)
            emit_OL(Vb[kvbuf(h)], sub["Ob"], sub["Lb"], pair, p, pi_, sub["npairs"], sub["acc"])
            if pi_ == sub["npairs"] - 1:
                fins.append([sub, FINLAG])

        def tick_fins(force=False):
            for f in list(fins):
                f[1] -= 1
                if f[1] <= 0 or force:
                    sub = f[0]
                    finalize(sub["h"], sub["qc"], sub["comp"], sub["Ob"], sub["Lb"], sub["acc"])
                    fins.remove(f)

        for (sub, pi_, pair) in steps:
            gi, h, qc, comp = sub["gi"], sub["h"], sub["qc"], sub["comp"]
            if pi_ == 0 and sub["first_of_group"]:
                if gi + 1 < len(groups):
                    load_group(gi + 1)
            kT = kTb[kvbuf(h)]
            q = qT[gi % 2]
            q2 = qT2[gi % 2] if kind == "mla" else None
            tb = tab[gi % 2] if kind in ("na", "swa") else None
            p = emit_S(kT, q, q2, tb, h, comp, pair)
            pend.append((sub, pi_, pair, p))
            if len(pend) > LA:
                do_OL(pend.pop(0))
                tick_fins()
        while pend:
            do_OL(pend.pop(0))
            tick_fins()
        while fins:
            tick_fins(force=True)

    def gates_for(lt, tt):
        L8, A8, B8 = lt.ap[:, 0:8], lt.ap[:, 8:16], lt.ap[:, 16:24]
        m1, m2, nm1, den = lt.ap[:, 24:25], lt.ap[:, 25:26], lt.ap[:, 26:27], lt.ap[:, 27:28]
        op("dve", lambda e: e.reduce_max(m1, L8, AX.X), [lt], [lt])
        op("dve", lambda e: e.tensor_scalar(A8, L8, m1, None, ALU.is_equal), [lt], [lt])
        op("dve", lambda e: e.scalar_tensor_tensor(A8, A8, -1e30, L8, ALU.mult, ALU.add), [lt], [lt])
        op("dve", lambda e: e.reduce_max(m2, A8, AX.X), [lt], [lt])
        op("dve", lambda e: e.tensor_scalar(B8, L8, m2, None, ALU.is_ge), [lt], [lt])
        op("dve", lambda e: e.tensor_scalar(nm1, m1, -1.0, None, ALU.mult), [lt], [lt])
        op("act", lambda e: e.activation(out=A8, in_=L8, func=AF.Exp, bias=nm1, scale=1.0), [lt], [lt])
        op("dve", lambda e: e.tensor_tensor(B8, B8, A8, ALU.mult), [lt], [lt])
        op("dve", lambda e: e.reduce_sum(den, B8, AX.X), [lt], [lt])
        op("dve", lambda e: e.reciprocal(den, den), [lt], [lt])
        op("dve", lambda e: e.tensor_scalar(B8, B8, den, None, ALU.mult), [lt], [lt])
        dma("sp", GATES.ap[tt * 128:(tt + 1) * 128, :], B8, [lt], [GATES])

    def phase_outproj(L):
        ntt = 32 if L == 0 else 16
        w = ar.alloc([128, 16, D], BF16, "op_w")
        mx = [ar.alloc([128, 16, 128], BF16, "op_mx%d" % i) for i in range(2)]
        hb = [ar.alloc([128, D], F32, "op_h%d" % i) for i in range(3)]
        nctx = NormCtx()
        moe = (L == 1)
        if moe:
            rTg = ar.alloc([128, 8, D], F32, "op_rT")
            dma("sp", rTg.ap, routerT.ap.unsqueeze(0).to_broadcast([128, 8, D]), [routerT], [rTg])
            grow = ar.alloc([128, D], F32, "op_grow")
            dma("sp", grow.ap, ffng1.ap.to_broadcast([128, D]), [ffng1], [grow])
            for e_ in range(8):
                op("dve", lambda e, e_=e_: e.tensor_tensor(rTg.ap[:, e_, :], rTg.ap[:, e_, :], grow.ap, ALU.mult), [rTg, grow], [rTg])
            junk = ar.alloc([128, D], F32, "op_junk")
            lg = [ar.alloc([128, 32], F32, "op_lg%d" % i) for i in range(2)]
        def op_loads(tt):
            m, h = mx[tt % 2], hb[tt % 3]
            dma("sp", m.ap, MIXT.ap[:, :, tt * 128:(tt + 1) * 128].rearrange("c p t -> p c t"), [MIXT], [m])
            dma("sp", h.ap, Hres.ap[tt * 128:(tt + 1) * 128, :], [H_b[tt]], [h])

        op_loads(0)
        prev = None
        for tt in range(ntt):
            m, h = mx[tt % 2], hb[tt % 3]
            if tt + 1 < ntt:
                op_loads(tt + 1)
            for db in range(4):
                pb = ps[(tt * 4 + db) % 4]
                for c in range(16):
                    op("pe", lambda e, c=c, pb=pb, m=m, db=db: e.matmul(pb.ap, m.ap[:, c, :], w.ap[:, c, db * 512:(db + 1) * 512],
                                                                        start=(c == 0), stop=(c == 15)), [m, w], [pb])
                op("dve", lambda e, pb=pb, h=h, db=db: e.tensor_tensor(h.ap[:, db * 512:(db + 1) * 512], h.ap[:, db * 512:(db + 1) * 512], pb.ap, ALU.add),
                   [pb, h], [h])
            dma("sp", Hres.ap[tt * 128:(tt + 1) * 128, :], h.ap, [h], [H_b[tt]])
            ctx = nctx.stats(h)
            rstd = ctx["rstd"]
            if prev is not None:
                nctx.trans(prev[0], 3 * L + 1, prev[1], (ps[4 + (prev[1] % 2) * 2], ps[5 + (prev[1] % 2) * 2]))
            prev = (ctx, tt)
            if moe:
                lt = lg[tt % 2]
                for e_ in range(8):
                    op("dve", lambda e, e_=e_, lt=lt, h=h, rstd=rstd: e.scalar_tensor_tensor(
                        junk.ap, h.ap, rstd.ap, rTg.ap[:, e_, :], ALU.mult, ALU.mult, accum_out=lt.ap[:, e_:e_ + 1]),
                       [h, rstd, rTg], [junk, lt])
                gates_for(lt, tt)
        nctx.trans(prev[0], 3 * L + 1, prev[1], (ps[4 + (prev[1] % 2) * 2], ps[5 + (prev[1] % 2) * 2]))

    def ffn_prefix():
        wd0 = ar.alloc([128, NFT, 512], BF16, "f_wd0")
        hnc0 = ar.alloc([128, 16, 512], BF16, "f_hnc0")
        wgA = ar.alloc([128, 16, 256], BF16, "f_wgA")
        wuA = ar.alloc([128, 16, 256], BF16, "f_wuA")
        return wd0, hnc0, wgA, wuA

    def load_hn_chunk(dst, sci, q4):
        t0 = sci * 2048
        dma("sp", dst.ap, HNT.ap[:, :, t0 + q4 * 512:t0 + (q4 + 1) * 512].rearrange("c p t -> p c t"),
            [HNT_b[sci * 16 + q4 * 4 + j] for j in range(4)], [dst])

    def load_wd_block(w, wd_ap, wsrc, db):
        for q4 in range(4):
            dma("pool", w.ap[:, q4 * 14:(q4 + 1) * 14, :],
                wd_ap[q4 * 14 * 128:(q4 + 1) * 14 * 128, db * 512:(db + 1) * 512].rearrange("(c p) n -> p c n", p=128), [wsrc], [w])

    def load_wgu_block(wg_, wu_, wg_ap, wu_ap, wsrc, fb):
        dma("pool", wg_.ap, wg_ap[:, fb * 256:(fb + 1) * 256].rearrange("(c p) n -> p c n", p=128), [wsrc], [wg_])
        dma("pool", wu_.ap, wu_ap[:, fb * 256:(fb + 1) * 256].rearrange("(c p) n -> p c n", p=128), [wsrc], [wu_])

    def ffn_up(sci, wg_ap, wu_ap, wsrc, pre_loaded=False, next_down=None):
        wd0, hnc0, wgA, wuA = ffn_prefix()
        hnc = [hnc0] + [ar.alloc([128, 16, 512], BF16, "fu_hn%d" % i) for i in range(1, 4)]
        wgb = [wgA] + [ar.alloc([128, 16, 256], BF16, "fu_wg%d" % i) for i in range(1, 3)]
        wub = [wuA] + [ar.alloc([128, 16, 256], BF16, "fu_wu%d" % i) for i in range(1, 3)]
        sg = [ar.alloc([128, 512], F32, "fu_sg%d" % i) for i in range(2)]
        ab = [ar.alloc([128, 512], BF16, "fu_ab%d" % i) for i in range(4)]
        for q4 in range(4):
            if q4 == 0 and pre_loaded:
                continue
            load_hn_chunk(hnc[q4], sci, q4)
        k = 0
        nfb = NFT // 2
        for fb in range(nfb):
            wg_, wu_ = wgb[fb % 3], wub[fb % 3]
            if not (fb == 0 and pre_loaded):
                load_wgu_block(wg_, wu_, wg_ap, wu_ap, wsrc, fb)
            if fb == nfb - 1 and next_down is not None:
                load_wd_block(wd0, next_down[0], next_down[1], 0)
            for f2 in range(2):
                ft = fb * 2 + f2
                for ch in range(4):
                    pg, pu = ps[(2 * k) % 8], ps[(2 * k + 1) % 8]
                    s_, a_ = sg[k % 2], ab[k % 4]
                    hn = hnc[ch]
                    k += 1
                    for c in range(16):
                        op("pe", lambda e, c=c, pg=pg, wg_=wg_, f2=f2, hn=hn: e.matmul(
                            pg.ap, wg_.ap[:, c, f2 * 128:(f2 + 1) * 128], hn.ap[:, c, :], start=(c == 0), stop=(c == 15)),
                           [wg_, hn], [pg])
                    for c in range(16):
                        op("pe", lambda e, c=c, pu=pu, wu_=wu_, f2=f2, hn=hn: e.matmul(
                            pu.ap, wu_.ap[:, c, f2 * 128:(f2 + 1) * 128], hn.ap[:, c, :], start=(c == 0), stop=(c == 15)),
                           [wu_, hn], [pu])
                    op("act", lambda e, s_=s_, pg=pg: e.activation(out=s_.ap, in_=pg.ap, func=AF.Silu), [pg], [s_])
                    op("dve", lambda e, s_=s_, pu=pu, a_=a_: e.tensor_tensor(a_.ap, s_.ap, pu.ap, ALU.mult), [s_, pu], [a_])
                    dma("sp", ACTT.ap[ch * 4:(ch + 1) * 4, :, ft, :].rearrange("t p k -> p t k"),
                        a_.ap.rearrange("p (t k) -> p t k", t=4), [a_], [ACTT])

    def ffn_down(sci, wd_ap, wsrc, gate_e, pre_loaded=False, next_up=None):
        wd0, hnc0, wgA, wuA = ffn_prefix()
        wd = [wd0, ar.alloc([128, NFT, 512], BF16, "fd_w1")]
        at = [ar.alloc([128, NFT, 128], BF16, "fd_a%d" % i) for i in range(2)]
        hb = [ar.alloc([128, 512], F32, "fd_h%d" % i) for i in range(3)]
        gt = None
        if gate_e is not None:
            gt = ar.alloc([128, 16, 8], F32, "fd_g")
            dma("sp", gt.ap, GATES.ap.rearrange("(t p) e -> p t e", p=128), [GATES], [gt])

        its = [(db, tt) for db in range(4) for tt in range(16)]

        def loads(k):
            db, tt = its[k]
            gtt = sci * 16 + tt
            dma("sp", at[k % 2].ap, ACTT.ap[tt], [ACTT], [at[k % 2]])
            dma("sp", hb[k % 3].ap, Hres.ap[gtt * 128:(gtt + 1) * 128, db * 512:(db + 1) * 512], [H_b[gtt]], [hb[k % 3]])

        def compute(k):
            db, tt = its[k]
            gtt = sci * 16 + tt
            a_, h, pb, w = at[k % 2], hb[k % 3], ps[k % 8], wd[db % 2]
            for ft in range(NFT):
                op("pe", lambda e, ft=ft: e.matmul(pb.ap, a_.ap[:, ft, :], w.ap[:, ft, :], start=(ft == 0), stop=(ft == NFT - 1)),
                   [a_, w], [pb])
            if gate_e is None:
                op("dve", lambda e: e.tensor_tensor(h.ap, h.ap, pb.ap, ALU.add), [h, pb], [h])
            else:
                op("dve", lambda e: e.scalar_tensor_tensor(h.ap, pb.ap, gt.ap[:, tt, gate_e:gate_e + 1], h.ap, ALU.mult, ALU.add),
                   [h, pb, gt], [h])
            dma("sp", Hres.ap[gtt * 128:(gtt + 1) * 128, db * 512:(db + 1) * 512], h.ap, [h], [H_b[gtt]])

        if not pre_loaded:
            load_wd_block(wd[0], wd_ap, wsrc, 0)
        load_wd_block(wd[1], wd_ap, wsrc, 1)
        loads(0)
        for k, (db, tt) in enumerate(its):
            if k + 1 < len(its):
                loads(k + 1)
            compute(k)
            if tt == 15 and db + 2 < 4:
                load_wd_block(wd[db % 2], wd_ap, wsrc, db + 2)
                if db == 1 and next_up is not None:
                    sci2, wg2, wu2, wsrc2 = next_up
                    load_wgu_block(wgA, wuA, wg2, wu2, wsrc2, 0)
                    load_hn_chunk(hnc0, sci2, 0)

    def phase_ple(L):
        ntt = 32 if L == 0 else 16
        pg = ar.alloc([128, 16, D], BF16, "pl_pg")
        for c4 in range(4):
            dma("pool", pg.ap[:, c4 * 4:(c4 + 1) * 4, :], plegate.ap[L, c4 * 512:(c4 + 1) * 512, :].rearrange("(c p) n -> p c n", p=128), [plegate], [pg])
        pp = ar.alloc([128, 2, D], BF16, "pl_pp")
        dma("pool", pp.ap, pleproj.ap[L].rearrange("(c p) n -> p c n", p=128), [pleproj], [pp])
        nc1 = NormCtx()
        nc2 = NormCtx() if L == 0 else None
        hb = [ar.alloc([128, D], F32, "pl_h%d" % i) for i in range(3)]
        ptb = [ar.alloc([128, 2, 128], BF16, "pl_pt%d" % i) for i in range(3)]
        sg = [ar.alloc([128, 512], F32, "pl_sg%d" % i) for i in range(2)]
        if L == 1:
            gfin = ar.alloc([128, D], F32, "pl_gfin")
            dma("sp", gfin.ap, finalg.ap.to_broadcast([128, D]), [finalg], [gfin])
            ob = [ar.alloc([128, D], F32, "pl_ob%d" % i) for i in range(2)]
            ss = [ar.alloc([128, 2], F32, "pl_ss%d" % i) for i in range(2)]
            junk = ar.alloc([128, D], BF16, "pl_junk")
        k = 0
        def pl_loads(tt):
            h, pt = hb[tt % 3], ptb[tt % 3]
            dma("sp", h.ap, Hres.ap[tt * 128:(tt + 1) * 128, :], [H_b[tt]], [h])
            dma("pool", pt.ap, pT_in.ap[L, :, tt * 128:(tt + 1) * 128].rearrange("(c p) t -> p c t", p=128), [pT_in], [pt])

        pl_loads(0)
        pl_loads(1)
        hT_next = nc1.trans(nc1.stats(hb[0]), 3 * L + 2, 0, (ps[6], ps[7]), to_hnt=False)
        prev2 = None
        for tt in range(ntt):
            h, pt = hb[tt % 3], ptb[tt % 3]
            hT = hT_next
            if tt + 1 < ntt:
                hT_next = nc1.trans(nc1.stats(hb[(tt + 1) % 3]), 3 * L + 2, tt + 1, (ps[6], ps[7]), to_hnt=False)
            for db in range(4):
                pG, pE = ps[(2 * k) % 6], ps[(2 * k + 1) % 6]
                s_ = sg[k % 2]
                k += 1
                for c in range(16):
                    op("pe", lambda e, c=c, pG=pG, hT=hT, db=db: e.matmul(pG.ap, hT.ap[:, c, :], pg.ap[:, c, db * 512:(db + 1) * 512],
                                                                          start=(c == 0), stop=(c == 15)), [hT, pg], [pG])
                for c in range(2):
                    op("pe", lambda e, c=c, pE=pE, pt=pt, db=db: e.matmul(pE.ap, pt.ap[:, c, :], pp.ap[:, c, db * 512:(db + 1) * 512],
                                                                          start=(c == 0), stop=(c == 1)), [pt, pp], [pE])
                op("act", lambda e, s_=s_, pG=pG: e.activation(out=s_.ap, in_=pG.ap, func=AF.Sigmoid), [pG], [s_])
                op("dve", lambda e, s_=s_, pE=pE: e.tensor_tensor(s_.ap, s_.ap, pE.ap, ALU.mult), [s_, pE], [s_])
                op("dve", lambda e, s_=s_, h=h, db=db: e.tensor_tensor(h.ap[:, db * 512:(db + 1) * 512], h.ap[:, db * 512:(db + 1) * 512], s_.ap, ALU.add),
                   [s_, h], [h])
            if tt + 2 < ntt:
                pl_loads(tt + 2)
            if L == 0:
                dma("sp", Hres.ap[tt * 128:(tt + 1) * 128, :], h.ap, [h], [H_b[tt]])
                ctx2 = nc2.stats(h)
                if prev2 is not None:
                    nc2.trans(prev2[0], 3, prev2[1], (ps[6], ps[7]))
                prev2 = (ctx2, tt)
            else:
                s2, o = ss[tt % 2], ob[tt % 2]
                op("act", lambda e, s2=s2, h=h: e.activation(out=junk.ap, in_=h.ap, func=AF.Square, accum_out=s2.ap[:, 0:1]), [h], [junk, s2])
                op("act", lambda e, s2=s2: e.activation(out=s2.ap[:, 1:2], in_=s2.ap[:, 0:1], func=AF.Sqrt, scale=1.0 / D, bias=eps_t.ap), [s2, eps_t], [s2])
                op("dve", lambda e, s2=s2: e.reciprocal(s2.ap[:, 1:2], s2.ap[:, 1:2]), [s2], [s2])
                op("dve", lambda e, s2=s2, o=o, h=h: e.scalar_tensor_tensor(o.ap, h.ap, s2.ap[:, 1:2], gfin.ap, ALU.mult, ALU.mult), [h, s2, gfin], [o])
                dma("sp", out_ap.ap[tt * 128:(tt + 1) * 128, :], o.ap, [o], [out_ap])
        if prev2 is not None:
            nc2.trans(prev2[0], 3, prev2[1], (ps[6], ps[7]))

    def run_all():
        phase_prologue()
        if P.phase_end("prologue"):
            return
        for L in range(2):
            phase_inproj(L)
            if P.phase_end("inproj%d" % L):
                return
            phase_mla_prep(L)
            if P.phase_end("mlaprep%d" % L):
                return
            for kind in ("diff", "na", "swa", "mla"):
                attention(L, kind)
                if P.phase_end("%s%d" % (kind, L)):
                    return
            phase_outproj(L)
            if P.phase_end("outproj%d" % L):
                return
            if L == 0:
                for sci in range(2):
                    ffn_up(sci, dwg.ap, dwu.ap, dwg, pre_loaded=(sci == 1), next_down=(dwd.ap, dwd))
                    if P.phase_end("ffnup%d_%d" % (L, sci)):
                        return
                    ffn_down(sci, dwd.ap, dwd, None, pre_loaded=True,
                             next_up=((1, dwg.ap, dwu.ap, dwg) if sci == 0 else None))
                    if P.phase_end("ffndown%d_%d" % (L, sci)):
                        return
            else:
                for e_ in range(8):
                    ffn_up(0, mwg.ap[e_], mwu.ap[e_], mwg, pre_loaded=(e_ > 0), next_down=(mwd.ap[e_], mwd))
                    if P.phase_end("moeup%d" % e_):
                        return
                    ffn_down(0, mwd.ap[e_], mwd, e_, pre_loaded=True,
                             next_up=((0, mwg.ap[e_ + 1], mwu.ap[e_ + 1], mwg) if e_ < 7 else None))
                    if P.phase_end("moedown%d" % e_):
                        return
            phase_ple(L)
            if P.phase_end("ple%d" % L):
                return

    run_all()
    sc.barrier()
    sc.emit()
    return P


def _rope_tables(pos):
    pos = pos.astype(np.float32)
    f = np.arange(128)

    def tab(dim, fidx, sign):
        inv = (10000.0 ** (-np.arange(0, dim, 2, dtype=np.float32) / dim)).astype(np.float32)
        ang = pos[None, :] * inv[fidx][:, None]
        return np.stack([np.cos(ang), np.sin(ang) * sign[:, None]]).astype(np.float32)

    ropeD = tab(64, (f % 64) % 32, np.where((f % 64) < 32, -1.0, 1.0).astype(np.float32))
    ropeH = tab(128, f % 64, np.where(f < 64, -1.0, 1.0).astype(np.float32))
    return ropeD, ropeH


def _na_tables(rpb, half):
    out = np.full((8, 4, 8, 128, 512), NEG, dtype=np.float32)
    ki = np.arange(128)
    qi = np.arange(512)
    kc = ki % 64
    qr_l = qi // 64
    qcol = qi % 64
    ws = np.clip(qcol - 8, 0, 48)
    colok = (kc[:, None] >= ws[None, :]) & (kc[:, None] < ws[None, :] + 16)
    cidx = np.clip(kc[:, None] - qcol[None, :], -15, 15) + 15
    for qc in range(8):
        R0 = (8 * qc + 32 * half) % 64
        r = R0 + qr_l
        rs = np.clip(r - 4, 0, 56)
        for j in range(8):
            kr0 = R0 - 4 + 2 * j
            if kr0 < 0 or kr0 > 62:
                continue
            kr = kr0 + ki // 64
            rowok = (kr[:, None] >= rs[None, :]) & (kr[:, None] < rs[None, :] + 8)
            ridx = np.clip(kr[:, None] - r[None, :] + 7, 0, 14)
            ok = rowok & colok
            for h in range(4):
                out[qc, h, j] = np.where(ok, rpb[h][ridx, cidx], NEG)
    return out.astype(ml_dtypes.bfloat16)


def _swa_tables(half):
    out = np.full((8, 6, 128, 512), NEG, dtype=np.float32)
    ki = np.arange(128)
    qi = np.arange(512)
    for qc in range(8):
        Q0 = (512 * qc + 2048 * half) % 4096
        for j in range(6):
            k0 = Q0 - 128 + 128 * j
            if k0 < 0 or k0 >= 4096:
                continue
            ok = np.abs((Q0 + qi)[None, :] - (k0 + ki)[:, None]) <= 128
            out[qc, j] = np.where(ok, 0.0, NEG)
    return out.astype(ml_dtypes.bfloat16)


def _fm_cols(g):
    return np.ascontiguousarray(g.reshape(16, 128).T)


def make_in_maps(inputs, cores=range(8)):
    f32 = lambda a: np.ascontiguousarray(a, dtype=np.float32)
    I = inputs
    shared = {}
    shared["wext"] = f32(I["w_in"][:, :, WIN_COLS])
    shared["wout"] = f32(I["w_out"])
    shared["wuq"] = f32(I["mla_w_uq"][:, :, UQ_COLS])
    shared["wukv"] = f32(I["mla_w_ukv"][:, :, UKV_COLS])
    shared["dwg"] = f32(I["dense_w_gate"][0])
    shared["dwu"] = f32(I["dense_w_up"][0])
    shared["dwd"] = f32(I["dense_w_down"][0])
    shared["mwg"] = f32(I["moe_w_gate"][0])
    shared["mwu"] = f32(I["moe_w_up"][0])
    shared["mwd"] = f32(I["moe_w_down"][0])
    shared["routerT"] = f32(I["moe_router"][0].T)
    shared["plegate"] = f32(I["ple_gate"])
    shared["pleproj"] = f32(I["ple_proj"])
    gl = []
    for L in range(2):
        gl += [_fm_cols(I["attn_norm"][L]), _fm_cols(I["ffn_norm"][L]), _fm_cols(I["ple_norm"][L])]
    shared["gains"] = f32(np.stack(gl))
    shared["ffng1"] = f32(I["ffn_norm"][1][None, :])
    shared["finalg"] = f32(I["final_norm"][None, :])
    shared["mlan"] = f32(np.stack([np.concatenate([I["mla_q_norm"][L].reshape(4, 128).T, I["mla_kv_norm"][L].reshape(4, 128).T], axis=1)
                                   for L in range(2)]))
    shared["subln"] = f32(I["diff_subln"][:, :, None])
    shared["lamv"] = f32(np.stack([I["diff_lq1"], I["diff_lk1"], I["diff_lq2"], I["diff_lk2"]], axis=1))
    shared["sinks"] = f32(I["swa_sinks"])
    shared["ident"] = np.eye(128, dtype=np.float32).astype(ml_dtypes.bfloat16)
    per_half = {}
    for half in range(2):
        pos = (np.arange(S) + 2048 * half) % S
        ropeD, ropeH = _rope_tables(pos)
        per_half[half] = dict(
            ropeD=ropeD, ropeH=ropeH,
            natab=np.stack([_na_tables(np.asarray(I["na_rpb"][L], dtype=np.float32), half) for L in range(2)]),
            swatab=_swa_tables(half),
        )
    maps = []
    for c in cores:
        b, half = c // 2, c % 2
        m = dict(shared)
        m.update(per_half[half])
        m["x"] = f32(np.roll(I["x"][b], -2048 * half, axis=0))
        m["pT"] = f32(np.stack([np.roll(I["p"][L, b], -2048 * half, axis=0).T for L in range(2)]))
        maps.append(m)
    return maps


_PROG = None


def kernel(**inputs):
    global _PROG
    if _PROG is None:
        _PROG = build_program()
    P = _PROG
    maps = make_in_maps(inputs)
    maps = [{k: v for k, v in m.items() if k in P.inputs} for m in maps]
    res = run_bass_kernel_spmd(P.nc, maps, core_ids=list(range(8)))
    out = np.empty((4, S, D), dtype=np.float32)
    for c in range(8):
        b, half = c // 2, c % 2
        out[b, 2048 * half:2048 * (half + 1)] = np.asarray(res.results[c]["out"], dtype=np.float32)
    return out
```
